# Optimizing a Trainium2 kernel written in Bass

```python
import jax
import jax.numpy as jnp
from jax import lax
import numpy as np

D_MODEL = 1024
BATCH = 4
SEQ = 8192
DEPTH = 4

CHUNK = 64
N_MIXERS = 3
PLE_DIM = 256
EPS = 1e-6

HG_HEADS = D_MODEL // 128
HG_DK = D_MODEL // HG_HEADS
HG_DV = D_MODEL // HG_HEADS

FOX_HEADS = D_MODEL // 64
FOX_HD = D_MODEL // FOX_HEADS
Q_BLOCK = 128

RG_WIDTH = D_MODEL
RG_BLOCKS = 4
RG_BW = RG_WIDTH // RG_BLOCKS
CONV_W = 4
RG_C = 8.0

N_EXPERTS = 32
TOP_K = 4
D_EXPERT = D_MODEL
SWIGLU_LIMIT = 7.0
SWIGLU_ALPHA = 1.702
EXPERT_BLOCK = 128

N_HG = (DEPTH + 2) // 3
N_FOX = (DEPTH + 1) // 3
N_RG = DEPTH // 3

kernel_name = 'hybrid_hgrn2_fox_rglru_moe'


def rmsnorm(x, g):
    xf = x.astype(jnp.float32)
    y = xf * lax.rsqrt(jnp.mean(xf * xf, axis=-1, keepdims=True) + EPS)
    return (y * g.astype(jnp.float32)).astype(x.dtype)


def hgrn2_mixer(xn, w_in, w_out, g_norm, lb):
    b, s, _ = xn.shape
    nc = s // CHUNK
    q, z, v, g = jnp.split(xn @ w_in, 4, axis=-1)
    q = jax.nn.silu(q.astype(jnp.float32)) * HG_DK ** -0.5
    z = z.astype(jnp.float32)
    lbf = lb.astype(jnp.float32)
    log_f = jnp.logaddexp(jnp.log(lbf), jnp.log1p(-lbf) + jax.nn.log_sigmoid(z))
    k = (1.0 - lbf) * jax.nn.sigmoid(-z)

    def to_chunks(t, dh):
        return t.reshape(b, nc, CHUNK, HG_HEADS, dh).transpose(1, 0, 3, 2, 4)

    qc, kc, lfc = to_chunks(q, HG_DK), to_chunks(k, HG_DK), to_chunks(log_f, HG_DK)
    vc = to_chunks(v.astype(jnp.float32), HG_DV)
    causal = jnp.tril(jnp.ones((CHUNK, CHUNK), dtype=bool))

    def step(state, inp):
        qi, ki, vi, lfi = inp
        cum = jnp.cumsum(lfi, axis=2)
        diff = cum[:, :, :, None, :] - cum[:, :, None, :, :]
        decay = jnp.exp(jnp.where(causal[:, :, None], diff, -jnp.inf))
        scores = jnp.einsum('bhtd,bhtsd,bhsd->bhts', qi, decay, ki)
        o = (jnp.einsum('bhts,bhsv->bhtv', scores, vi)
             + jnp.einsum('bhtd,bhdv->bhtv', qi * jnp.exp(cum), state))
        last = cum[:, :, -1:, :]
        state = (jnp.exp(last[:, :, 0, :, None]) * state
                 + jnp.einsum('bhsd,bhsv->bhdv', ki * jnp.exp(last - cum), vi))
        return state, o

    state0 = jnp.zeros((b, HG_HEADS, HG_DK, HG_DV), jnp.float32)
    _, o = lax.scan(step, state0, (qc, kc, vc, lfc))
    o = o.transpose(1, 0, 3, 2, 4).reshape(b, s, HG_HEADS, HG_DV)
    o = rmsnorm(o, g_norm) * jax.nn.silu(g.astype(jnp.float32)).reshape(b, s, HG_HEADS, HG_DV)
    return o.reshape(b, s, D_MODEL).astype(xn.dtype) @ w_out


def fox_mixer(xn, w_in, f_bias, q_norm, k_norm, w_out):
    b, s, _ = xn.shape
    proj = xn @ w_in
    q, k, v, g = jnp.split(proj[..., :4 * D_MODEL], 4, axis=-1)
    f_logit = (proj[..., 4 * D_MODEL:] + f_bias).astype(jnp.float32)
    q = rmsnorm(q.reshape(b, s, FOX_HEADS, FOX_HD), q_norm).transpose(0, 2, 1, 3)
    k = rmsnorm(k.reshape(b, s, FOX_HEADS, FOX_HD), k_norm).transpose(0, 2, 1, 3)
    v = v.reshape(b, s, FOX_HEADS, FOX_HD).transpose(0, 2, 1, 3)
    cum = jnp.cumsum(jax.nn.log_sigmoid(f_logit), axis=1).transpose(0, 2, 1)
    scale = FOX_HD ** -0.5
    outs = []
    for blk in range(s // Q_BLOCK):
        q0, q1 = blk * Q_BLOCK, (blk + 1) * Q_BLOCK
        logits = jnp.einsum('bhqd,bhkd->bhqk', q[:, :, q0:q1], k[:, :, :q1]).astype(jnp.float32) * scale
        logits = logits + cum[:, :, q0:q1, None] - cum[:, :, None, :q1]
        mask = jnp.arange(q1)[None, :] <= jnp.arange(q0, q1)[:, None]
        probs = jax.nn.softmax(jnp.where(mask, logits, -jnp.inf), axis=-1)
        outs.append(jnp.einsum('bhqk,bhkd->bhqd', probs.astype(v.dtype), v[:, :, :q1]))
    o = jnp.concatenate(outs, axis=2).transpose(0, 2, 1, 3).reshape(b, s, D_MODEL)
    return (o * jax.nn.sigmoid(g)) @ w_out


def _linear_combine(left, right):
    a_l, b_l = left
    a_r, b_r = right
    return a_l * a_r, a_r * b_l + b_r


def rglru_mixer(xn, w_in, conv_w, conv_b, w_a, b_a, w_x, b_x, lam, w_out):
    b, s, _ = xn.shape
    gate, u = jnp.split(xn @ w_in, 2, axis=-1)
    gate = jax.nn.gelu(gate, approximate=True)
    up = jnp.pad(u, ((0, 0), (CONV_W - 1, 0), (0, 0)))
    u = conv_b + up[:, 0:s] * conv_w[0]
    for tap in range(1, CONV_W):
        u = u + up[:, tap:tap + s] * conv_w[tap]
    ub = u.reshape(b, s, RG_BLOCKS, RG_BW)
    r = jax.nn.sigmoid(jnp.einsum('bsnd,nde->bsne', ub, w_a).reshape(b, s, RG_WIDTH) + b_a)
    i = jax.nn.sigmoid(jnp.einsum('bsnd,nde->bsne', ub, w_x).reshape(b, s, RG_WIDTH) + b_x)
    log_a = -RG_C * r.astype(jnp.float32) * jax.nn.softplus(-lam.astype(jnp.float32))
    a = jnp.exp(log_a)
    inp = jnp.sqrt(-jnp.expm1(2.0 * log_a)) * (i * u).astype(jnp.float32)
    _, h = lax.associative_scan(_linear_combine, (a, inp), axis=1)
    return (h.astype(xn.dtype) * gate) @ w_out


def moe(xn, w_r, b_r, w_gu, b_gu, w_dn, b_dn):
    b, s, d = xn.shape
    n = b * s
    xt = xn.reshape(n, d)
    logits = (xt @ w_r + b_r).astype(jnp.float32)
    top_val, top_idx = lax.top_k(logits, TOP_K)
    gates = jax.nn.softmax(top_val, axis=-1).astype(xn.dtype).reshape(-1)
    flat_e = top_idx.reshape(-1)
    order = jnp.argsort(flat_e)
    e_sorted = flat_e[order]
    counts = jnp.bincount(flat_e, length=N_EXPERTS)
    padded = (counts + EXPERT_BLOCK - 1) // EXPERT_BLOCK * EXPERT_BLOCK
    padded_end = jnp.cumsum(padded)
    start = jnp.cumsum(counts) - counts
    dest = padded_end[e_sorted] - padded[e_sorted] + jnp.arange(n * TOP_K) - start[e_sorted]
    n_blocks = -(-(n * TOP_K) // EXPERT_BLOCK) + N_EXPERTS
    cap = n_blocks * EXPERT_BLOCK
    buf_tok = jnp.full((cap,), n, jnp.int32).at[dest].set((order // TOP_K).astype(jnp.int32))
    buf_gate = jnp.zeros((cap,), xn.dtype).at[dest].set(gates[order])
    block_expert = jnp.minimum(
        jnp.searchsorted(padded_end, jnp.arange(n_blocks) * EXPERT_BLOCK, side='right'), N_EXPERTS - 1)
    xp = jnp.concatenate([xt, jnp.zeros((1, d), xt.dtype)], axis=0)

    def expert_block(args):
        tok, gate, e = args
        hgu = xp[tok] @ w_gu[e] + b_gu[e]
        glu, lin = jnp.split(hgu, 2, axis=-1)
        glu = jnp.minimum(glu, SWIGLU_LIMIT)
        lin = jnp.clip(lin, -SWIGLU_LIMIT, SWIGLU_LIMIT)
        act = glu * jax.nn.sigmoid(SWIGLU_ALPHA * glu) * (lin + 1.0)
        return (act @ w_dn[e] + b_dn[e]) * gate[:, None]

    ys = lax.map(expert_block, (buf_tok.reshape(n_blocks, EXPERT_BLOCK),
                                buf_gate.reshape(n_blocks, EXPERT_BLOCK), block_expert))
    out = jnp.zeros((n + 1, d), xn.dtype).at[buf_tok].add(ys.reshape(cap, d))
    return out[:n].reshape(b, s, d)


def setup_inputs(seed: int = 0) -> dict:
    key = jax.random.key(seed)
    ks = iter(jax.random.split(key, 40))
    d = D_MODEL
    out_scale = d ** -0.5 * (2 * DEPTH) ** -0.5

    def nrm(shape, scale):
        return jax.random.normal(next(ks), shape, jnp.float32) * scale

    def gain(shape):
        return 1.0 + nrm(shape, 0.05)

    x = nrm((BATCH, SEQ, d), 1.0)
    p = nrm((DEPTH, BATCH, SEQ, PLE_DIM), 1.0)
    norm_mix = gain((DEPTH, d))
    norm_ffn = gain((DEPTH, d))
    hg_w_in = nrm((N_HG, d, 4 * d), d ** -0.5)
    hg_w_out = nrm((N_HG, d, d), out_scale)
    hg_gnorm = gain((N_HG, HG_DV))
    hg_lb_param = nrm((DEPTH, d), 0.5)
    fox_w_in = nrm((N_FOX, d, 4 * d + FOX_HEADS), d ** -0.5)
    fox_f_bias = 2.0 + nrm((N_FOX, FOX_HEADS), 0.5)
    fox_qnorm = gain((N_FOX, FOX_HD))
    fox_knorm = gain((N_FOX, FOX_HD))
    fox_w_out = nrm((N_FOX, d, d), out_scale)
    rg_w_in = nrm((N_RG, d, 2 * RG_WIDTH), d ** -0.5)
    rg_conv_w = nrm((N_RG, CONV_W, RG_WIDTH), CONV_W ** -0.5)
    rg_conv_b = nrm((N_RG, RG_WIDTH), 0.02)
    rg_wa = nrm((N_RG, RG_BLOCKS, RG_BW, RG_BW), RG_BW ** -0.5)
    rg_ba = nrm((N_RG, RG_WIDTH), 0.1)
    rg_wx = nrm((N_RG, RG_BLOCKS, RG_BW, RG_BW), RG_BW ** -0.5)
    rg_bx = nrm((N_RG, RG_WIDTH), 0.1)
    a_c = jax.random.uniform(next(ks), (N_RG, RG_WIDTH), jnp.float32, 0.9, 0.999)
    sig = a_c ** (1.0 / RG_C)
    rg_lambda = jnp.log(sig) - jnp.log1p(-sig)
    rg_w_out = nrm((N_RG, RG_WIDTH, d), out_scale)
    router_w = nrm((DEPTH, d, N_EXPERTS), d ** -0.5)
    router_b = nrm((DEPTH, N_EXPERTS), 0.01)
    moe_w_gu = nrm((DEPTH, N_EXPERTS, d, 2 * D_EXPERT), d ** -0.5)
    moe_b_gu = nrm((DEPTH, N_EXPERTS, 2 * D_EXPERT), 0.02)
    moe_w_dn = nrm((DEPTH, N_EXPERTS, D_EXPERT, d), D_EXPERT ** -0.5 * (2 * DEPTH) ** -0.5)
    moe_b_dn = nrm((DEPTH, N_EXPERTS, d), 0.02)
    ple_w = nrm((DEPTH, PLE_DIM, d), PLE_DIM ** -0.5)
    ple_norm = gain((DEPTH, d))
    ple_gate_norm = gain((DEPTH, d))
    ple_gate_w = nrm((DEPTH, d, d), d ** -0.5)
    return {'x': x, 'p': p, 'norm_mix': norm_mix, 'norm_ffn': norm_ffn,
            'hg_w_in': hg_w_in, 'hg_w_out': hg_w_out, 'hg_gnorm': hg_gnorm, 'hg_lb_param': hg_lb_param,
            'fox_w_in': fox_w_in, 'fox_f_bias': fox_f_bias, 'fox_qnorm': fox_qnorm, 'fox_knorm': fox_knorm,
            'fox_w_out': fox_w_out,
            'rg_w_in': rg_w_in, 'rg_conv_w': rg_conv_w, 'rg_conv_b': rg_conv_b, 'rg_wa': rg_wa, 'rg_ba': rg_ba,
            'rg_wx': rg_wx, 'rg_bx': rg_bx, 'rg_lambda': rg_lambda, 'rg_w_out': rg_w_out,
            'router_w': router_w, 'router_b': router_b, 'moe_w_gu': moe_w_gu, 'moe_b_gu': moe_b_gu,
            'moe_w_dn': moe_w_dn, 'moe_b_dn': moe_b_dn,
            'ple_w': ple_w, 'ple_norm': ple_norm, 'ple_gate_norm': ple_gate_norm, 'ple_gate_w': ple_gate_w}


def reference(x, p, norm_mix, norm_ffn, hg_w_in, hg_w_out, hg_gnorm, hg_lb_param,
              fox_w_in, fox_f_bias, fox_qnorm, fox_knorm, fox_w_out,
              rg_w_in, rg_conv_w, rg_conv_b, rg_wa, rg_ba, rg_wx, rg_bx, rg_lambda, rg_w_out,
              router_w, router_b, moe_w_gu, moe_b_gu, moe_w_dn, moe_b_dn,
              ple_w, ple_norm, ple_gate_norm, ple_gate_w):
    lb_all = jnp.cumsum(jax.nn.softmax(hg_lb_param.astype(jnp.float32), axis=0), axis=0)
    lb_all = lb_all - lb_all[0]
    h = x
    for i in range(DEPTH):
        xn = rmsnorm(h, norm_mix[i])
        j = i // N_MIXERS
        kind = i % N_MIXERS
        if kind == 0:
            mix = hgrn2_mixer(xn, hg_w_in[j], hg_w_out[j], hg_gnorm[j], lb_all[i])
        elif kind == 1:
            mix = fox_mixer(xn, fox_w_in[j], fox_f_bias[j], fox_qnorm[j], fox_knorm[j], fox_w_out[j])
        else:
            mix = rglru_mixer(xn, rg_w_in[j], rg_conv_w[j], rg_conv_b[j], rg_wa[j], rg_ba[j],
                              rg_wx[j], rg_bx[j], rg_lambda[j], rg_w_out[j])
        h = h + mix
        h = h + moe(rmsnorm(h, norm_ffn[i]), router_w[i], router_b[i],
                    moe_w_gu[i], moe_b_gu[i], moe_w_dn[i], moe_b_dn[i])
        ple = (rmsnorm(p[i] @ ple_w[i], ple_norm[i])
               * jax.nn.sigmoid(rmsnorm(h, ple_gate_norm[i]) @ ple_gate_w[i]))
        h = h + ple
    return h
```

```python
import numpy as np
import concourse.bass as bass
import concourse.mybir as mybir
from concourse.bass_utils import run_bass_kernel_spmd
from contextlib import ExitStack

F32 = mybir.dt.float32
BF16 = mybir.dt.bfloat16
I32 = mybir.dt.int32
AF = mybir.ActivationFunctionType
ALU = mybir.AluOpType
AX = mybir.AxisListType


class _St:
    __slots__ = ("w", "r")

    def __init__(self):
        self.w = []
        self.r = {}


class Tile:
    def __init__(self, t, name):
        self.t = t
        self.name = name
        self.whole = _St()
        self.parts = {}
        self.psum = False

    def __getitem__(self, idx):
        return self.t[idx]


class Sem:
    def __init__(self, h, name, is_dma=False):
        self.h = h
        self.name = name
        self.is_dma = is_dma
        self.issued = 0
        self.last_wait = 0


class Eng:
    def __init__(self, P, name, e, sem):
        self.P = P
        self.name = name
        self.e = e
        self.sem = sem
        self.known = {}


class Prog:
    def __init__(self):
        self.nc = bass.Bass("TRN2", target_bir_lowering=False)
        self.es = ExitStack()
        self.gstack = self.es
        self.dsems = []
        nc = self.nc
        self.engs = {}
        for nm, e in (("pe", nc.tensor), ("dve", nc.vector), ("act", nc.scalar),
                      ("pool", nc.gpsimd), ("sp", nc.sync)):
            s = Sem(self.es.enter_context(nc.semaphore("s_" + nm)), "s_" + nm)
            self.engs[nm] = Eng(self, nm, e, s)
        self.n_inst = 0

    def dram(self, name, shape, dt, kind):
        return self.nc.dram_tensor(name, list(shape), dt, kind=kind)

    def sb(self, name, shape, dt):
        self.uid = getattr(self, "uid", 0) + 1
        name = f"{name}_u{self.uid}"
        t = self.es.enter_context(self.nc.sbuf_tensor(name, list(shape), dt))
        return Tile(t, name)

    def ps(self, name, shape, dt=F32):
        t = self.es.enter_context(self.nc.psum_tensor(name, list(shape), dt))
        tl = Tile(t, name)
        tl.psum = True
        return tl

    def dsem(self, name):
        return Sem(self.es.enter_context(self.nc.semaphore(name)), name, is_dma=True)

    @staticmethod
    def _norm(lst):
        out = []
        for x in lst or []:
            if x is None:
                continue
            if isinstance(x, tuple):
                out.append(x)
            else:
                out.append((x, None))
        return out

    def _deps(self, reads, writes):
        deps = []
        for (t, k) in reads:
            deps += t.whole.w
            if k is None:
                for st in t.parts.values():
                    deps += st.w
            elif k in t.parts:
                deps += t.parts[k].w
            if t.psum:
                deps += list(t.whole.r.items())
                for st in t.parts.values():
                    deps += list(st.r.items())
        for (t, k) in writes:
            deps += t.whole.w + list(t.whole.r.items())
            if k is None:
                for st in t.parts.values():
                    deps += st.w + list(st.r.items())
            elif k in t.parts:
                deps += t.parts[k].w + list(t.parts[k].r.items())
        return deps

    def _mark(self, reads, writes, tok):
        for (t, k) in reads:
            st = t.whole if k is None else t.parts.setdefault(k, _St())
            if st.r.get(tok[0], 0) < tok[1]:
                st.r[tok[0]] = tok[1]
        for (t, k) in writes:
            if k is None:
                t.whole.w = [tok]
                t.whole.r = {}
                t.parts = {}
            else:
                st = t.parts.setdefault(k, _St())
                st.w = [tok]
                st.r = {}

    def _wait(self, E, deps, skip_self=False):
        need = {}
        for (s, v) in deps:
            if skip_self and s is E.sem:
                continue
            if s.is_dma:
                assert v == s.issued or E.known.get(s, 0) >= v or True
                v = s.issued
            if v > need.get(s, 0):
                need[s] = v
        for s, v in need.items():
            if E.known.get(s, 0) < v:
                E.e.wait_ge(s.h, v)
                E.known[s] = v
                if s.is_dma and v > s.last_wait:
                    s.last_wait = v

    def op(self, eng, fn, reads=None, writes=None):
        E = self.engs[eng]
        reads = self._norm(reads)
        writes = self._norm(writes)
        deps = self._deps(reads, writes)
        self._wait(E, deps, skip_self=(eng == "pe"))
        inst = fn(E.e)
        E.sem.issued += 1
        inst.then_inc(E.sem.h, 1)
        self._mark(reads, writes, (E.sem, E.sem.issued))
        self.n_inst += 1
        return inst

    def dma(self, q, out, in_, sem, reads=None, writes=None, **kw):
        E = self.engs[q]
        reads = self._norm(reads)
        writes = self._norm(writes)
        deps = self._deps(reads, writes)
        self._wait(E, deps)
        if sem.last_wait > E.known.get(sem, 0):
            E.e.wait_ge(sem.h, sem.issued)
            E.known[sem] = sem.issued
        kind = "sw" if q == "pool" else "hw"
        assert getattr(sem, "qkind", kind) == kind, f"sem {sem.name} mixes SW and HW DGE"
        sem.qkind = kind
        inst = E.e.dma_start(out=out, in_=in_, **kw)
        sem.issued += 16
        inst.then_inc(sem.h, 16)
        self._mark(reads, writes, (sem, sem.issued))
        self.n_inst += 1
        return inst

    def finish(self, out_sems):
        E = self.engs["sp"]
        for s in out_sems:
            if E.known.get(s, 0) < s.issued:
                E.e.wait_ge(s.h, s.issued)
                E.known[s] = s.issued
        self.es.close()
        return self.nc


D = 1024
EPS = 1e-6


class Ctx:
    def __init__(self, P):
        self.P = P
        self.identf = P.sb("identf", [128, 128], F32)
        self.identb = P.sb("identb", [128, 128], BF16)
        P.op("pool", lambda e: e.memset(self.identf[:], 0.0), writes=[self.identf])
        P.op("pool", lambda e: e.affine_select(out=self.identf[:], in_=self.identf[:], pattern=[[-1, 128]],
                                               compare_op=ALU.not_equal, fill=1.0, base=0, channel_multiplier=1),
             reads=[self.identf], writes=[self.identf])
        P.op("dve", lambda e: e.tensor_copy(out=self.identb[:], in_=self.identf[:]),
             reads=[self.identf], writes=[self.identb])
        self.mm = [P.ps(f"mm{i}", [128, 512], F32) for i in range(4)]
        self.mm_i = 0
        self.trp = [P.ps(f"trp{i}", [128, 512], F32) for i in range(2)]
        self.trb = P.ps("trb", [128, 1024], BF16)
        self.small = P.ps("smallps", [128, 512], F32)
        self.junk = P.sb("junk", [128, 1024], F32)
        self.st = {}
        for nm, w in (("ss", 1), ("rstd", 1), ("negm", 1), ("ssum", 1), ("ssa", 2), ("ssa1", 1), ("rstda", 1), ("ss2", 1),
                      ("rstd2", 1)):
            self.st[nm] = P.sb("st_" + nm, [128, w], F32)

    def bank(self):
        b = self.mm[self.mm_i % 4]
        self.mm_i += 1
        return b

    def stat(self, name, w=1):
        return self.st[name]


def bc_load(P, q, name, src_ap, n, sem):
    t = P.sb(name, [128, n], F32)
    P.dma(q, t[:], src_ap.partition_broadcast(128), sem, writes=[t])
    return t


def emit_rstd(P, C, ss, rstd, n, p=128):
    (sst, ssa), (rt, ra) = ss, rstd
    P.op("dve", lambda e: e.tensor_scalar(out=ra, in0=ssa, scalar1=1.0 / n, scalar2=EPS,
                                          op0=ALU.mult, op1=ALU.add), reads=[sst], writes=[rt])
    P.op("act", lambda e: e.activation(out=ra, in_=ra, func=AF.Sqrt), reads=[rt], writes=[rt])
    P.op("dve", lambda e: e.reciprocal(out=ra, in_=ra), reads=[rt], writes=[rt])


def load_w_bf16(P, dst, dst_ap, src_ap, sem, key=None):
    P.dma("pool", dst_ap, src_ap.rearrange("(k p) n -> p k n", p=128), sem, writes=[(dst, key)])


GT = 512
NE = 32


def emit_ffn(P, C, NT, h_in, parts, p_in, norm_ffn, router_w, router_b, w_gu, b_gu, w_dn, b_dn, ple_w, ple_norm,
             ple_gate_norm, ple_gate_w, h_out, n_exp=NE, stop=None):
    n_part = len(parts)
    with Phase(P):
        return _emit_ffn(P, C, NT, h_in, parts, p_in, norm_ffn, router_w, router_b, w_gu, b_gu, w_dn, b_dn, ple_w, ple_norm,
                         ple_gate_norm, ple_gate_w, h_out, n_exp, stop, n_part)


def _emit_ffn(P, C, NT, h_in, parts, p_in, norm_ffn, router_w, router_b, w_gu, b_gu, w_dn, b_dn, ple_w, ple_norm,
              ple_gate_norm, ple_gate_w, h_out, n_exp, stop, n_part):
    s_c = P.dsem("d_const")
    s_out = P.dsem("d_out")
    g_ffn = bc_load(P, "sp", "g_ffn", norm_ffn, D, s_c)
    g_ple = bc_load(P, "sp", "g_ple", ple_norm, D, s_c)
    g_gate = bc_load(P, "sp", "g_gate", ple_gate_norm, D, s_c)
    br_bc = bc_load(P, "sp", "br_bc", router_b, NE, s_c)
    wr_sb = P.sb("wr_sb", [128, 8, NE], F32)
    P.dma("sp", wr_sb[:], router_w.rearrange("(k p) n -> p k n", p=128), s_c, writes=[wr_sb])
    s_cw = P.dsem("d_constw")
    plew = P.sb("plew", [128, 2, D], BF16)
    load_w_bf16(P, plew, plew[:], ple_w, s_cw)
    plegw = P.sb("plegw", [128, 8, D], BF16)
    load_w_bf16(P, plegw, plegw[:], ple_gate_w, s_cw)
    big = [P.sb(f"big{i}", [128, D], F32) for i in range(3)]
    s_b = P.dsem("d_brow")
    P.dma("sp", big[0][0:NE, :], b_gu[:, 0:D], s_b, writes=[big[0]])
    P.dma("sp", big[1][0:NE, :], b_gu[:, D:2 * D], s_b, writes=[big[1]])
    P.dma("sp", big[2][0:NE, :], b_dn, s_b, writes=[big[2]])
    bfm = P.sb("bfm", [128, 24, NE], F32)
    for m in range(24):
        brow = big[m // 8]
        P.op("pe", lambda e: e.transpose(out=C.small[:, 0:NE], in_=brow[0:NE, (m % 8) * 128:(m % 8 + 1) * 128],
                                         identity=C.identf[0:NE, 0:NE]),
             reads=[brow, C.identf], writes=[C.small])
        P.op("dve", lambda e: e.tensor_copy(out=bfm[:, m, :], in_=C.small[:, 0:NE]),
             reads=[C.small], writes=[(bfm, m)])

    if stop == "setup":
        P.dma("sp", h_out[0:128, 0:24 * NE], bfm[:].rearrange("p m e -> p (m e)"), s_out, reads=[bfm])
        return None
    NS = 8
    wslot = [P.sb(f"wslot{i}", [128, 8, 512], BF16) for i in range(NS)]
    wsem = [P.dsem(f"d_w{i}") for i in range(NS)]
    hsem = [P.dsem(f"d_h{i}") for i in range(2)]
    pbuf = [P.sb(f"pb{i}", [128, 256], F32) for i in range(2)]
    psem = [P.dsem(f"d_p{i}") for i in range(2)]
    h1 = P.sb("h1", [128, GT // 128, D], F32)
    xn = P.sb("xn", [128, D], F32)
    xnb = P.sb("xnb", [128, D], BF16)
    xnT = P.sb("xnT", [128, 8, GT], BF16)
    xnTf = P.sb("xnTf", [128, 8, 128], F32)
    G = P.sb("G", [NE, GT], F32)
    selt = P.sb("selt", [NE, 128], F32)
    Gbc = P.sb("Gbc", [128, GT], F32)
    actT = P.sb("actT", [128, 8, GT], BF16)
    yacc = P.sb("yacc", [128, 8, GT], F32)
    NEW = 6
    ew = [P.sb(f"ew{i}", [128, GT], F32) for i in range(NEW)]
    ew_i = [0]

    def tmp():
        t = ew[ew_i[0] % NEW]
        ew_i[0] += 1
        return t
    lg = P.sb("lg", [128, NE], F32)
    top8 = P.sb("top8", [128, 8], F32)
    msk = P.sb("msk", [128, NE], F32)
    exs = P.sb("exs", [128, NE], F32)
    pT = P.sb("pT", [128, 2, 128], BF16)
    hnT = P.sb("hnT", [128, 8, 128], BF16)
    otile = [P.sb(f"otile{i}", [128, D], F32) for i in range(1)]

    NG = NT // GT
    JT = GT // 128
    chunks = []
    for g in range(NG):
        for e in range(n_exp):
            chunks.append(w_gu[e][:, 0:512])
            chunks.append(w_gu[e][:, D:D + 512])
            chunks.append(w_gu[e][:, 512:D])
            chunks.append(w_gu[e][:, D + 512:2 * D])
            chunks.append(w_dn[e][:, 0:512])
            chunks.append(w_dn[e][:, 512:D])
    issued = [0]

    def ensure(ci):
        while issued[0] <= min(ci, len(chunks) - 1):
            c = issued[0]
            load_w_bf16(P, wslot[c % NS], wslot[c % NS][:], chunks[c], wsem[c % NS])
            issued[0] += 1

    for g in range(NG):
        t0 = g * GT
        for j in range(JT):
            r0 = t0 + j * 128
            P.dma("sp", h1[:, j, :], h_in[r0:r0 + 128, :], hsem[0], writes=[(h1, j)])
            for i in range(n_part):
                stg = big[1 + i % 2]
                P.dma("sp", stg[:], parts[i][r0:r0 + 128, :], hsem[1], writes=[stg])
                P.op("pool", lambda e: e.tensor_tensor(out=h1[:, j, :], in0=h1[:, j, :], in1=stg[:], op=ALU.add),
                     reads=[(h1, j), stg], writes=[(h1, j)])
            def bail(ap, rd, w):
                P.dma("sp", h_out[0:ap.shape[0], 0:w], ap, s_out, reads=rd)
                return None
            if stop == "A1":
                return bail(h1[:, 0, :], [h1], 1024)
            ss = C.stat("ss")
            rstd = C.stat("rstd")
            P.op("act", lambda e: e.activation(out=C.junk[:], in_=h1[:, j, :], func=AF.Square, accum_out=ss[:]),
                 reads=[(h1, j)], writes=[C.junk, ss])
            emit_rstd(P, C, (ss, ss[:]), (rstd, rstd[:]), D)
            P.op("dve", lambda e: e.scalar_tensor_tensor(out=xn[:], in0=h1[:, j, :], scalar=rstd[:, 0:1], in1=g_ffn[:],
                                                         op0=ALU.mult, op1=ALU.mult),
                 reads=[(h1, j), rstd, g_ffn], writes=[xn])
            if stop == "A2":
                return bail(xn[:], [xn], 1024)
            for k in range(8):
                tr = C.trp[k // 4]
                P.op("pe", lambda e: e.transpose(out=tr[:, (k % 4) * 128:(k % 4 + 1) * 128],
                                                 in_=xn[:, k * 128:(k + 1) * 128], identity=C.identf[:]),
                     reads=[xn, C.identf], writes=[(tr, k % 4)])
            import os as _os
            VAR = _os.environ.get("VAR", "")
            for hh in range(2):
                tr = C.trp[hh]
                if VAR != "1":
                    P.op("act", lambda e: e.copy(out=xnT[:, hh * 4:(hh + 1) * 4, j * 128:(j + 1) * 128],
                                                 in_=tr[:].rearrange("p (k t) -> p k t", k=4)),
                         reads=[tr], writes=[(xnT, (j, hh))])
                if VAR != "2":
                    P.op("dve", lambda e: e.tensor_copy(out=xnTf[:, hh * 4:(hh + 1) * 4, :],
                                                        in_=tr[:].rearrange("p (k t) -> p k t", k=4)),
                         reads=[tr], writes=[(xnTf, hh)])
            if stop == "A3":
                return bail(xnTf[:].rearrange("p k t -> p (k t)"), [xnTf], 1024)
            for k in range(8):
                P.op("pe", lambda e: e.matmul(out=C.small[:, 0:NE], lhsT=xnTf[:, k, :], rhs=wr_sb[:, k, :],
                                              start=(k == 0), stop=(k == 7)),
                     reads=[(xnTf, k // 4), wr_sb], writes=[C.small])
            P.op("dve", lambda e: e.tensor_tensor(out=lg[:], in0=C.small[:, 0:NE], in1=br_bc[:], op=ALU.add),
                 reads=[C.small, br_bc], writes=[lg])
            if stop == "A4":
                return bail(lg[:], [lg], NE)
            P.op("dve", lambda e: e.max(out=top8[:], in_=lg[:]), reads=[lg], writes=[top8])
            P.op("dve", lambda e: e.tensor_scalar(out=msk[:], in0=lg[:], scalar1=top8[:, 3:4], scalar2=None,
                                                  op0=ALU.is_ge), reads=[lg, top8], writes=[msk])
            negm = C.stat("negm")
            P.op("dve", lambda e: e.tensor_scalar(out=negm[:], in0=top8[:, 0:1], scalar1=-1.0, scalar2=None,
                                                  op0=ALU.mult), reads=[top8], writes=[negm])
            P.op("act", lambda e: e.activation(out=exs[:], in_=lg[:], func=AF.Exp, bias=negm[:, 0:1], scale=1.0),
                 reads=[lg, negm], writes=[exs])
            P.op("dve", lambda e: e.tensor_tensor(out=exs[:], in0=exs[:], in1=msk[:], op=ALU.mult),
                 reads=[exs, msk], writes=[exs])
            ssum = C.stat("ssum")
            P.op("dve", lambda e: e.tensor_reduce(out=ssum[:], in_=exs[:], axis=AX.X, op=ALU.add),
                 reads=[exs], writes=[ssum])
            P.op("dve", lambda e: e.reciprocal(out=ssum[:], in_=ssum[:]), reads=[ssum], writes=[ssum])
            P.op("dve", lambda e: e.tensor_scalar(out=exs[:], in0=exs[:], scalar1=ssum[:, 0:1], scalar2=None,
                                                  op0=ALU.mult), reads=[exs, ssum], writes=[exs])
            if stop == "A5":
                return bail(exs[:], [exs], NE)
            P.op("pe", lambda e: e.transpose(out=C.small[0:NE, 128:256], in_=exs[:], identity=C.identf[:]),
                 reads=[exs, C.identf], writes=[C.small])
            P.op("dve", lambda e: e.tensor_copy(out=G[:, j * 128:(j + 1) * 128], in_=C.small[0:NE, 128:256]),
                 reads=[C.small], writes=[(G, j)])

        if stop == "A":
            P.dma("sp", h_out[0:NE, 0:GT], G[:], s_out, reads=[G])
            return None
        for ei in range(n_exp):
            ci = (g * n_exp + ei) * 6
            pg = C.bank()
            P.op("dve", lambda e: e.tensor_copy(out=selt[:], in_=C.identf[0:NE, ei:ei + 1].to_broadcast([NE, 128])),
                 reads=[C.identf], writes=[selt])
            P.op("pe", lambda e: e.matmul(out=pg[:], lhsT=selt[:], rhs=G[:], start=True, stop=True),
                 reads=[selt, G], writes=[pg])
            P.op("act", lambda e: e.copy(out=Gbc[:], in_=pg[:]), reads=[pg], writes=[Gbc])
            for m in range(8):
                ms = slice((m % 4) * 128, (m % 4 + 1) * 128)
                if m % 4 == 0:
                    ensure(ci + 2 * (m // 4) + 7)
                wa, wb = wslot[(ci + 2 * (m // 4)) % NS], wslot[(ci + 2 * (m // 4) + 1) % NS]
                pgl = C.bank()
                for k in range(8):
                    P.op("pe", lambda e: e.matmul(out=pgl[:], lhsT=wa[:, k, ms], rhs=xnT[:, k, :],
                                                  start=(k == 0), stop=(k == 7)),
                         reads=[wa, xnT], writes=[pgl])
                pli = C.bank()
                for k in range(8):
                    P.op("pe", lambda e: e.matmul(out=pli[:], lhsT=wb[:, k, ms], rhs=xnT[:, k, :],
                                                  start=(k == 0), stop=(k == 7)),
                         reads=[wb, xnT], writes=[pli])
                g1, sg, l1 = tmp(), tmp(), tmp()
                P.op("dve", lambda e: e.tensor_scalar(out=g1[:], in0=pgl[:], scalar1=bfm[:, m, ei:ei + 1], scalar2=7.0,
                                                      op0=ALU.add, op1=ALU.min), reads=[pgl, bfm], writes=[g1])
                P.op("act", lambda e: e.activation(out=sg[:], in_=g1[:], func=AF.Sigmoid, scale=1.702),
                     reads=[g1], writes=[sg])
                P.op("dve", lambda e: e.tensor_scalar(out=l1[:], in0=pli[:], scalar1=bfm[:, 8 + m, ei:ei + 1], scalar2=7.0,
                                                      op0=ALU.add, op1=ALU.min), reads=[pli, bfm], writes=[l1])
                P.op("dve", lambda e: e.tensor_scalar(out=l1[:], in0=l1[:], scalar1=-7.0, scalar2=1.0,
                                                      op0=ALU.max, op1=ALU.add), reads=[l1], writes=[l1])
                P.op("pool", lambda e: e.tensor_tensor(out=g1[:], in0=g1[:], in1=sg[:], op=ALU.mult),
                     reads=[g1, sg], writes=[g1])
                P.op("pool", lambda e: e.tensor_tensor(out=actT[:, m, :], in0=g1[:], in1=l1[:], op=ALU.mult),
                     reads=[g1, l1], writes=[(actT, m)])
            for m in range(8):
                ms = slice((m % 4) * 128, (m % 4 + 1) * 128)
                if m % 4 == 0:
                    ensure(ci + 4 + (m // 4) + 7)
                wd = wslot[(ci + 4 + m // 4) % NS]
                py = C.bank()
                for k in range(8):
                    P.op("pe", lambda e: e.matmul(out=py[:], lhsT=wd[:, k, ms], rhs=actT[:, k, :],
                                                  start=(k == 0), stop=(k == 7)),
                         reads=[wd, (actT, k)], writes=[py])
                if ei == 0:
                    P.op("dve", lambda e: e.scalar_tensor_tensor(out=yacc[:, m, :], in0=py[:], scalar=bfm[:, 16 + m, ei:ei + 1],
                                                                 in1=Gbc[:], op0=ALU.add, op1=ALU.mult),
                         reads=[py, bfm, Gbc], writes=[(yacc, m)])
                else:
                    t1 = tmp()
                    P.op("dve", lambda e: e.scalar_tensor_tensor(out=t1[:], in0=py[:], scalar=bfm[:, 16 + m, ei:ei + 1],
                                                                 in1=Gbc[:], op0=ALU.add, op1=ALU.mult),
                         reads=[py, bfm, Gbc], writes=[t1])
                    P.op("pool", lambda e: e.tensor_tensor(out=yacc[:, m, :], in0=yacc[:, m, :], in1=t1[:], op=ALU.add),
                         reads=[(yacc, m), t1], writes=[(yacc, m)])

        if stop == "B":
            P.dma("sp", h_out[0:128, 0:GT], yacc[:, 0, :], s_out, reads=[yacc])
            return None
        for j in range(JT):
            r0 = t0 + j * 128
            js = slice(j * 128, (j + 1) * 128)
            pb_ = pbuf[j % 2]
            P.dma("sp", pb_[:], p_in[r0:r0 + 128, :], psem[j % 2], writes=[pb_])
            for m in range(8):
                tr = C.trp[m // 4]
                P.op("pe", lambda e: e.transpose(out=tr[:, (m % 4) * 128:(m % 4 + 1) * 128],
                                                 in_=yacc[:, m, js], identity=C.identf[:]),
                     reads=[(yacc, m), C.identf], writes=[(tr, m % 4)])
            h2 = big[0]
            for hh in range(2):
                P.op("dve", lambda e: e.tensor_tensor(out=h2[:, hh * 512:(hh + 1) * 512], in0=C.trp[hh][:],
                                                      in1=h1[:, j, hh * 512:(hh + 1) * 512], op=ALU.add),
                     reads=[C.trp[hh], (h1, j)], writes=[(h2, hh)])
            for k in range(2):
                P.op("pe", lambda e: e.transpose(out=C.trp[0][:, k * 128:(k + 1) * 128], in_=pb_[:, k * 128:(k + 1) * 128],
                                                 identity=C.identf[:]), reads=[pb_, C.identf], writes=[(C.trp[0], k)])
            P.op("act", lambda e: e.copy(out=pT[:], in_=C.trp[0][:, 0:256].rearrange("p (k t) -> p k t", k=2)),
                 reads=[C.trp[0]], writes=[pT])
            pA = [C.bank(), C.bank()]
            for hh in range(2):
                for k in range(2):
                    P.op("pe", lambda e: e.matmul(out=pA[hh][:], lhsT=pT[:, k, :], rhs=plew[:, k, hh * 512:(hh + 1) * 512],
                                                  start=(k == 0), stop=(k == 1)),
                         reads=[pT, plew], writes=[pA[hh]])
            ssa = C.stat("ssa", 2)
            for hh in range(2):
                P.op("act", lambda e: e.activation(out=C.junk[:, hh * 512:(hh + 1) * 512], in_=pA[hh][:], func=AF.Square,
                                                   accum_out=ssa[:, hh:hh + 1]),
                     reads=[pA[hh]], writes=[(C.junk, hh), (ssa, hh)])
            ssa1 = C.stat("ssa1")
            P.op("dve", lambda e: e.tensor_tensor(out=ssa1[:], in0=ssa[:, 0:1], in1=ssa[:, 1:2], op=ALU.add),
                 reads=[ssa], writes=[ssa1])
            rstda = C.stat("rstda")
            emit_rstd(P, C, (ssa1, ssa1[:]), (rstda, rstda[:]), D)
            An = big[1]
            for hh in range(2):
                hs = slice(hh * 512, (hh + 1) * 512)
                P.op("dve", lambda e: e.scalar_tensor_tensor(out=An[:, hs], in0=pA[hh][:], scalar=rstda[:, 0:1],
                                                             in1=g_ple[:, hs], op0=ALU.mult, op1=ALU.mult),
                     reads=[pA[hh], rstda, g_ple], writes=[(An, hh)])
            ss2 = C.stat("ss2")
            rstd2 = C.stat("rstd2")
            P.op("act", lambda e: e.activation(out=C.junk[:], in_=h2[:], func=AF.Square, accum_out=ss2[:]),
                 reads=[h2], writes=[C.junk, ss2])
            emit_rstd(P, C, (ss2, ss2[:]), (rstd2, rstd2[:]), D)
            P.op("dve", lambda e: e.scalar_tensor_tensor(out=xnb[:], in0=h2[:], scalar=rstd2[:, 0:1], in1=g_gate[:],
                                                         op0=ALU.mult, op1=ALU.mult),
                 reads=[h2, rstd2, g_gate], writes=[xnb])
            for k in range(8):
                P.op("pe", lambda e: e.transpose(out=C.trb[:, k * 128:(k + 1) * 128], in_=xnb[:, k * 128:(k + 1) * 128],
                                                 identity=C.identb[:]), reads=[xnb, C.identb], writes=[(C.trb, k)])
            P.op("act", lambda e: e.copy(out=hnT[:], in_=C.trb[:].rearrange("p (k t) -> p k t", k=8)),
                 reads=[C.trb], writes=[hnT])
            pB = [C.bank(), C.bank()]
            for hh in range(2):
                for k in range(8):
                    P.op("pe", lambda e: e.matmul(out=pB[hh][:], lhsT=hnT[:, k, :], rhs=plegw[:, k, hh * 512:(hh + 1) * 512],
                                                  start=(k == 0), stop=(k == 7)),
                         reads=[hnT, plegw], writes=[pB[hh]])
            sB = big[2]
            ot = otile[0]
            for hh in range(2):
                hs = slice(hh * 512, (hh + 1) * 512)
                P.op("act", lambda e: e.activation(out=sB[:, hs], in_=pB[hh][:], func=AF.Sigmoid),
                     reads=[pB[hh]], writes=[(sB, hh)])
                P.op("pool", lambda e: e.tensor_tensor(out=sB[:, hs], in0=sB[:, hs], in1=An[:, hs], op=ALU.mult),
                     reads=[(sB, hh), (An, hh)], writes=[(sB, hh)])
                P.op("pool", lambda e: e.tensor_tensor(out=ot[:, hs], in0=sB[:, hs], in1=h2[:, hs], op=ALU.add),
                     reads=[(sB, hh), (h2, hh)], writes=[(ot, hh)])
            P.dma("sp", h_out[r0:r0 + 128, :], ot[:], s_out, reads=[ot])
    return None


def build_ffn(NT, n_part=2, n_exp=NE, stop=None):
    P = Prog()
    C = Ctx(P)
    din = lambda n, s: P.dram(n, s, F32, "ExternalInput")
    h_in = din("h_in", [NT, D])
    parts = [din(f"part{i}", [NT, D]) for i in range(n_part)]
    p_in = din("p_in", [NT, 256])
    a = {}
    for n, shp in (("norm_ffn", [D]), ("router_w", [D, NE]), ("router_b", [NE]), ("w_gu", [n_exp, D, 2 * D]), ("b_gu", [NE, 2 * D]),
                   ("w_dn", [n_exp, D, D]), ("b_dn", [NE, D]), ("ple_w", [256, D]), ("ple_norm", [D]), ("ple_gate_norm", [D]),
                   ("ple_gate_w", [D, D])):
        a[n] = din(n, shp).ap()
    h_out = P.dram("h_out", [NT, D], F32, "ExternalOutput")
    emit_ffn(P, C, NT, h_in.ap(), [x.ap() for x in parts], p_in.ap(), a["norm_ffn"], a["router_w"], a["router_b"], a["w_gu"], a["b_gu"],
             a["w_dn"], a["b_dn"], a["ple_w"], a["ple_norm"], a["ple_gate_norm"], a["ple_gate_w"], h_out.ap(), n_exp=n_exp, stop=stop)
    print("ffn program: n_inst", P.n_inst)
    return P.finish(P.dsems)


class Phase:
    def __init__(self, P):
        self.P = P

    def __enter__(self):
        self.saved = self.P.es
        self.P.es = ExitStack()
        return self

    def __exit__(self, *a):
        P = self.P
        P.barrier()
        P.es.close()
        P.es = self.saved
        return False


def _barrier(self):
    sems = [E.sem for E in self.engs.values()] + list(self.dsems)
    for E in self.engs.values():
        for s in sems:
            if s is E.sem:
                continue
            if s.issued > 0 and E.known.get(s, 0) < s.issued:
                E.e.wait_ge(s.h, s.issued)
                E.known[s] = s.issued
                if s.is_dma and s.issued > s.last_wait:
                    s.last_wait = s.issued


Prog.barrier = _barrier
_old_dsem = Prog.dsem


def _dsem(self, name):
    if not hasattr(self, "dsem_by_name"):
        self.dsem_by_name = {}
    if name in self.dsem_by_name:
        return self.dsem_by_name[name]
    s = self.dsem_by_name[name] = Sem(self.gstack.enter_context(self.nc.semaphore(name)), name, is_dma=True)
    self.dsems.append(s)
    return s


Prog.dsem = _dsem


class NormT:
    def __init__(self, P, C, tag):
        self.P, self.C = P, C
        self.hb = [P.sb(f"nt_hb{i}_{tag}", [128, D], F32) for i in range(2)]
        self.sem = [P.dsem(f"d_nt{i}_{tag}") for i in range(2)]
        self.xnb = P.sb(f"nt_xnb_{tag}", [128, D], BF16)
        self.i = 0

    def emit(self, src_ap, src_deps, gain, dst, dst_ap, dst_key):
        P, C = self.P, self.C
        hb, sem = self.hb[self.i % 2], self.sem[self.i % 2]
        self.i += 1
        P.dma("sp", hb[:], src_ap, sem, reads=src_deps, writes=[hb])
        ss, rstd = C.stat("ss"), C.stat("rstd")
        P.op("act", lambda e: e.activation(out=C.junk[:], in_=hb[:], func=AF.Square, accum_out=ss[:]),
             reads=[hb], writes=[C.junk, ss])
        emit_rstd(P, C, (ss, ss[:]), (rstd, rstd[:]), D)
        P.op("dve", lambda e: e.scalar_tensor_tensor(out=self.xnb[:], in0=hb[:], scalar=rstd[:, 0:1], in1=gain[:],
                                                     op0=ALU.mult, op1=ALU.mult),
             reads=[hb, rstd, gain], writes=[self.xnb])
        for k in range(8):
            P.op("pe", lambda e: e.transpose(out=C.trb[:, k * 128:(k + 1) * 128], in_=self.xnb[:, k * 128:(k + 1) * 128],
                                             identity=C.identb[:]), reads=[self.xnb, C.identb], writes=[C.trb])
        P.op("act", lambda e: e.copy(out=dst_ap, in_=C.trb[:].rearrange("p (k t) -> p k t", k=8)),
             reads=[C.trb], writes=[(dst, dst_key)])


def emit_outproj(P, C, yT, Wout, ntile, dst_rows_fn, dst_tile, osem, obufs, cnt):
    for j in range(ntile):
        ot = obufs[cnt[0] % len(obufs)]
        cnt[0] += 1
        for hh in range(2):
            po = C.bank()
            for k in range(8):
                P.op("pe", lambda e: e.matmul(out=po[:], lhsT=yT[:, k, j * 128:(j + 1) * 128],
                                              rhs=Wout[:, k, hh * 512:(hh + 1) * 512], start=(k == 0), stop=(k == 7)),
                     reads=[yT, Wout], writes=[po])
            if hh == 0:
                P.op("act", lambda e: e.copy(out=ot[:, 0:512], in_=po[:]), reads=[po], writes=[(ot, 0)])
            else:
                P.op("dve", lambda e: e.tensor_copy(out=ot[:, 512:1024], in_=po[:]), reads=[po], writes=[(ot, 1)])
        P.dma("sp", dst_rows_fn(j), ot[:], osem, reads=[ot], writes=[dst_tile] if dst_tile is not None else None)


def emit_hgrn2(P, C, S, layer, h_src, h_deps, mix_dst, mix_tile, g_mix_ap, w_in, w_out, gnorm_ap, lb_param):
    ST = 512
    with Phase(P):
        s_w = P.dsem("d_hgw")
        s_c = P.dsem("d_hgc")
        s_o = P.dsem("d_hgo")
        Win = P.sb("hg_win", [128, 8, 4 * D], BF16)
        for q in range(4):
            load_w_bf16(P, Win, Win[:, :, q * D:(q + 1) * D], w_in[:, q * D:(q + 1) * D], s_w, key=q)
        Wout = P.sb("hg_wout", [128, 8, D], BF16)
        load_w_bf16(P, Wout, Wout[:], w_out, s_w)
        g_mix = bc_load(P, "sp", "hg_gmix", g_mix_ap, D, s_c)
        gn = P.sb("hg_gn", [128, 1], F32)
        P.dma("sp", gn[:], gnorm_ap.rearrange("(p o) -> p o", o=1), s_c, writes=[gn])
        lbrow = P.sb("hg_lbrow", [32, 128], F32)
        P.dma("sp", lbrow[:], lb_param.rearrange("l (h p) -> (l h) p", p=128), s_c, writes=[lbrow])
        P.op("pe", lambda e: e.transpose(out=C.small[:, 0:32], in_=lbrow[:], identity=C.identf[0:32, 0:32]),
             reads=[lbrow, C.identf], writes=[C.small])
        el = P.sb("hg_el", [128, 4, 8], F32)
        P.op("act", lambda e: e.activation(out=el[:], in_=C.small[:, 0:32].rearrange("p (l h) -> p l h", l=4), func=AF.Exp),
             reads=[C.small], writes=[el])
        den = P.sb("hg_den", [128, 8], F32)
        num = P.sb("hg_num", [128, 8], F32)
        P.op("dve", lambda e: e.tensor_tensor(out=den[:], in0=el[:, 0, :], in1=el[:, 1, :], op=ALU.add), reads=[el], writes=[den])
        P.op("dve", lambda e: e.tensor_tensor(out=den[:], in0=den[:], in1=el[:, 2, :], op=ALU.add), reads=[el, den], writes=[den])
        P.op("dve", lambda e: e.tensor_tensor(out=den[:], in0=den[:], in1=el[:, 3, :], op=ALU.add), reads=[el, den], writes=[den])
        P.op("dve", lambda e: e.memset(num[:], 0.0), writes=[num])
        for l in range(1, layer + 1):
            P.op("dve", lambda e: e.tensor_tensor(out=num[:], in0=num[:], in1=el[:, l, :], op=ALU.add), reads=[el, num], writes=[num])
        P.op("dve", lambda e: e.reciprocal(out=den[:], in_=den[:]), reads=[den], writes=[den])
        lb = P.sb("hg_lb", [128, 8], F32)
        oml = P.sb("hg_oml", [128, 8], F32)
        noml = P.sb("hg_noml", [128, 8], F32)
        P.op("dve", lambda e: e.tensor_tensor(out=lb[:], in0=num[:], in1=den[:], op=ALU.mult), reads=[num, den], writes=[lb])
        P.op("dve", lambda e: e.tensor_scalar(out=oml[:], in0=lb[:], scalar1=-1.0, scalar2=1.0, op0=ALU.mult, op1=ALU.add),
             reads=[lb], writes=[oml])
        P.op("dve", lambda e: e.tensor_scalar(out=noml[:], in0=oml[:], scalar1=-1.0, scalar2=None, op0=ALU.mult),
             reads=[oml], writes=[noml])
        mask01 = P.sb("hg_mask01", [128, ST], F32)
        P.op("pool", lambda e: e.memset(mask01[:], 1.0), writes=[mask01])
        for c in range(ST // 64):
            P.op("pool", lambda e: e.memset(mask01[:, c * 64:c * 64 + 1], 0.0), reads=[mask01], writes=[mask01])
        mc = P.sb("hg_mc", [64, ST], F32)
        P.op("pool", lambda e: e.memset(mc[:], 1.0), writes=[mc])
        P.op("pool", lambda e: e.affine_select(out=mc[:], in_=mc[:], pattern=[[0, ST // 64], [1, 64]], compare_op=ALU.is_ge,
                                               fill=0.0, base=0, channel_multiplier=-1), reads=[mc], writes=[mc])
        onesf = P.sb("hg_ones", [128, 128], F32)
        P.op("pool", lambda e: e.memset(onesf[:], 1.0), writes=[onesf])
        Sf = P.sb("hg_Sf", [128, 8, 128], F32)
        Sb = P.sb("hg_Sb", [128, 8, 128], BF16)
        P.op("pool", lambda e: e.memset(Sf[:], 0.0), writes=[Sf])
        P.op("pool", lambda e: e.memset(Sb[:], 0.0), writes=[Sb])
        Tt = P.sb("hg_T", [128, 128], F32)
        xnT = P.sb("hg_xnT", [128, 8, ST], BF16)
        v_sb = P.sb("hg_v", [64, ST // 64, D], BF16)
        f32t = {n: P.sb("hg_" + n, [128, ST], F32) for n in ["sg", "lf", "cum", "A", "Ai", "qs", "kk", "sgate", "sq", "rr"]}
        qt = P.sb("hg_qt", [128, ST], BF16)
        kt = P.sb("hg_kt", [128, ST], BF16)
        ktm = P.sb("hg_ktm", [64, ST // 64, 128], BF16)
        scm = P.sb("hg_scm", [64, ST], BF16)
        ogT = P.sb("hg_ogT", [128, 8, ST], BF16)
        obufs = [P.sb(f"hg_ob{i}", [128, D], F32) for i in range(2)]
        ocnt = [0]
        nt = NormT(P, C, "hg")
        oT, dS, ssps = C.trp[0], C.trp[1], C.small
        NCH = ST // 64
        for st in range(S // ST):
            t0 = st * ST
            for j in range(ST // 128):
                nt.emit(h_src[t0 + j * 128:t0 + (j + 1) * 128, :], h_deps, g_mix, xnT, xnT[:, :, j * 128:(j + 1) * 128], j)
            for c in range(NCH):
                for hh in range(2):
                    pv = C.bank()
                    for k in range(8):
                        P.op("pe", lambda e: e.matmul(out=pv[0:64, :], lhsT=xnT[:, k, c * 64:(c + 1) * 64],
                                                      rhs=Win[:, k, 2 * D + hh * 512:2 * D + (hh + 1) * 512],
                                                      start=(k == 0), stop=(k == 7)), reads=[xnT, (Win, 2)], writes=[pv])
                    if hh == 0:
                        P.op("act", lambda e: e.copy(out=v_sb[:, c, 0:512], in_=pv[0:64, :]), reads=[pv], writes=[(v_sb, (c, 0))])
                    else:
                        P.op("dve", lambda e: e.tensor_copy(out=v_sb[:, c, 512:1024], in_=pv[0:64, :]), reads=[pv],
                             writes=[(v_sb, (c, 1))])
            for H in range(8):
                Hs = slice(H * 128, (H + 1) * 128)
                pq, pz, pg = C.bank(), C.bank(), C.bank()
                for (pp, off, wk) in ((pq, 0, 0), (pz, D, 1), (pg, 3 * D, 3)):
                    for k in range(8):
                        P.op("pe", lambda e: e.matmul(out=pp[:], lhsT=Win[:, k, off + H * 128:off + (H + 1) * 128], rhs=xnT[:, k, :],
                                                      start=(k == 0), stop=(k == 7)), reads=[xnT, (Win, wk)], writes=[pp])
                T = f32t
                P.op("act", lambda e: e.activation(out=T["sg"][:], in_=pz[:], func=AF.Sigmoid), reads=[pz], writes=[T["sg"]])
                P.op("act", lambda e: e.activation(out=T["qs"][:], in_=pq[:], func=AF.Silu), reads=[pq], writes=[T["qs"]])
                P.op("act", lambda e: e.activation(out=T["sgate"][:], in_=pg[:], func=AF.Silu), reads=[pg], writes=[T["sgate"]])
                P.op("dve", lambda e: e.tensor_scalar(out=T["lf"][:], in0=T["sg"][:], scalar1=oml[:, H:H + 1], scalar2=lb[:, H:H + 1],
                                                      op0=ALU.mult, op1=ALU.add), reads=[T["sg"], oml, lb], writes=[T["lf"]])
                P.op("act", lambda e: e.activation(out=T["lf"][:], in_=T["lf"][:], func=AF.Ln), reads=[T["lf"]], writes=[T["lf"]])
                P.op("dve", lambda e: e.tensor_tensor_scan(out=T["cum"][:], data0=mask01[:], data1=T["lf"][:], initial=0.0,
                                                           op0=ALU.mult, op1=ALU.add), reads=[mask01, T["lf"]], writes=[T["cum"]])
                P.op("act", lambda e: e.activation(out=T["A"][:], in_=T["cum"][:], func=AF.Exp), reads=[T["cum"]], writes=[T["A"]])
                P.op("act", lambda e: e.activation(out=T["Ai"][:], in_=T["cum"][:], func=AF.Exp, scale=-1.0),
                     reads=[T["cum"]], writes=[T["Ai"]])
                P.op("dve", lambda e: e.scalar_tensor_tensor(out=qt[:], in0=T["qs"][:], scalar=128.0 ** -0.5, in1=T["A"][:],
                                                             op0=ALU.mult, op1=ALU.mult), reads=[T["qs"], T["A"]], writes=[qt])
                P.op("dve", lambda e: e.tensor_scalar(out=T["kk"][:], in0=T["sg"][:], scalar1=noml[:, H:H + 1], scalar2=oml[:, H:H + 1],
                                                      op0=ALU.mult, op1=ALU.add), reads=[T["sg"], noml, oml], writes=[T["kk"]])
                P.op("pool", lambda e: e.tensor_tensor(out=kt[:], in0=T["kk"][:], in1=T["Ai"][:], op=ALU.mult),
                     reads=[T["kk"], T["Ai"]], writes=[kt])
                for c in range(NCH):
                    P.op("pe", lambda e: e.transpose(out=C.trb[0:64, c * 128:(c + 1) * 128], in_=kt[:, c * 64:(c + 1) * 64],
                                                     identity=C.identb[:]), reads=[kt, C.identb], writes=[C.trb])
                P.op("act", lambda e: e.copy(out=ktm[:], in_=C.trb[0:64, :].rearrange("p (c d) -> p c d", c=NCH)),
                     reads=[C.trb], writes=[ktm])
                sc = C.bank()
                for c in range(NCH):
                    cs = slice(c * 64, (c + 1) * 64)
                    P.op("pe", lambda e: e.matmul(out=sc[0:64, cs], lhsT=kt[:, cs], rhs=qt[:, cs], start=True, stop=True),
                         reads=[kt, qt], writes=[sc])
                P.op("dve", lambda e: e.tensor_tensor(out=scm[:], in0=sc[0:64, :], in1=mc[:], op=ALU.mult),
                     reads=[sc, mc], writes=[scm])
                for c in range(NCH):
                    cs = slice(c * 64, (c + 1) * 64)
                    P.op("pe", lambda e: e.matmul(out=oT[:, cs], lhsT=v_sb[:, c, Hs], rhs=scm[:, cs], start=True, stop=False),
                         reads=[v_sb, scm], writes=[oT])
                    P.op("pe", lambda e: e.matmul(out=oT[:, cs], lhsT=Sb[:, H, :], rhs=qt[:, cs], start=False, stop=True),
                         reads=[(Sb, H), qt], writes=[oT])
                    P.op("pe", lambda e: e.matmul(out=dS[:, 0:128], lhsT=ktm[:, c, :], rhs=v_sb[:, c, Hs], start=True, stop=True),
                         reads=[ktm, v_sb], writes=[dS])
                    acol = T["A"][:, c * 64 + 63:c * 64 + 64]
                    P.op("dve", lambda e: e.tensor_tensor(out=Tt[:], in0=dS[:, 0:128], in1=Sf[:, H, :], op=ALU.add),
                         reads=[dS, (Sf, H)], writes=[Tt])
                    P.op("dve", lambda e: e.tensor_scalar(out=Sf[:, H, :], in0=Tt[:], scalar1=acol, scalar2=None, op0=ALU.mult),
                         reads=[Tt, T["A"]], writes=[(Sf, H)])
                    P.op("act", lambda e: e.activation(out=Sb[:, H, :], in_=Tt[:], func=AF.Copy, scale=acol),
                         reads=[Tt, T["A"]], writes=[(Sb, H)])
                P.op("act", lambda e: e.activation(out=T["sq"][:], in_=oT[:], func=AF.Square), reads=[oT], writes=[T["sq"]])
                P.op("pe", lambda e: e.matmul(out=ssps[:], lhsT=onesf[:], rhs=T["sq"][:], start=True, stop=True),
                     reads=[onesf, T["sq"]], writes=[ssps])
                emit_rstd(P, C, (ssps, ssps[:]), (T["rr"], T["rr"][:]), 128)
                P.op("dve", lambda e: e.tensor_tensor(out=T["sq"][:], in0=oT[:], in1=T["rr"][:], op=ALU.mult),
                     reads=[oT, T["rr"]], writes=[T["sq"]])
                P.op("dve", lambda e: e.scalar_tensor_tensor(out=ogT[:, H, :], in0=T["sq"][:], scalar=gn[:, 0:1], in1=T["sgate"][:],
                                                             op0=ALU.mult, op1=ALU.mult),
                     reads=[T["sq"], gn, T["sgate"]], writes=[(ogT, H)])
            emit_outproj(P, C, ogT, Wout, ST // 128, lambda j: mix_dst[t0 + j * 128:t0 + (j + 1) * 128, :], mix_tile, s_o,
                         obufs, ocnt)


def emit_rglru(P, C, S, h_src, h_deps, mix_dst, mix_tile, g_mix_ap, w_in, conv_w, conv_b, w_a, b_a, w_x, b_x, lam, w_out):
    ST = 512
    with Phase(P):
        s_w = P.dsem("d_rgw")
        s_c = P.dsem("d_rgc")
        s_o = P.dsem("d_rgo")
        Win = P.sb("rg_win", [128, 8, 2 * D], BF16)
        for q in range(2):
            load_w_bf16(P, Win, Win[:, :, q * D:(q + 1) * D], w_in[:, q * D:(q + 1) * D], s_w, key=q)
        Wout = P.sb("rg_wout", [128, 8, D], BF16)
        load_w_bf16(P, Wout, Wout[:], w_out, s_w)
        wa = P.sb("rg_wa_sb", [128, 8, 256], BF16)
        wx = P.sb("rg_wx_sb", [128, 8, 256], BF16)
        P.dma("pool", wa[:], w_a.rearrange("n (dh p) e -> p (n dh) e", p=128), s_w, writes=[wa])
        P.dma("pool", wx[:], w_x.rearrange("n (dh p) e -> p (n dh) e", p=128), s_w, writes=[wx])
        g_mix = bc_load(P, "sp", "rg_gmix", g_mix_ap, D, s_c)
        vrow = P.sb("rg_vrow", [64, 128], F32)
        P.dma("sp", vrow[0:32, :], conv_w.rearrange("t (k p) -> (t k) p", p=128), s_c, writes=[vrow])
        for i, v in enumerate((conv_b, b_a, b_x, lam)):
            P.dma("sp", vrow[32 + 8 * i:40 + 8 * i, :], v.rearrange("(k p) -> k p", p=128), s_c, writes=[vrow])
        P.op("pe", lambda e: e.transpose(out=C.small[:, 0:64], in_=vrow[:], identity=C.identf[0:64, 0:64]),
             reads=[vrow, C.identf], writes=[C.small])
        vT = P.sb("rg_vT", [128, 64], F32)
        P.op("dve", lambda e: e.tensor_copy(out=vT[:], in_=C.small[:, 0:64]), reads=[C.small], writes=[vT])
        cw = lambda t, k: vT[:, t * 8 + k:t * 8 + k + 1]
        cb = lambda k: vT[:, 32 + k:33 + k]
        ba = lambda k: vT[:, 40 + k:41 + k]
        bx = lambda k: vT[:, 48 + k:49 + k]
        cl = P.sb("rg_cl", [128, 8], F32)
        P.op("act", lambda e: e.activation(out=cl[:], in_=vT[:, 56:64], func=AF.Exp, scale=-1.0), reads=[vT], writes=[cl])
        P.op("act", lambda e: e.activation(out=cl[:], in_=cl[:], func=AF.Ln, bias=1.0), reads=[cl], writes=[cl])
        P.op("dve", lambda e: e.tensor_scalar(out=cl[:], in0=cl[:], scalar1=-8.0, scalar2=None, op0=ALU.mult),
             reads=[cl], writes=[cl])
        xnT = P.sb("rg_xnT", [128, 8, ST], BF16)
        gate = P.sb("rg_gate", [128, 8, ST], F32)
        ubuf = [P.sb(f"rg_ubuf{i}", [128, 8, ST + 3], F32) for i in range(2)]
        P.op("pool", lambda e: e.memset(ubuf[1][:], 0.0), writes=[ubuf[1]])
        ucf = P.sb("rg_ucf", [128, 8, ST], F32)
        ucb = P.sb("rg_ucb", [128, 8, ST], BF16)
        tt = [P.sb(f"rg_t{i}", [128, ST], F32) for i in range(6)]
        ti = [0]

        def tmp():
            t = tt[ti[0] % len(tt)]
            ti[0] += 1
            return t
        hlast = P.sb("rg_hlast", [128, 8], F32)
        P.op("pool", lambda e: e.memset(hlast[:], 0.0), writes=[hlast])
        yT = P.sb("rg_yT", [128, 8, ST], BF16)
        obufs = [P.sb(f"rg_ob{i}", [128, D], F32) for i in range(2)]
        ocnt = [0]
        nt = NormT(P, C, "rg")
        for st in range(S // ST):
            t0 = st * ST
            ub, ubp = ubuf[st % 2], ubuf[(st + 1) % 2]
            for j in range(ST // 128):
                nt.emit(h_src[t0 + j * 128:t0 + (j + 1) * 128, :], h_deps, g_mix, xnT, xnT[:, :, j * 128:(j + 1) * 128], j)
            for kc in range(8):
                ks = slice(kc * 128, (kc + 1) * 128)
                pg, pu = C.bank(), C.bank()
                for k in range(8):
                    P.op("pe", lambda e: e.matmul(out=pg[:], lhsT=Win[:, k, ks], rhs=xnT[:, k, :], start=(k == 0), stop=(k == 7)),
                         reads=[xnT, (Win, 0)], writes=[pg])
                for k in range(8):
                    P.op("pe", lambda e: e.matmul(out=pu[:], lhsT=Win[:, k, D + kc * 128:D + (kc + 1) * 128], rhs=xnT[:, k, :],
                                                  start=(k == 0), stop=(k == 7)), reads=[xnT, (Win, 1)], writes=[pu])
                t1, t2 = tmp(), tmp()
                P.op("act", lambda e: e.activation(out=t1[:], in_=pg[:], func=AF.Square), reads=[pg], writes=[t1])
                P.op("dve", lambda e: e.tensor_scalar(out=t1[:], in0=t1[:], scalar1=0.044715, scalar2=1.0, op0=ALU.mult, op1=ALU.add),
                     reads=[t1], writes=[t1])
                P.op("dve", lambda e: e.tensor_tensor(out=t1[:], in0=t1[:], in1=pg[:], op=ALU.mult), reads=[t1, pg], writes=[t1])
                P.op("act", lambda e: e.activation(out=t2[:], in_=t1[:], func=AF.Sigmoid, scale=1.5957691216),
                     reads=[t1], writes=[t2])
                P.op("dve", lambda e: e.tensor_tensor(out=gate[:, kc, :], in0=t2[:], in1=pg[:], op=ALU.mult),
                     reads=[t2, pg], writes=[(gate, kc)])
                P.op("pool", lambda e: e.tensor_copy(out=ub[:, kc, 0:3], in_=ubp[:, kc, ST:ST + 3]),
                     reads=[(ubp, kc)], writes=[(ub, kc)])
                P.op("act", lambda e: e.copy(out=ub[:, kc, 3:ST + 3], in_=pu[:]), reads=[pu], writes=[(ub, kc)])
                P.op("dve", lambda e: e.tensor_scalar(out=ucf[:, kc, :], in0=ub[:, kc, 0:ST], scalar1=cw(0, kc), scalar2=cb(kc),
                                                      op0=ALU.mult, op1=ALU.add), reads=[(ub, kc), vT], writes=[(ucf, kc)])
                for tap in range(1, 4):
                    P.op("dve", lambda e: e.scalar_tensor_tensor(out=ucf[:, kc, :], in0=ub[:, kc, tap:tap + ST], scalar=cw(tap, kc),
                                                                 in1=ucf[:, kc, :], op0=ALU.mult, op1=ALU.add),
                         reads=[(ub, kc), vT, (ucf, kc)], writes=[(ucf, kc)])
                P.op("pool", lambda e: e.tensor_copy(out=ucb[:, kc, :], in_=ucf[:, kc, :]), reads=[(ucf, kc)], writes=[(ucb, kc)])
            for oc in range(8):
                n, eh = oc // 2, oc % 2
                es_ = slice(eh * 128, (eh + 1) * 128)
                pr, pi = C.bank(), C.bank()
                for dh in range(2):
                    P.op("pe", lambda e: e.matmul(out=pr[:], lhsT=wa[:, 2 * n + dh, es_], rhs=ucb[:, 2 * n + dh, :],
                                                  start=(dh == 0), stop=(dh == 1)), reads=[wa, (ucb, 2 * n + dh)], writes=[pr])
                for dh in range(2):
                    P.op("pe", lambda e: e.matmul(out=pi[:], lhsT=wx[:, 2 * n + dh, es_], rhs=ucb[:, 2 * n + dh, :],
                                                  start=(dh == 0), stop=(dh == 1)), reads=[wx, (ucb, 2 * n + dh)], writes=[pi])
                r, ig, a, om = tmp(), tmp(), tmp(), tmp()
                P.op("act", lambda e: e.activation(out=r[:], in_=pr[:], func=AF.Sigmoid, bias=ba(oc)), reads=[pr, vT], writes=[r])
                P.op("act", lambda e: e.activation(out=ig[:], in_=pi[:], func=AF.Sigmoid, bias=bx(oc)), reads=[pi, vT], writes=[ig])
                P.op("act", lambda e: e.activation(out=a[:], in_=r[:], func=AF.Exp, scale=cl[:, oc:oc + 1]), reads=[r, cl], writes=[a])
                P.op("pool", lambda e: e.tensor_tensor(out=om[:], in0=a[:], in1=a[:], op=ALU.mult), reads=[a], writes=[om])
                P.op("dve", lambda e: e.tensor_scalar(out=om[:], in0=om[:], scalar1=-1.0, scalar2=1.0, op0=ALU.mult, op1=ALU.add),
                     reads=[om], writes=[om])
                P.op("act", lambda e: e.activation(out=om[:], in_=om[:], func=AF.Sqrt), reads=[om], writes=[om])
                P.op("pool", lambda e: e.tensor_tensor(out=ig[:], in0=ig[:], in1=ucf[:, oc, :], op=ALU.mult),
                     reads=[ig, (ucf, oc)], writes=[ig])
                P.op("dve", lambda e: e.tensor_tensor(out=ig[:], in0=ig[:], in1=om[:], op=ALU.mult), reads=[ig, om], writes=[ig])
                P.op("dve", lambda e: e.tensor_tensor_scan(out=r[:], data0=a[:], data1=ig[:], initial=hlast[:, oc:oc + 1],
                                                           op0=ALU.mult, op1=ALU.add), reads=[a, ig, hlast], writes=[r])
                P.op("pool", lambda e: e.tensor_copy(out=hlast[:, oc:oc + 1], in_=r[:, ST - 1:ST]), reads=[r], writes=[hlast])
                P.op("dve", lambda e: e.tensor_tensor(out=yT[:, oc, :], in0=r[:], in1=gate[:, oc, :], op=ALU.mult),
                     reads=[r, (gate, oc)], writes=[(yT, oc)])
            emit_outproj(P, C, yT, Wout, ST // 128, lambda j: mix_dst[t0 + j * 128:t0 + (j + 1) * 128, :], mix_tile, s_o,
                         obufs, ocnt)


def emit_fox(P, C, S, h_src, h_deps, mix_dst, mix_tile, g_mix_ap, w_in, f_bias, q_norm, k_norm, w_out):
    ST = 512
    NH, HD = 16, 64
    NT_ = S // 128
    NQ = S // ST
    nc = P.nc
    QTd = nc.dram_tensor("fox_QTd", [NH * HD, S], BF16, kind="Internal").ap()
    KTd = nc.dram_tensor("fox_KTd", [NH * HD, S], BF16, kind="Internal").ap()
    Vd = nc.dram_tensor("fox_Vd", [NH, 128, NT_ * 65], BF16, kind="Internal").ap()
    SGd = nc.dram_tensor("fox_SGd", [S, D], F32, kind="Internal").ap()
    Od = nc.dram_tensor("fox_Od", [S, D], F32, kind="Internal").ap()
    CUMd = nc.dram_tensor("fox_CUMd", [NH, S], F32, kind="Internal").ap()
    with Phase(P):
        s_w = P.dsem("d_fxw")
        s_c = P.dsem("d_fxc")
        s_o = [P.dsem(f"d_fxo{i}") for i in range(2)]
        Win = P.sb("fx_win", [128, 8, 4 * D + NH], BF16)
        for q in range(4):
            load_w_bf16(P, Win, Win[:, :, q * D:(q + 1) * D], w_in[:, q * D:(q + 1) * D], s_w, key=q)
        load_w_bf16(P, Win, Win[:, :, 4 * D:4 * D + NH], w_in[:, 4 * D:4 * D + NH], s_w, key=4)
        g_mix = bc_load(P, "sp", "fx_gmix", g_mix_ap, D, s_c)
        qn_bc = bc_load(P, "sp", "fx_qn", q_norm, HD, s_c)
        kn_bc = bc_load(P, "sp", "fx_kn", k_norm, HD, s_c)
        fb = P.sb("fx_fb", [NH, 1], F32)
        P.dma("sp", fb[:], f_bias.rearrange("(p o) -> p o", o=1), s_c, writes=[fb])
        ones16 = P.sb("fx_ones16", [NH, ST], F32)
        P.op("pool", lambda e: e.memset(ones16[:], 1.0), writes=[ones16])
        xnT = P.sb("fx_xnT", [128, 8, ST], BF16)
        cumst = [P.sb(f"fx_cumst{i}", [NH, ST], F32) for i in range(2)]
        P.op("pool", lambda e: e.memset(cumst[1][:], 0.0), writes=[cumst[1]])
        ls = P.sb("fx_ls", [NH, ST], F32)
        sq = P.sb("fx_sq", [128, ST], F32)
        qb = P.sb("fx_qb", [128, ST], BF16)
        ssq = P.sb("fx_ssq", [128, 8], F32)
        rs = P.sb("fx_rs", [128, 8], F32)
        Q2 = [P.sb(f"fx_Q2_{i}", [128, 8, 128], BF16) for i in range(2)]
        K2 = [P.sb(f"fx_K2_{i}", [128, 8, 128], BF16) for i in range(2)]
        vb = [P.sb(f"fx_vb{i}", [128, NH, 65], BF16) for i in range(2)]
        for i in range(2):
            P.op("pool", lambda e: e.memset(vb[i][:], 1.0), writes=[vb[i]])
        sgt = [P.sb(f"fx_sg{i}", [128, D], F32) for i in range(2)]
        nt = NormT(P, C, "fx")
        tix = 0
        for st in range(S // ST):
            t0 = st * ST
            for j in range(ST // 128):
                nt.emit(h_src[t0 + j * 128:t0 + (j + 1) * 128, :], h_deps, g_mix, xnT, xnT[:, :, j * 128:(j + 1) * 128], j)
            pf = C.bank()
            for k in range(8):
                P.op("pe", lambda e: e.matmul(out=pf[0:NH, :], lhsT=Win[:, k, 4 * D:4 * D + NH], rhs=xnT[:, k, :],
                                              start=(k == 0), stop=(k == 7)), reads=[xnT, (Win, 4)], writes=[pf])
            P.op("act", lambda e: e.activation(out=ls[:], in_=pf[0:NH, :], func=AF.Sigmoid, bias=fb[:, 0:1]),
                 reads=[pf, fb], writes=[ls])
            P.op("act", lambda e: e.activation(out=ls[:], in_=ls[:], func=AF.Ln), reads=[ls], writes=[ls])
            cs_, csp = cumst[st % 2], cumst[(st + 1) % 2]
            P.op("dve", lambda e: e.tensor_tensor_scan(out=cs_[:], data0=ones16[:], data1=ls[:], initial=csp[:, ST - 1:ST],
                                                       op0=ALU.mult, op1=ALU.add), reads=[ones16, ls, csp], writes=[cs_])
            P.dma("sp", CUMd[:, t0:t0 + ST], cs_[:], s_o[0], reads=[cs_])
            for j in range(ST // 128):
                r0 = t0 + j * 128
                tj = r0 // 128
                q2, k2, vbt, sg = Q2[tix % 2], K2[tix % 2], vb[tix % 2], sgt[tix % 2]
                osem = s_o[tix % 2]
                tix += 1
                for blk in range(8):
                    pp = C.bank()
                    for k in range(8):
                        P.op("pe", lambda e: e.matmul(out=pp[:], lhsT=xnT[:, k, j * 128:(j + 1) * 128],
                                                      rhs=Win[:, k, blk * 512:(blk + 1) * 512], start=(k == 0), stop=(k == 7)),
                             reads=[(xnT, j), (Win, blk // 2)], writes=[pp])
                    if blk < 4:
                        isq = blk < 2
                        P.op("act", lambda e: e.activation(out=sq[:], in_=pp[:], func=AF.Square), reads=[pp], writes=[sq])
                        P.op("dve", lambda e: e.tensor_reduce(out=ssq[:], in_=sq[:].rearrange("p (h d) -> p h d", d=HD), axis=AX.X,
                                                              op=ALU.add), reads=[sq], writes=[ssq])
                        emit_rstd(P, C, (ssq, ssq[:]), (rs, rs[:]), HD)
                        P.op("dve", lambda e: e.tensor_tensor(out=sq[:].rearrange("p (h d) -> p h d", d=HD),
                                                              in0=pp[:].rearrange("p (h d) -> p h d", d=HD),
                                                              in1=rs[:].unsqueeze(2).to_broadcast([128, 8, HD]), op=ALU.mult),
                             reads=[pp, rs], writes=[sq])
                        gbc = qn_bc if isq else kn_bc
                        P.op("dve", lambda e: e.scalar_tensor_tensor(out=qb[:].rearrange("p (h d) -> p h d", d=HD),
                                                                     in0=sq[:].rearrange("p (h d) -> p h d", d=HD),
                                                                     scalar=(1.0 if isq else HD ** -0.5),
                                                                     in1=gbc[:].unsqueeze(1).to_broadcast([128, 8, HD]),
                                                                     op0=ALU.mult, op1=ALU.mult), reads=[sq, gbc], writes=[qb])
                        for r in range(4):
                            P.op("pe", lambda e: e.transpose(out=C.trb[:, r * 128:(r + 1) * 128], in_=qb[:, r * 128:(r + 1) * 128],
                                                             identity=C.identb[:]), reads=[qb, C.identb], writes=[C.trb])
                        dst = q2 if isq else k2
                        P.op("act", lambda e: e.copy(out=dst[:, (blk % 2) * 4:(blk % 2) * 4 + 4, :],
                                                     in_=C.trb[:, 0:512].rearrange("p (r t) -> p r t", r=4)),
                             reads=[C.trb], writes=[(dst, blk % 2)])
                    elif blk < 6:
                        P.op("act", lambda e: e.copy(out=vbt[:, (blk - 4) * 8:(blk - 4) * 8 + 8, 0:HD],
                                                     in_=pp[:].rearrange("p (h d) -> p h d", d=HD)),
                             reads=[pp], writes=[(vbt, blk - 4)])
                    else:
                        P.op("act", lambda e: e.activation(out=sg[:, (blk - 6) * 512:(blk - 5) * 512], in_=pp[:], func=AF.Sigmoid),
                             reads=[pp], writes=[(sg, blk - 6)])
                P.dma("sp", QTd[:, r0:r0 + 128].rearrange("(pr p) t -> p pr t", p=128), q2[:], osem, reads=[q2])
                P.dma("sp", KTd[:, r0:r0 + 128].rearrange("(pr p) t -> p pr t", p=128), k2[:], osem, reads=[k2])
                P.dma("sp", Vd[:, :, tj * 65:(tj + 1) * 65].rearrange("h p c -> p h c"), vbt[:], osem, reads=[vbt])
                P.dma("sp", SGd[r0:r0 + 128, :], sg[:], osem, reads=[sg])
    with Phase(P):
        s_l = [P.dsem(f"d_fxl{i}") for i in range(2)]
        s_c2 = P.dsem("d_fxc2")
        s_o2 = P.dsem("d_fxo2")
        cumT = P.sb("fx_cumT", [NH, S], F32)
        P.dma("sp", cumT[:], CUMd, s_c2, writes=[cumT])
        cumtm = P.sb("fx_cumtm", [128, NT_, NH], F32)
        for t in range(NT_):
            P.op("pe", lambda e: e.transpose(out=C.small[:, 0:NH], in_=cumT[:, t * 128:(t + 1) * 128], identity=C.identf[0:NH, 0:NH]),
                 reads=[cumT, C.identf], writes=[C.small])
            P.op("dve", lambda e: e.tensor_copy(out=cumtm[:, t, :], in_=C.small[:, 0:NH]), reads=[C.small], writes=[(cumtm, t)])
        E0 = P.sb("fx_E0", [128, 128], F32)
        P.op("pool", lambda e: e.memset(E0[:], 0.0), writes=[E0])
        P.op("pool", lambda e: e.memset(E0[0:1, :], 1.0), reads=[E0], writes=[E0])
        Cbc = P.sb("fx_Cbc", [128, NQ, NH], F32)
        nCbc = P.sb("fx_nCbc", [128, NQ, NH], F32)
        for qt_ in range(NQ):
            P.op("pe", lambda e: e.matmul(out=C.small[:, 0:NH], lhsT=E0[:], rhs=cumtm[:, 4 * qt_, :], start=True, stop=True),
                 reads=[E0, (cumtm, 4 * qt_)], writes=[C.small])
            P.op("dve", lambda e: e.tensor_copy(out=Cbc[:, qt_, :], in_=C.small[:, 0:NH]), reads=[C.small], writes=[(Cbc, qt_)])
        P.op("dve", lambda e: e.tensor_scalar(out=nCbc[:], in0=Cbc[:], scalar1=-1.0, scalar2=None, op0=ALU.mult),
             reads=[Cbc], writes=[nCbc])
        Sel = P.sb("fx_Sel", [NH, NH, 128], F32)
        P.op("pool", lambda e: e.memset(Sel[:], 0.0), writes=[Sel])
        for col in (64, 96):
            P.op("dve", lambda e: e.tensor_copy(out=Sel[:, :, col], in_=C.identf[0:NH, 0:NH]), reads=[Sel, C.identf], writes=[Sel])
        mneg = P.sb("fx_mneg", [128, 4, ST], F32)
        P.op("pool", lambda e: e.memset(mneg[:], 0.0), writes=[mneg])
        for r in range(4):
            P.op("pool", lambda e: e.affine_select(out=mneg[:, r, :], in_=mneg[:, r, :], pattern=[[1, ST]], compare_op=ALU.is_ge,
                                                   fill=-30000.0, base=-r * 128, channel_multiplier=-1),
                 reads=[mneg], writes=[mneg])
        KT = [P.sb(f"fx_KT{i}", [128, S], BF16) for i in range(2)]
        QT = [P.sb(f"fx_QT{i}", [128, S], BF16) for i in range(2)]
        VV = [P.sb(f"fx_VV{i}", [128, NT_ * 65], BF16) for i in range(2)]
        for i in range(2):
            P.op("pool", lambda e: e.memset(KT[i][64:128, :], 0.0), writes=[KT[i]])
            P.op("pool", lambda e: e.memset(KT[i][64:65, :], 1.0), reads=[KT[i]], writes=[KT[i]])
            P.op("pool", lambda e: e.memset(KT[i][96:97, :], 1.0), reads=[KT[i]], writes=[KT[i]])
            P.op("pool", lambda e: e.memset(QT[i][64:128, :], 0.0), writes=[QT[i]])
        bias = P.sb("fx_bias", [128, NT_], F32)
        hi96 = P.sb("fx_hi96", [128, ST], BF16)
        smk = [P.sb(f"fx_smk{i}", [128, ST], F32) for i in range(2)]
        PT = [P.sb(f"fx_PT{i}", [128, ST], BF16) for i in range(3)]
        OTs = P.sb("fx_OTs", [65, ST], F32)
        rden = P.sb("fx_rden", [128, 4], F32)
        otm = [P.sb(f"fx_otm{i}", [128, 4, HD], F32) for i in range(2)]
        OTp, TRp = C.trp[0], C.trp[1]
        pti = 0
        for h in range(NH):
            kt_, qt2, vv, sl = KT[h % 2], QT[h % 2], VV[h % 2], s_l[h % 2]
            P.dma("sp", kt_[0:HD, :], KTd[h * HD:(h + 1) * HD, :], sl, writes=[kt_])
            P.dma("sp", qt2[0:HD, :], QTd[h * HD:(h + 1) * HD, :], sl, writes=[qt2])
            P.dma("sp", vv[:], Vd[h], sl, writes=[vv])
            for Q in range(NQ):
                qs_ = slice(Q * ST, (Q + 1) * ST)
                nkt = 4 * (Q + 1)
                pgm = C.bank()
                P.op("pe", lambda e: e.matmul(out=pgm[:], lhsT=Sel[:, h, :], rhs=cumT[:, qs_], start=True, stop=True),
                     reads=[Sel, cumT], writes=[pgm])
                P.op("act", lambda e: e.activation(out=qt2[64:65, qs_], in_=pgm[64:65, :], func=AF.Identity,
                                                   bias=nCbc[64:65, Q, h:h + 1]), reads=[pgm, nCbc], writes=[qt2])
                P.op("act", lambda e: e.activation(out=hi96[96:97, :], in_=pgm[96:97, :], func=AF.Identity,
                                                   bias=nCbc[96:97, Q, h:h + 1]), reads=[pgm, nCbc], writes=[hi96])
                P.op("dve", lambda e: e.scalar_tensor_tensor(out=qt2[96:97, qs_], in0=pgm[96:97, :], scalar=Cbc[96:97, Q, h:h + 1],
                                                             in1=hi96[96:97, :], op0=ALU.subtract, op1=ALU.subtract),
                     reads=[pgm, Cbc, hi96], writes=[qt2])
                P.op("dve", lambda e: e.tensor_scalar(out=bias[:, 0:nkt], in0=cumtm[:, 0:nkt, h], scalar1=-1.0,
                                                      scalar2=Cbc[:, Q, h:h + 1], op0=ALU.mult, op1=ALU.add),
                     reads=[cumtm, Cbc], writes=[bias])
                for kt in range(nkt):
                    ps_ = C.bank()
                    P.op("pe", lambda e: e.matmul(out=ps_[:], lhsT=kt_[:, kt * 128:(kt + 1) * 128], rhs=qt2[:, qs_],
                                                  start=True, stop=True), reads=[kt_, qt2], writes=[ps_])
                    pt = PT[pti % 3]
                    pti += 1
                    r = kt - 4 * Q
                    if r >= 0:
                        sm = smk[r % 2]
                        P.op("dve", lambda e: e.tensor_tensor(out=sm[:], in0=ps_[:], in1=mneg[:, r, :], op=ALU.add),
                             reads=[ps_, mneg], writes=[sm])
                        P.op("act", lambda e: e.activation(out=pt[:], in_=sm[:], func=AF.Exp, bias=bias[:, kt:kt + 1]),
                             reads=[sm, bias], writes=[pt])
                    else:
                        P.op("act", lambda e: e.activation(out=pt[:], in_=ps_[:], func=AF.Exp, bias=bias[:, kt:kt + 1]),
                             reads=[ps_, bias], writes=[pt])
                    P.op("pe", lambda e: e.matmul(out=OTp[0:65, :], lhsT=vv[:, kt * 65:(kt + 1) * 65], rhs=pt[:],
                                                  start=(kt == 0), stop=(kt == nkt - 1)), reads=[vv, pt], writes=[OTp])
                P.op("act", lambda e: e.copy(out=OTs[:], in_=OTp[0:65, :]), reads=[OTp], writes=[OTs])
                for r in range(4):
                    P.op("pe", lambda e: e.transpose(out=TRp[:, r * 128:r * 128 + 65], in_=OTs[:, r * 128:(r + 1) * 128],
                                                     identity=C.identf[0:65, 0:65]), reads=[OTs, C.identf], writes=[TRp])
                ot = otm[(h * NQ + Q) % 2]
                trv = TRp[:].rearrange("p (r c) -> p r c", r=4)
                P.op("dve", lambda e: e.reciprocal(out=rden[:], in_=trv[:, :, 64]), reads=[TRp], writes=[rden])
                P.op("dve", lambda e: e.tensor_tensor(out=ot[:], in0=trv[:, :, 0:HD], in1=rden[:].unsqueeze(2).to_broadcast([128, 4, HD]),
                                                      op=ALU.mult), reads=[TRp, rden], writes=[ot])
                P.dma("sp", Od[Q * ST:(Q + 1) * ST, h * HD:(h + 1) * HD].rearrange("(t p) d -> p t d", p=128), ot[:], s_o2, reads=[ot])
    with Phase(P):
        s_w3 = P.dsem("d_fxw3")
        s_l3 = [P.dsem(f"d_fxl3{i}") for i in range(2)]
        s_o3 = P.dsem("d_fxo3")
        Wout = P.sb("fx_wout", [128, 8, D], BF16)
        load_w_bf16(P, Wout, Wout[:], w_out, s_w3)
        ob_ = [P.sb(f"fx_o{i}", [128, D], F32) for i in range(2)]
        sb_ = [P.sb(f"fx_s{i}", [128, D], F32) for i in range(2)]
        yb = P.sb("fx_yb", [128, D], BF16)
        yT = P.sb("fx_yT", [128, 8, ST], BF16)
        obufs = [P.sb(f"fx_ob{i}", [128, D], F32) for i in range(2)]
        ocnt = [0]
        ti = 0
        for st in range(S // ST):
            t0 = st * ST
            for j in range(ST // 128):
                r0 = t0 + j * 128
                o_, s_, sl = ob_[ti % 2], sb_[ti % 2], s_l3[ti % 2]
                ti += 1
                P.dma("sp", o_[:], Od[r0:r0 + 128, :], sl, writes=[o_])
                P.dma("sp", s_[:], SGd[r0:r0 + 128, :], sl, writes=[s_])
                P.op("dve", lambda e: e.tensor_tensor(out=yb[:], in0=o_[:], in1=s_[:], op=ALU.mult), reads=[o_, s_], writes=[yb])
                for k in range(8):
                    P.op("pe", lambda e: e.transpose(out=C.trb[:, k * 128:(k + 1) * 128], in_=yb[:, k * 128:(k + 1) * 128],
                                                     identity=C.identb[:]), reads=[yb, C.identb], writes=[C.trb])
                P.op("act", lambda e: e.copy(out=yT[:, :, j * 128:(j + 1) * 128], in_=C.trb[:].rearrange("p (k t) -> p k t", k=8)),
                     reads=[C.trb], writes=[(yT, j)])
            emit_outproj(P, C, yT, Wout, ST // 128, lambda j: mix_dst[t0 + j * 128:t0 + (j + 1) * 128, :], mix_tile, s_o3,
                         obufs, ocnt)


DEPTH = 4
IN_SHAPES = {
    "norm_mix": [4, D], "norm_ffn": [4, D], "hg_w_in": [2, D, 4 * D], "hg_w_out": [2, D, D], "hg_gnorm": [2, 128],
    "hg_lb_param": [4, D], "fox_w_in": [1, D, 4 * D + 16], "fox_f_bias": [1, 16], "fox_qnorm": [1, 64], "fox_knorm": [1, 64],
    "fox_w_out": [1, D, D], "rg_w_in": [1, D, 2 * D], "rg_conv_w": [1, 4, D], "rg_conv_b": [1, D], "rg_wa": [1, 4, 256, 256],
    "rg_ba": [1, D], "rg_wx": [1, 4, 256, 256], "rg_bx": [1, D], "rg_lambda": [1, D], "rg_w_out": [1, D, D],
    "router_w": [4, D, NE], "router_b": [4, NE], "moe_w_gu": [4, NE, D, 2 * D], "moe_b_gu": [4, NE, 2 * D],
    "moe_w_dn": [4, NE, D, D], "moe_b_dn": [4, NE, D], "ple_w": [4, 256, D], "ple_norm": [4, D], "ple_gate_norm": [4, D],
    "ple_gate_w": [4, D, D],
}


def build_full(S, depth=DEPTH):
    P = Prog()
    C = Ctx(P)
    nc = P.nc
    a = {}
    x = P.dram("i_x", [S, D], F32, "ExternalInput").ap()
    p = P.dram("i_p", [4, S, 256], F32, "ExternalInput").ap()
    for n, shp in IN_SHAPES.items():
        a[n] = P.dram("i_" + n, shp, F32, "ExternalInput").ap()
    out = P.dram("o_h", [S, D], F32, "ExternalOutput").ap()
    hb = [nc.dram_tensor(f"hbuf{i}", [S, D], F32, kind="Internal").ap() for i in range(2)]
    mixd = nc.dram_tensor("mixd", [S, D], F32, kind="Internal").ap()
    hsrc = x
    for i in range(depth):
        kind, j = i % 3, i // 3
        if kind == 0:
            emit_hgrn2(P, C, S, i, hsrc, None, mixd, None, a["norm_mix"][i], a["hg_w_in"][j], a["hg_w_out"][j], a["hg_gnorm"][j],
                       a["hg_lb_param"])
        elif kind == 1:
            emit_fox(P, C, S, hsrc, None, mixd, None, a["norm_mix"][i], a["fox_w_in"][j], a["fox_f_bias"][j], a["fox_qnorm"][j],
                     a["fox_knorm"][j], a["fox_w_out"][j])
        else:
            emit_rglru(P, C, S, hsrc, None, mixd, None, a["norm_mix"][i], a["rg_w_in"][j], a["rg_conv_w"][j], a["rg_conv_b"][j],
                       a["rg_wa"][j], a["rg_ba"][j], a["rg_wx"][j], a["rg_bx"][j], a["rg_lambda"][j], a["rg_w_out"][j])
        dst = out if i == depth - 1 else hb[i % 2]
        emit_ffn(P, C, S, hsrc, [mixd], p[i], a["norm_ffn"][i], a["router_w"][i], a["router_b"][i], a["moe_w_gu"][i], a["moe_b_gu"][i],
                 a["moe_w_dn"][i], a["moe_b_dn"][i], a["ple_w"][i], a["ple_norm"][i], a["ple_gate_norm"][i], a["ple_gate_w"][i], dst)
        hsrc = dst
    n_inst = P.n_inst
    ncf = P.finish(P.dsems)
    return ncf, n_inst


_NC_CACHE = {}


def kernel(**inputs):
    x = np.ascontiguousarray(np.asarray(inputs["x"], dtype=np.float32))
    p = np.asarray(inputs["p"], dtype=np.float32)
    B, S, _ = x.shape
    if S not in _NC_CACHE:
        _NC_CACHE[S] = build_full(S)[0]
    nc = _NC_CACHE[S]
    shared = {"i_" + n: np.ascontiguousarray(np.asarray(inputs[n], dtype=np.float32)) for n in IN_SHAPES}
    in_maps = []
    for c in range(B):
        m = dict(shared)
        m["i_x"] = np.ascontiguousarray(x[c])
        m["i_p"] = np.ascontiguousarray(p[:, c])
        in_maps.append(m)
    res = run_bass_kernel_spmd(nc, in_maps, core_ids=list(range(B)))
    return np.stack([np.asarray(res.results[c]["o_h"], dtype=np.float32) for c in range(B)], axis=0)
```

```python
import numpy as np
import concourse.bass as bass
import concourse.mybir as mybir
from concourse.bass_utils import run_bass_kernel_spmd
from contextlib import ExitStack

F32 = mybir.dt.float32
BF16 = mybir.dt.bfloat16
I32 = mybir.dt.int32
AF = mybir.ActivationFunctionType
ALU = mybir.AluOpType
AX = mybir.AxisListType


class _St:
    __slots__ = ("w", "r")

    def __init__(self):
        self.w = []
        self.r = {}


class Tile:
    def __init__(self, t, name):
        self.t = t
        self.name = name
        self.whole = _St()
        self.parts = {}
        self.psum = False

    def __getitem__(self, idx):
        return self.t[idx]


class Sem:
    def __init__(self, h, name, is_dma=False):
        self.h = h
        self.name = name
        self.is_dma = is_dma
        self.issued = 0
        self.last_wait = 0


class Eng:
    def __init__(self, P, name, e, sem):
        self.P = P
        self.name = name
        self.e = e
        self.sem = sem
        self.known = {}


class Prog:
    def __init__(self):
        self.nc = bass.Bass("TRN2", target_bir_lowering=False)
        self.es = ExitStack()
        self.gstack = self.es
        self.dsems = []
        nc = self.nc
        self.engs = {}
        for nm, e in (("pe", nc.tensor), ("dve", nc.vector), ("act", nc.scalar),
                      ("pool", nc.gpsimd), ("sp", nc.sync)):
            s = Sem(self.es.enter_context(nc.semaphore("s_" + nm)), "s_" + nm)
            self.engs[nm] = Eng(self, nm, e, s)
        self.n_inst = 0

    def dram(self, name, shape, dt, kind):
        return self.nc.dram_tensor(name, list(shape), dt, kind=kind)

    def sb(self, name, shape, dt):
        self.uid = getattr(self, "uid", 0) + 1
        name = f"{name}_u{self.uid}"
        t = self.es.enter_context(self.nc.sbuf_tensor(name, list(shape), dt))
        return Tile(t, name)

    def ps(self, name, shape, dt=F32):
        t = self.es.enter_context(self.nc.psum_tensor(name, list(shape), dt))
        tl = Tile(t, name)
        tl.psum = True
        return tl

    def dsem(self, name):
        return Sem(self.es.enter_context(self.nc.semaphore(name)), name, is_dma=True)

    @staticmethod
    def _norm(lst):
        out = []
        for x in lst or []:
            if x is None:
                continue
            if isinstance(x, tuple):
                out.append(x)
            else:
                out.append((x, None))
        return out

    def _deps(self, reads, writes):
        deps = []
        for (t, k) in reads:
            deps += t.whole.w
            if k is None:
                for st in t.parts.values():
                    deps += st.w
            elif k in t.parts:
                deps += t.parts[k].w
            if t.psum:
                deps += list(t.whole.r.items())
                for st in t.parts.values():
                    deps += list(st.r.items())
        for (t, k) in writes:
            deps += t.whole.w + list(t.whole.r.items())
            if k is None:
                for st in t.parts.values():
                    deps += st.w + list(st.r.items())
            elif k in t.parts:
                deps += t.parts[k].w + list(t.parts[k].r.items())
        return deps

    def _mark(self, reads, writes, tok):
        for (t, k) in reads:
            st = t.whole if k is None else t.parts.setdefault(k, _St())
            if st.r.get(tok[0], 0) < tok[1]:
                st.r[tok[0]] = tok[1]
        for (t, k) in writes:
            if k is None:
                t.whole.w = [tok]
                t.whole.r = {}
                t.parts = {}
            else:
                st = t.parts.setdefault(k, _St())
                st.w = [tok]
                st.r = {}

    def _wait(self, E, deps, skip_self=False):
        need = {}
        for (s, v) in deps:
            if skip_self and s is E.sem:
                continue
            if s.is_dma:
                assert v == s.issued or E.known.get(s, 0) >= v or True
                v = s.issued
            if v > need.get(s, 0):
                need[s] = v
        for s, v in need.items():
            if E.known.get(s, 0) < v:
                E.e.wait_ge(s.h, v)
                E.known[s] = v
                if s.is_dma and v > s.last_wait:
                    s.last_wait = v

    def op(self, eng, fn, reads=None, writes=None):
        E = self.engs[eng]
        reads = self._norm(reads)
        writes = self._norm(writes)
        deps = self._deps(reads, writes)
        self._wait(E, deps, skip_self=(eng == "pe"))
        inst = fn(E.e)
        E.sem.issued += 1
        inst.then_inc(E.sem.h, 1)
        self._mark(reads, writes, (E.sem, E.sem.issued))
        self.n_inst += 1
        return inst

    def dma(self, q, out, in_, sem, reads=None, writes=None, **kw):
        E = self.engs[q]
        reads = self._norm(reads)
        writes = self._norm(writes)
        deps = self._deps(reads, writes)
        self._wait(E, deps)
        if sem.last_wait > E.known.get(sem, 0):
            E.e.wait_ge(sem.h, sem.issued)
            E.known[sem] = sem.issued
        kind = "sw" if q == "pool" else "hw"
        assert getattr(sem, "qkind", kind) == kind, f"sem {sem.name} mixes SW and HW DGE"
        sem.qkind = kind
        inst = E.e.dma_start(out=out, in_=in_, **kw)
        sem.issued += 16
        inst.then_inc(sem.h, 16)
        self._mark(reads, writes, (sem, sem.issued))
        self.n_inst += 1
        return inst

    def finish(self, out_sems):
        E = self.engs["sp"]
        for s in out_sems:
            if E.known.get(s, 0) < s.issued:
                E.e.wait_ge(s.h, s.issued)
                E.known[s] = s.issued
        self.es.close()
        return self.nc


D = 1024
EPS = 1e-6


class Ctx:
    def __init__(self, P):
        self.P = P
        self.identf = P.sb("identf", [128, 128], F32)
        self.identb = P.sb("identb", [128, 128], BF16)
        P.op("pool", lambda e: e.memset(self.identf[:], 0.0), writes=[self.identf])
        P.op("pool", lambda e: e.affine_select(out=self.identf[:], in_=self.identf[:], pattern=[[-1, 128]],
                                               compare_op=ALU.not_equal, fill=1.0, base=0, channel_multiplier=1),
             reads=[self.identf], writes=[self.identf])
        P.op("dve", lambda e: e.tensor_copy(out=self.identb[:], in_=self.identf[:]),
             reads=[self.identf], writes=[self.identb])
        self.mm = [P.ps(f"mm{i}", [128, 512], F32) for i in range(4)]
        self.mm_i = 0
        self.trp = [P.ps(f"trp{i}", [128, 512], F32) for i in range(2)]
        self.trb = P.ps("trb", [128, 1024], BF16)
        self.small = P.ps("smallps", [128, 512], F32)
        self.junk = P.sb("junk", [128, 1024], F32)
        self.st = {}
        for nm, w in (("ss", 1), ("rstd", 1), ("negm", 1), ("ssum", 1), ("ssa", 2), ("ssa1", 1), ("rstda", 1), ("ss2", 1),
                      ("rstd2", 1)):
            self.st[nm] = P.sb("st_" + nm, [128, w], F32)

    def bank(self):
        b = self.mm[self.mm_i % 4]
        self.mm_i += 1
        return b

    def stat(self, name, w=1):
        return self.st[name]


def bc_load(P, q, name, src_ap, n, sem):
    t = P.sb(name, [128, n], F32)
    P.dma(q, t[:], src_ap.partition_broadcast(128), sem, writes=[t])
    return t


def emit_rstd(P, C, ss, rstd, n, p=128):
    (sst, ssa), (rt, ra) = ss, rstd
    P.op("dve", lambda e: e.tensor_scalar(out=ra, in0=ssa, scalar1=1.0 / n, scalar2=EPS,
                                          op0=ALU.mult, op1=ALU.add), reads=[sst], writes=[rt])
    P.op("act", lambda e: e.activation(out=ra, in_=ra, func=AF.Sqrt), reads=[rt], writes=[rt])
    P.op("dve", lambda e: e.reciprocal(out=ra, in_=ra), reads=[rt], writes=[rt])


def load_w_bf16(P, dst, dst_ap, src_ap, sem, key=None):
    P.dma("pool", dst_ap, src_ap.rearrange("(k p) n -> p k n", p=128), sem, writes=[(dst, key)])


GT = 512
NE = 32
SPARSE = True
CAP = 128


def emit_ffn(P, C, NT, h_in, parts, p_in, norm_ffn, router_w, router_b, w_gu, b_gu, w_dn, b_dn, ple_w, ple_norm,
             ple_gate_norm, ple_gate_w, h_out, n_exp=NE, stop=None):
    n_part = len(parts)
    with Phase(P):
        return _emit_ffn(P, C, NT, h_in, parts, p_in, norm_ffn, router_w, router_b, w_gu, b_gu, w_dn, b_dn, ple_w, ple_norm,
                         ple_gate_norm, ple_gate_w, h_out, n_exp, stop, n_part)


def _emit_ffn(P, C, NT, h_in, parts, p_in, norm_ffn, router_w, router_b, w_gu, b_gu, w_dn, b_dn, ple_w, ple_norm,
              ple_gate_norm, ple_gate_w, h_out, n_exp, stop, n_part):
    s_c = P.dsem("d_const")
    s_out = P.dsem("d_out")
    g_ffn = bc_load(P, "sp", "g_ffn", norm_ffn, D, s_c)
    g_ple = bc_load(P, "sp", "g_ple", ple_norm, D, s_c)
    g_gate = bc_load(P, "sp", "g_gate", ple_gate_norm, D, s_c)
    br_bc = bc_load(P, "sp", "br_bc", router_b, NE, s_c)
    wr_sb = P.sb("wr_sb", [128, 8, NE], F32)
    P.dma("sp", wr_sb[:], router_w.rearrange("(k p) n -> p k n", p=128), s_c, writes=[wr_sb])
    s_cw = P.dsem("d_constw")
    plew = P.sb("plew", [128, 2, D], BF16)
    load_w_bf16(P, plew, plew[:], ple_w, s_cw)
    plegw = P.sb("plegw", [128, 8, D], BF16)
    load_w_bf16(P, plegw, plegw[:], ple_gate_w, s_cw)
    big = [P.sb(f"big{i}", [128, D], F32) for i in range(3)]
    s_b = P.dsem("d_brow")
    P.dma("sp", big[0][0:NE, :], b_gu[:, 0:D], s_b, writes=[big[0]])
    P.dma("sp", big[1][0:NE, :], b_gu[:, D:2 * D], s_b, writes=[big[1]])
    P.dma("sp", big[2][0:NE, :], b_dn, s_b, writes=[big[2]])
    bfm = P.sb("bfm", [128, 24, NE], F32)
    for m in range(24):
        brow = big[m // 8]
        P.op("pe", lambda e: e.transpose(out=C.small[:, 0:NE], in_=brow[0:NE, (m % 8) * 128:(m % 8 + 1) * 128],
                                         identity=C.identf[0:NE, 0:NE]),
             reads=[brow, C.identf], writes=[C.small])
        P.op("dve", lambda e: e.tensor_copy(out=bfm[:, m, :], in_=C.small[:, 0:NE]),
             reads=[C.small], writes=[(bfm, m)])

    if stop == "setup":
        P.dma("sp", h_out[0:128, 0:24 * NE], bfm[:].rearrange("p m e -> p (m e)"), s_out, reads=[bfm])
        return None
    NS = 8
    wslot = [P.sb(f"wslot{i}", [128, 8, 512], BF16) for i in range(NS)]
    wsem = [P.dsem(f"d_w{i}") for i in range(NS)]
    hsem = [P.dsem(f"d_h{i}") for i in range(2)]
    pbuf = [P.sb(f"pb{i}", [128, 256], F32) for i in range(2)]
    psem = [P.dsem(f"d_p{i}") for i in range(2)]
    h1 = P.sb("h1", [128, GT // 128, D], F32)
    xn = P.sb("xn", [128, D], F32)
    xnb = P.sb("xnb", [128, D], BF16)
    xnTf = P.sb("xnTf", [128, 8, 128], F32)
    G = P.sb("G", [NE, GT], F32)
    if not SPARSE:
        xnT = P.sb("xnT", [128, 8, GT], BF16)
        selt = P.sb("selt", [NE, 128], F32)
        Gbc = P.sb("Gbc", [128, GT], F32)
        actT = P.sb("actT", [128, 8, GT], BF16)
        yacc = P.sb("yacc", [128, 8, GT], F32)
    else:
        xnT = actT = yacc = None
        xn_tm = P.sb("xn_tm", [128, GT // 128, D], BF16)
        gates_tm = P.sb("gates_tm", [128, GT // 128, NE], F32)
        Rk = P.sb("Rk", [NE, GT], BF16)
        rks = P.sb("rks", [128, NE], F32)
        cnt_bc = P.sb("cnt_bc", [128, NE], F32)
        Ltri = P.sb("Ltri", [128, 128], F32)
        onesf = P.sb("onesf", [128, 128], F32)
        pidx = P.sb("pidx", [128, 1], F32)
        selb = P.sb("selb", [NE, 128], BF16)
        SelT = P.sb("SelT", [128, GT], BF16)
        Selsb = P.sb("Selsb", [128, GT // 128, 128], BF16)
        XcT = P.sb("XcT", [128, 8, CAP], BF16)
        actS = P.sb("actS", [128, 8, CAP], BF16)
        Ycs = P.sb("Ycs", [128, D], BF16)
        yacc_tm = P.sb("yacc_tm", [128, GT // 128, D], F32)
        bdnr = P.sb("bdnr", [NE, D], F32)
        P.dma("sp", bdnr[:], b_dn, s_b, writes=[bdnr])
        P.op("pool", lambda e: e.memset(onesf[:], 1.0), writes=[onesf])
        P.op("pool", lambda e: e.memset(Ltri[:], 1.0), writes=[Ltri])
        P.op("pool", lambda e: e.affine_select(out=Ltri[:], in_=Ltri[:], pattern=[[1, 128]], compare_op=ALU.is_ge, fill=0.0,
                                               base=-1, channel_multiplier=-1), reads=[Ltri], writes=[Ltri])
        P.op("pool", lambda e: e.iota(pidx[:], pattern=[[0, 1]], base=0, channel_multiplier=1,
                                      allow_small_or_imprecise_dtypes=True), writes=[pidx])
    NEW = 6
    ew = [P.sb(f"ew{i}", [128, GT], F32) for i in range(NEW)]
    ew_i = [0]

    def tmp():
        t = ew[ew_i[0] % NEW]
        ew_i[0] += 1
        return t
    lg = P.sb("lg", [128, NE], F32)
    top8 = P.sb("top8", [128, 8], F32)
    msk = P.sb("msk", [128, NE], F32)
    exs = P.sb("exs", [128, NE], F32)
    pT = P.sb("pT", [128, 2, 128], BF16)
    hnT = P.sb("hnT", [128, 8, 128], BF16)
    otile = [P.sb(f"otile{i}", [128, D], F32) for i in range(1)]

    NG = NT // GT
    JT = GT // 128
    chunks = []
    for g in range(NG):
        for e in range(n_exp):
            chunks.append(w_gu[e][:, 0:512])
            chunks.append(w_gu[e][:, D:D + 512])
            chunks.append(w_gu[e][:, 512:D])
            chunks.append(w_gu[e][:, D + 512:2 * D])
            chunks.append(w_dn[e][:, 0:512])
            chunks.append(w_dn[e][:, 512:D])
    issued = [0]

    def ensure(ci):
        while issued[0] <= min(ci, len(chunks) - 1):
            c = issued[0]
            load_w_bf16(P, wslot[c % NS], wslot[c % NS][:], chunks[c], wsem[c % NS])
            issued[0] += 1

    for g in range(NG):
        t0 = g * GT
        for j in range(JT):
            r0 = t0 + j * 128
            P.dma("sp", h1[:, j, :], h_in[r0:r0 + 128, :], hsem[0], writes=[(h1, j)])
            for i in range(n_part):
                stg = big[1 + i % 2]
                P.dma("sp", stg[:], parts[i][r0:r0 + 128, :], hsem[1], writes=[stg])
                P.op("pool", lambda e: e.tensor_tensor(out=h1[:, j, :], in0=h1[:, j, :], in1=stg[:], op=ALU.add),
                     reads=[(h1, j), stg], writes=[(h1, j)])
            def bail(ap, rd, w):
                P.dma("sp", h_out[0:ap.shape[0], 0:w], ap, s_out, reads=rd)
                return None
            if stop == "A1":
                return bail(h1[:, 0, :], [h1], 1024)
            ss = C.stat("ss")
            rstd = C.stat("rstd")
            P.op("act", lambda e: e.activation(out=C.junk[:], in_=h1[:, j, :], func=AF.Square, accum_out=ss[:]),
                 reads=[(h1, j)], writes=[C.junk, ss])
            emit_rstd(P, C, (ss, ss[:]), (rstd, rstd[:]), D)
            P.op("dve", lambda e: e.scalar_tensor_tensor(out=xn[:], in0=h1[:, j, :], scalar=rstd[:, 0:1], in1=g_ffn[:],
                                                         op0=ALU.mult, op1=ALU.mult),
                 reads=[(h1, j), rstd, g_ffn], writes=[xn])
            if stop == "A2":
                return bail(xn[:], [xn], 1024)
            for k in range(8):
                tr = C.trp[k // 4]
                P.op("pe", lambda e: e.transpose(out=tr[:, (k % 4) * 128:(k % 4 + 1) * 128],
                                                 in_=xn[:, k * 128:(k + 1) * 128], identity=C.identf[:]),
                     reads=[xn, C.identf], writes=[(tr, k % 4)])
            import os as _os
            VAR = _os.environ.get("VAR", "")
            for hh in range(2):
                tr = C.trp[hh]
                if VAR != "1" and not SPARSE:
                    P.op("act", lambda e: e.copy(out=xnT[:, hh * 4:(hh + 1) * 4, j * 128:(j + 1) * 128],
                                                 in_=tr[:].rearrange("p (k t) -> p k t", k=4)),
                         reads=[tr], writes=[(xnT, (j, hh))])
                if VAR != "2":
                    P.op("dve", lambda e: e.tensor_copy(out=xnTf[:, hh * 4:(hh + 1) * 4, :],
                                                        in_=tr[:].rearrange("p (k t) -> p k t", k=4)),
                         reads=[tr], writes=[(xnTf, hh)])
            if SPARSE:
                P.op("pool", lambda e: e.tensor_copy(out=xn_tm[:, j, :], in_=xn[:]), reads=[xn], writes=[(xn_tm, j)])
            if stop == "A3":
                return bail(xnTf[:].rearrange("p k t -> p (k t)"), [xnTf], 1024)
            for k in range(8):
                P.op("pe", lambda e: e.matmul(out=C.small[:, 0:NE], lhsT=xnTf[:, k, :], rhs=wr_sb[:, k, :],
                                              start=(k == 0), stop=(k == 7)),
                     reads=[(xnTf, k // 4), wr_sb], writes=[C.small])
            P.op("dve", lambda e: e.tensor_tensor(out=lg[:], in0=C.small[:, 0:NE], in1=br_bc[:], op=ALU.add),
                 reads=[C.small, br_bc], writes=[lg])
            if stop == "A4":
                return bail(lg[:], [lg], NE)
            P.op("dve", lambda e: e.max(out=top8[:], in_=lg[:]), reads=[lg], writes=[top8])
            P.op("dve", lambda e: e.tensor_scalar(out=msk[:], in0=lg[:], scalar1=top8[:, 3:4], scalar2=None,
                                                  op0=ALU.is_ge), reads=[lg, top8], writes=[msk])
            negm = C.stat("negm")
            P.op("dve", lambda e: e.tensor_scalar(out=negm[:], in0=top8[:, 0:1], scalar1=-1.0, scalar2=None,
                                                  op0=ALU.mult), reads=[top8], writes=[negm])
            P.op("act", lambda e: e.activation(out=exs[:], in_=lg[:], func=AF.Exp, bias=negm[:, 0:1], scale=1.0),
                 reads=[lg, negm], writes=[exs])
            P.op("dve", lambda e: e.tensor_tensor(out=exs[:], in0=exs[:], in1=msk[:], op=ALU.mult),
                 reads=[exs, msk], writes=[exs])
            ssum = C.stat("ssum")
            P.op("dve", lambda e: e.tensor_reduce(out=ssum[:], in_=exs[:], axis=AX.X, op=ALU.add),
                 reads=[exs], writes=[ssum])
            P.op("dve", lambda e: e.reciprocal(out=ssum[:], in_=ssum[:]), reads=[ssum], writes=[ssum])
            P.op("dve", lambda e: e.tensor_scalar(out=exs[:], in0=exs[:], scalar1=ssum[:, 0:1], scalar2=None,
                                                  op0=ALU.mult), reads=[exs, ssum], writes=[exs])
            if stop == "A5":
                return bail(exs[:], [exs], NE)
            P.op("pe", lambda e: e.transpose(out=C.small[0:NE, 128:256], in_=exs[:], identity=C.identf[:]),
                 reads=[exs, C.identf], writes=[C.small])
            P.op("dve", lambda e: e.tensor_copy(out=G[:, j * 128:(j + 1) * 128], in_=C.small[0:NE, 128:256]),
                 reads=[C.small], writes=[(G, j)])
            if SPARSE:
                P.op("pool", lambda e: e.tensor_copy(out=gates_tm[:, j, :], in_=exs[:]), reads=[exs], writes=[(gates_tm, j)])
                if j == 0:
                    P.op("pool", lambda e: e.memset(cnt_bc[:], 0.0), writes=[cnt_bc])
                P.op("pe", lambda e: e.matmul(out=C.small[:, 256:256 + NE], lhsT=Ltri[:], rhs=msk[:], start=True, stop=True),
                     reads=[Ltri, msk], writes=[C.small])
                P.op("dve", lambda e: e.tensor_tensor(out=rks[:], in0=C.small[:, 256:256 + NE], in1=cnt_bc[:], op=ALU.add),
                     reads=[C.small, cnt_bc], writes=[rks])
                P.op("pe", lambda e: e.matmul(out=C.small[:, 320:320 + NE], lhsT=onesf[:], rhs=msk[:], start=True, stop=True),
                     reads=[onesf, msk], writes=[C.small])
                P.op("dve", lambda e: e.tensor_tensor(out=cnt_bc[:], in0=C.small[:, 320:320 + NE], in1=cnt_bc[:], op=ALU.add),
                     reads=[C.small, cnt_bc], writes=[cnt_bc])
                P.op("dve", lambda e: e.scalar_tensor_tensor(out=rks[:], in0=rks[:], scalar=-255.0, in1=msk[:], op0=ALU.add,
                                                             op1=ALU.mult), reads=[rks, msk], writes=[rks])
                P.op("dve", lambda e: e.tensor_scalar(out=rks[:], in0=rks[:], scalar1=255.0, scalar2=None, op0=ALU.add),
                     reads=[rks], writes=[rks])
                P.op("pe", lambda e: e.transpose(out=C.small[0:NE, 128:256], in_=rks[:], identity=C.identf[:]),
                     reads=[rks, C.identf], writes=[C.small])
                P.op("dve", lambda e: e.tensor_copy(out=Rk[:, j * 128:(j + 1) * 128], in_=C.small[0:NE, 128:256]),
                     reads=[C.small], writes=[(Rk, j)])

        if stop == "A":
            P.dma("sp", h_out[0:NE, 0:GT], G[:], s_out, reads=[G])
            return None
        for ei in (range(n_exp) if SPARSE else []):
            ci = (g * n_exp + ei) * 6
            P.op("dve", lambda e: e.tensor_copy(out=selb[:], in_=C.identb[0:NE, ei:ei + 1].to_broadcast([NE, 128])),
                 reads=[C.identb], writes=[selb])
            pr = C.bank()
            P.op("pe", lambda e: e.matmul(out=pr[:], lhsT=selb[:], rhs=Rk[:], start=True, stop=True), reads=[selb, Rk], writes=[pr])
            P.op("dve", lambda e: e.tensor_scalar(out=SelT[:], in0=pr[:], scalar1=pidx[:, 0:1], scalar2=None, op0=ALU.is_equal),
                 reads=[pr, pidx], writes=[SelT])
            for tc in range(JT):
                P.op("pe", lambda e: e.transpose(out=C.trb[:, tc * 128:(tc + 1) * 128], in_=SelT[:, tc * 128:(tc + 1) * 128],
                                                 identity=C.identb[:]), reads=[SelT, C.identb], writes=[C.trb])
            P.op("act", lambda e: e.copy(out=Selsb[:], in_=C.trb[:, 0:GT].rearrange("p (c s) -> p c s", c=JT)),
                 reads=[C.trb], writes=[Selsb])
            for hf in range(2):
                pgx = C.bank()
                for kk in range(4):
                    k = hf * 4 + kk
                    for tc in range(JT):
                        P.op("pe", lambda e: e.matmul(out=pgx[:, kk * 128:(kk + 1) * 128], lhsT=xn_tm[:, tc, k * 128:(k + 1) * 128],
                                                      rhs=Selsb[:, tc, :], start=(tc == 0), stop=(tc == JT - 1)),
                             reads=[xn_tm, Selsb], writes=[pgx])
                if hf == 0:
                    P.op("act", lambda e: e.copy(out=XcT[:, 0:4, :], in_=pgx[:].rearrange("p (k s) -> p k s", k=4)),
                         reads=[pgx], writes=[(XcT, 0)])
                else:
                    P.op("dve", lambda e: e.tensor_copy(out=XcT[:, 4:8, :], in_=pgx[:].rearrange("p (k s) -> p k s", k=4)),
                         reads=[pgx], writes=[(XcT, 1)])
            for m in range(8):
                ms = slice((m % 4) * 128, (m % 4 + 1) * 128)
                if m % 4 == 0:
                    ensure(ci + 2 * (m // 4) + 7)
                wa, wb = wslot[(ci + 2 * (m // 4)) % NS], wslot[(ci + 2 * (m // 4) + 1) % NS]
                pgl = C.bank()
                for k in range(8):
                    P.op("pe", lambda e: e.matmul(out=pgl[:, 0:CAP], lhsT=wa[:, k, ms], rhs=XcT[:, k, :], start=(k == 0), stop=(k == 7)),
                         reads=[wa, XcT], writes=[pgl])
                pli = C.bank()
                for k in range(8):
                    P.op("pe", lambda e: e.matmul(out=pli[:, 0:CAP], lhsT=wb[:, k, ms], rhs=XcT[:, k, :], start=(k == 0), stop=(k == 7)),
                         reads=[wb, XcT], writes=[pli])
                g1, sg, l1 = tmp(), tmp(), tmp()
                P.op("dve", lambda e: e.tensor_scalar(out=g1[:, 0:CAP], in0=pgl[:, 0:CAP], scalar1=bfm[:, m, ei:ei + 1], scalar2=7.0,
                                                      op0=ALU.add, op1=ALU.min), reads=[pgl, bfm], writes=[g1])
                P.op("act", lambda e: e.activation(out=sg[:, 0:CAP], in_=g1[:, 0:CAP], func=AF.Sigmoid, scale=1.702),
                     reads=[g1], writes=[sg])
                P.op("dve", lambda e: e.tensor_scalar(out=l1[:, 0:CAP], in0=pli[:, 0:CAP], scalar1=bfm[:, 8 + m, ei:ei + 1], scalar2=7.0,
                                                      op0=ALU.add, op1=ALU.min), reads=[pli, bfm], writes=[l1])
                P.op("dve", lambda e: e.tensor_scalar(out=l1[:, 0:CAP], in0=l1[:, 0:CAP], scalar1=-7.0, scalar2=1.0,
                                                      op0=ALU.max, op1=ALU.add), reads=[l1], writes=[l1])
                P.op("pool", lambda e: e.tensor_tensor(out=g1[:, 0:CAP], in0=g1[:, 0:CAP], in1=sg[:, 0:CAP], op=ALU.mult),
                     reads=[g1, sg], writes=[g1])
                P.op("pool", lambda e: e.tensor_tensor(out=actS[:, m, :], in0=g1[:, 0:CAP], in1=l1[:, 0:CAP], op=ALU.mult),
                     reads=[g1, l1], writes=[(actS, m)])
            for hf in range(2):
                ensure(ci + 4 + hf + 7)
                wd = wslot[(ci + 4 + hf) % NS]
                py = C.bank()
                for k in range(8):
                    P.op("pe", lambda e: e.matmul(out=py[:], lhsT=actS[:, k, :], rhs=wd[:, k, :], start=(k == 0), stop=(k == 7)),
                         reads=[wd, (actS, k)], writes=[py])
                if hf == 0:
                    P.op("act", lambda e: e.copy(out=Ycs[:, 0:512], in_=py[:]), reads=[py], writes=[(Ycs, 0)])
                else:
                    P.op("dve", lambda e: e.tensor_copy(out=Ycs[:, 512:1024], in_=py[:]), reads=[py], writes=[(Ycs, 1)])
            for tc in range(JT):
                for hf in range(2):
                    hs = slice(hf * 512, (hf + 1) * 512)
                    psc = C.bank()
                    P.op("pe", lambda e: e.matmul(out=psc[:], lhsT=SelT[:, tc * 128:(tc + 1) * 128], rhs=Ycs[:, hs], start=True, stop=True),
                         reads=[SelT, (Ycs, hf)], writes=[psc])
                    if ei == 0:
                        P.op("dve", lambda e: e.tensor_scalar(out=yacc_tm[:, tc, hs], in0=psc[:], scalar1=gates_tm[:, tc, ei:ei + 1],
                                                              scalar2=None, op0=ALU.mult),
                             reads=[psc, (gates_tm, tc)], writes=[(yacc_tm, (tc, hf))])
                    else:
                        P.op("dve", lambda e: e.scalar_tensor_tensor(out=yacc_tm[:, tc, hs], in0=psc[:], scalar=gates_tm[:, tc, ei:ei + 1],
                                                                     in1=yacc_tm[:, tc, hs], op0=ALU.mult, op1=ALU.add),
                             reads=[psc, (gates_tm, tc), (yacc_tm, (tc, hf))], writes=[(yacc_tm, (tc, hf))])
        if SPARSE:
            for tc in range(JT):
                for hf in range(2):
                    hs = slice(hf * 512, (hf + 1) * 512)
                    pbd = C.bank()
                    P.op("pe", lambda e: e.matmul(out=pbd[:], lhsT=G[:, tc * 128:(tc + 1) * 128], rhs=bdnr[:, hs], start=True, stop=True),
                         reads=[G, bdnr], writes=[pbd])
                    P.op("dve", lambda e: e.tensor_tensor(out=yacc_tm[:, tc, hs], in0=pbd[:], in1=yacc_tm[:, tc, hs], op=ALU.add),
                         reads=[pbd, (yacc_tm, (tc, hf))], writes=[(yacc_tm, (tc, hf))])
        for ei in (range(n_exp) if not SPARSE else []):
            ci = (g * n_exp + ei) * 6
            pg = C.bank()
            P.op("dve", lambda e: e.tensor_copy(out=selt[:], in_=C.identf[0:NE, ei:ei + 1].to_broadcast([NE, 128])),
                 reads=[C.identf], writes=[selt])
            P.op("pe", lambda e: e.matmul(out=pg[:], lhsT=selt[:], rhs=G[:], start=True, stop=True),
                 reads=[selt, G], writes=[pg])
            P.op("act", lambda e: e.copy(out=Gbc[:], in_=pg[:]), reads=[pg], writes=[Gbc])
            for m in range(8):
                ms = slice((m % 4) * 128, (m % 4 + 1) * 128)
                if m % 4 == 0:
                    ensure(ci + 2 * (m // 4) + 7)
                wa, wb = wslot[(ci + 2 * (m // 4)) % NS], wslot[(ci + 2 * (m // 4) + 1) % NS]
                pgl = C.bank()
                for k in range(8):
                    P.op("pe", lambda e: e.matmul(out=pgl[:], lhsT=wa[:, k, ms], rhs=xnT[:, k, :],
                                                  start=(k == 0), stop=(k == 7)),
                         reads=[wa, xnT], writes=[pgl])
                pli = C.bank()
                for k in range(8):
                    P.op("pe", lambda e: e.matmul(out=pli[:], lhsT=wb[:, k, ms], rhs=xnT[:, k, :],
                                                  start=(k == 0), stop=(k == 7)),
                         reads=[wb, xnT], writes=[pli])
                g1, sg, l1 = tmp(), tmp(), tmp()
                P.op("dve", lambda e: e.tensor_scalar(out=g1[:], in0=pgl[:], scalar1=bfm[:, m, ei:ei + 1], scalar2=7.0,
                                                      op0=ALU.add, op1=ALU.min), reads=[pgl, bfm], writes=[g1])
                P.op("act", lambda e: e.activation(out=sg[:], in_=g1[:], func=AF.Sigmoid, scale=1.702),
                     reads=[g1], writes=[sg])
                P.op("dve", lambda e: e.tensor_scalar(out=l1[:], in0=pli[:], scalar1=bfm[:, 8 + m, ei:ei + 1], scalar2=7.0,
                                                      op0=ALU.add, op1=ALU.min), reads=[pli, bfm], writes=[l1])
                P.op("dve", lambda e: e.tensor_scalar(out=l1[:], in0=l1[:], scalar1=-7.0, scalar2=1.0,
                                                      op0=ALU.max, op1=ALU.add), reads=[l1], writes=[l1])
                P.op("pool", lambda e: e.tensor_tensor(out=g1[:], in0=g1[:], in1=sg[:], op=ALU.mult),
                     reads=[g1, sg], writes=[g1])
                P.op("pool", lambda e: e.tensor_tensor(out=actT[:, m, :], in0=g1[:], in1=l1[:], op=ALU.mult),
                     reads=[g1, l1], writes=[(actT, m)])
            for m in range(8):
                ms = slice((m % 4) * 128, (m % 4 + 1) * 128)
                if m % 4 == 0:
                    ensure(ci + 4 + (m // 4) + 7)
                wd = wslot[(ci + 4 + m // 4) % NS]
                py = C.bank()
                for k in range(8):
                    P.op("pe", lambda e: e.matmul(out=py[:], lhsT=wd[:, k, ms], rhs=actT[:, k, :],
                                                  start=(k == 0), stop=(k == 7)),
                         reads=[wd, (actT, k)], writes=[py])
                if ei == 0:
                    P.op("dve", lambda e: e.scalar_tensor_tensor(out=yacc[:, m, :], in0=py[:], scalar=bfm[:, 16 + m, ei:ei + 1],
                                                                 in1=Gbc[:], op0=ALU.add, op1=ALU.mult),
                         reads=[py, bfm, Gbc], writes=[(yacc, m)])
                else:
                    t1 = tmp()
                    P.op("dve", lambda e: e.scalar_tensor_tensor(out=t1[:], in0=py[:], scalar=bfm[:, 16 + m, ei:ei + 1],
                                                                 in1=Gbc[:], op0=ALU.add, op1=ALU.mult),
                         reads=[py, bfm, Gbc], writes=[t1])
                    P.op("pool", lambda e: e.tensor_tensor(out=yacc[:, m, :], in0=yacc[:, m, :], in1=t1[:], op=ALU.add),
                         reads=[(yacc, m), t1], writes=[(yacc, m)])

        if stop == "B":
            return None
        for j in range(JT):
            r0 = t0 + j * 128
            js = slice(j * 128, (j + 1) * 128)
            pb_ = pbuf[j % 2]
            P.dma("sp", pb_[:], p_in[r0:r0 + 128, :], psem[j % 2], writes=[pb_])
            for m in (range(8) if not SPARSE else []):
                tr = C.trp[m // 4]
                P.op("pe", lambda e: e.transpose(out=tr[:, (m % 4) * 128:(m % 4 + 1) * 128],
                                                 in_=yacc[:, m, js], identity=C.identf[:]),
                     reads=[(yacc, m), C.identf], writes=[(tr, m % 4)])
            h2 = big[0]
            for hh in range(2):
                if SPARSE:
                    P.op("pool", lambda e: e.tensor_tensor(out=h2[:, hh * 512:(hh + 1) * 512], in0=yacc_tm[:, j, hh * 512:(hh + 1) * 512],
                                                           in1=h1[:, j, hh * 512:(hh + 1) * 512], op=ALU.add),
                         reads=[(yacc_tm, (j, hh)), (h1, j)], writes=[(h2, hh)])
                    continue
                P.op("dve", lambda e: e.tensor_tensor(out=h2[:, hh * 512:(hh + 1) * 512], in0=C.trp[hh][:],
                                                      in1=h1[:, j, hh * 512:(hh + 1) * 512], op=ALU.add),
                     reads=[C.trp[hh], (h1, j)], writes=[(h2, hh)])
            for k in range(2):
                P.op("pe", lambda e: e.transpose(out=C.trp[0][:, k * 128:(k + 1) * 128], in_=pb_[:, k * 128:(k + 1) * 128],
                                                 identity=C.identf[:]), reads=[pb_, C.identf], writes=[(C.trp[0], k)])
            P.op("act", lambda e: e.copy(out=pT[:], in_=C.trp[0][:, 0:256].rearrange("p (k t) -> p k t", k=2)),
                 reads=[C.trp[0]], writes=[pT])
            pA = [C.bank(), C.bank()]
            for hh in range(2):
                for k in range(2):
                    P.op("pe", lambda e: e.matmul(out=pA[hh][:], lhsT=pT[:, k, :], rhs=plew[:, k, hh * 512:(hh + 1) * 512],
                                                  start=(k == 0), stop=(k == 1)),
                         reads=[pT, plew], writes=[pA[hh]])
            ssa = C.stat("ssa", 2)
            for hh in range(2):
                P.op("act", lambda e: e.activation(out=C.junk[:, hh * 512:(hh + 1) * 512], in_=pA[hh][:], func=AF.Square,
                                                   accum_out=ssa[:, hh:hh + 1]),
                     reads=[pA[hh]], writes=[(C.junk, hh), (ssa, hh)])
            ssa1 = C.stat("ssa1")
            P.op("dve", lambda e: e.tensor_tensor(out=ssa1[:], in0=ssa[:, 0:1], in1=ssa[:, 1:2], op=ALU.add),
                 reads=[ssa], writes=[ssa1])
            rstda = C.stat("rstda")
            emit_rstd(P, C, (ssa1, ssa1[:]), (rstda, rstda[:]), D)
            An = big[1]
            for hh in range(2):
                hs = slice(hh * 512, (hh + 1) * 512)
                P.op("dve", lambda e: e.scalar_tensor_tensor(out=An[:, hs], in0=pA[hh][:], scalar=rstda[:, 0:1],
                                                             in1=g_ple[:, hs], op0=ALU.mult, op1=ALU.mult),
                     reads=[pA[hh], rstda, g_ple], writes=[(An, hh)])
            ss2 = C.stat("ss2")
            rstd2 = C.stat("rstd2")
            P.op("act", lambda e: e.activation(out=C.junk[:], in_=h2[:], func=AF.Square, accum_out=ss2[:]),
                 reads=[h2], writes=[C.junk, ss2])
            emit_rstd(P, C, (ss2, ss2[:]), (rstd2, rstd2[:]), D)
            P.op("dve", lambda e: e.scalar_tensor_tensor(out=xnb[:], in0=h2[:], scalar=rstd2[:, 0:1], in1=g_gate[:],
                                                         op0=ALU.mult, op1=ALU.mult),
                 reads=[h2, rstd2, g_gate], writes=[xnb])
            for k in range(8):
                P.op("pe", lambda e: e.transpose(out=C.trb[:, k * 128:(k + 1) * 128], in_=xnb[:, k * 128:(k + 1) * 128],
                                                 identity=C.identb[:]), reads=[xnb, C.identb], writes=[(C.trb, k)])
            P.op("act", lambda e: e.copy(out=hnT[:], in_=C.trb[:].rearrange("p (k t) -> p k t", k=8)),
                 reads=[C.trb], writes=[hnT])
            pB = [C.bank(), C.bank()]
            for hh in range(2):
                for k in range(8):
                    P.op("pe", lambda e: e.matmul(out=pB[hh][:], lhsT=hnT[:, k, :], rhs=plegw[:, k, hh * 512:(hh + 1) * 512],
                                                  start=(k == 0), stop=(k == 7)),
                         reads=[hnT, plegw], writes=[pB[hh]])
            sB = big[2]
            ot = otile[0]
            for hh in range(2):
                hs = slice(hh * 512, (hh + 1) * 512)
                P.op("act", lambda e: e.activation(out=sB[:, hs], in_=pB[hh][:], func=AF.Sigmoid),
                     reads=[pB[hh]], writes=[(sB, hh)])
                P.op("pool", lambda e: e.tensor_tensor(out=sB[:, hs], in0=sB[:, hs], in1=An[:, hs], op=ALU.mult),
                     reads=[(sB, hh), (An, hh)], writes=[(sB, hh)])
                P.op("pool", lambda e: e.tensor_tensor(out=ot[:, hs], in0=sB[:, hs], in1=h2[:, hs], op=ALU.add),
                     reads=[(sB, hh), (h2, hh)], writes=[(ot, hh)])
            P.dma("sp", h_out[r0:r0 + 128, :], ot[:], s_out, reads=[ot])
    return None


def build_ffn(NT, n_part=2, n_exp=NE, stop=None):
    P = Prog()
    C = Ctx(P)
    din = lambda n, s: P.dram(n, s, F32, "ExternalInput")
    h_in = din("h_in", [NT, D])
    parts = [din(f"part{i}", [NT, D]) for i in range(n_part)]
    p_in = din("p_in", [NT, 256])
    a = {}
    for n, shp in (("norm_ffn", [D]), ("router_w", [D, NE]), ("router_b", [NE]), ("w_gu", [n_exp, D, 2 * D]), ("b_gu", [NE, 2 * D]),
                   ("w_dn", [n_exp, D, D]), ("b_dn", [NE, D]), ("ple_w", [256, D]), ("ple_norm", [D]), ("ple_gate_norm", [D]),
                   ("ple_gate_w", [D, D])):
        a[n] = din(n, shp).ap()
    h_out = P.dram("h_out", [NT, D], F32, "ExternalOutput")
    emit_ffn(P, C, NT, h_in.ap(), [x.ap() for x in parts], p_in.ap(), a["norm_ffn"], a["router_w"], a["router_b"], a["w_gu"], a["b_gu"],
             a["w_dn"], a["b_dn"], a["ple_w"], a["ple_norm"], a["ple_gate_norm"], a["ple_gate_w"], h_out.ap(), n_exp=n_exp, stop=stop)
    print("ffn program: n_inst", P.n_inst)
    return P.finish(P.dsems)


class Phase:
    def __init__(self, P):
        self.P = P

    def __enter__(self):
        self.saved = self.P.es
        self.P.es = ExitStack()
        return self

    def __exit__(self, *a):
        P = self.P
        P.barrier()
        P.es.close()
        P.es = self.saved
        return False


def _barrier(self):
    sems = [E.sem for E in self.engs.values()] + list(self.dsems)
    for E in self.engs.values():
        for s in sems:
            if s is E.sem:
                continue
            if s.issued > 0 and E.known.get(s, 0) < s.issued:
                E.e.wait_ge(s.h, s.issued)
                E.known[s] = s.issued
                if s.is_dma and s.issued > s.last_wait:
                    s.last_wait = s.issued


Prog.barrier = _barrier
_old_dsem = Prog.dsem


def _dsem(self, name):
    if not hasattr(self, "dsem_by_name"):
        self.dsem_by_name = {}
    if name in self.dsem_by_name:
        return self.dsem_by_name[name]
    s = self.dsem_by_name[name] = Sem(self.gstack.enter_context(self.nc.semaphore(name)), name, is_dma=True)
    self.dsems.append(s)
    return s


Prog.dsem = _dsem


class NormT:
    def __init__(self, P, C, tag):
        self.P, self.C = P, C
        self.hb = [P.sb(f"nt_hb{i}_{tag}", [128, D], F32) for i in range(2)]
        self.sem = [P.dsem(f"d_nt{i}_{tag}") for i in range(2)]
        self.xnb = P.sb(f"nt_xnb_{tag}", [128, D], BF16)
        self.i = 0

    def emit(self, src_ap, src_deps, gain, dst, dst_ap, dst_key):
        P, C = self.P, self.C
        hb, sem = self.hb[self.i % 2], self.sem[self.i % 2]
        self.i += 1
        P.dma("sp", hb[:], src_ap, sem, reads=src_deps, writes=[hb])
        ss, rstd = C.stat("ss"), C.stat("rstd")
        P.op("act", lambda e: e.activation(out=C.junk[:], in_=hb[:], func=AF.Square, accum_out=ss[:]),
             reads=[hb], writes=[C.junk, ss])
        emit_rstd(P, C, (ss, ss[:]), (rstd, rstd[:]), D)
        P.op("dve", lambda e: e.scalar_tensor_tensor(out=self.xnb[:], in0=hb[:], scalar=rstd[:, 0:1], in1=gain[:],
                                                     op0=ALU.mult, op1=ALU.mult),
             reads=[hb, rstd, gain], writes=[self.xnb])
        for k in range(8):
            P.op("pe", lambda e: e.transpose(out=C.trb[:, k * 128:(k + 1) * 128], in_=self.xnb[:, k * 128:(k + 1) * 128],
                                             identity=C.identb[:]), reads=[self.xnb, C.identb], writes=[C.trb])
        P.op("act", lambda e: e.copy(out=dst_ap, in_=C.trb[:].rearrange("p (k t) -> p k t", k=8)),
             reads=[C.trb], writes=[(dst, dst_key)])


def emit_outproj(P, C, yT, Wout, ntile, dst_rows_fn, dst_tile, osem, obufs, cnt):
    for j in range(ntile):
        ot = obufs[cnt[0] % len(obufs)]
        cnt[0] += 1
        for hh in range(2):
            po = C.bank()
            for k in range(8):
                P.op("pe", lambda e: e.matmul(out=po[:], lhsT=yT[:, k, j * 128:(j + 1) * 128],
                                              rhs=Wout[:, k, hh * 512:(hh + 1) * 512], start=(k == 0), stop=(k == 7)),
                     reads=[yT, Wout], writes=[po])
            if hh == 0:
                P.op("act", lambda e: e.copy(out=ot[:, 0:512], in_=po[:]), reads=[po], writes=[(ot, 0)])
            else:
                P.op("dve", lambda e: e.tensor_copy(out=ot[:, 512:1024], in_=po[:]), reads=[po], writes=[(ot, 1)])
        P.dma("sp", dst_rows_fn(j), ot[:], osem, reads=[ot], writes=[dst_tile] if dst_tile is not None else None)


def emit_hgrn2(P, C, S, layer, h_src, h_deps, mix_dst, mix_tile, g_mix_ap, w_in, w_out, gnorm_ap, lb_param):
    ST = 512
    with Phase(P):
        s_w = P.dsem("d_hgw")
        s_c = P.dsem("d_hgc")
        s_o = P.dsem("d_hgo")
        Win = P.sb("hg_win", [128, 8, 4 * D], BF16)
        for q in range(4):
            load_w_bf16(P, Win, Win[:, :, q * D:(q + 1) * D], w_in[:, q * D:(q + 1) * D], s_w, key=q)
        Wout = P.sb("hg_wout", [128, 8, D], BF16)
        load_w_bf16(P, Wout, Wout[:], w_out, s_w)
        g_mix = bc_load(P, "sp", "hg_gmix", g_mix_ap, D, s_c)
        gn = P.sb("hg_gn", [128, 1], F32)
        P.dma("sp", gn[:], gnorm_ap.rearrange("(p o) -> p o", o=1), s_c, writes=[gn])
        lbrow = P.sb("hg_lbrow", [32, 128], F32)
        P.dma("sp", lbrow[:], lb_param.rearrange("l (h p) -> (l h) p", p=128), s_c, writes=[lbrow])
        P.op("pe", lambda e: e.transpose(out=C.small[:, 0:32], in_=lbrow[:], identity=C.identf[0:32, 0:32]),
             reads=[lbrow, C.identf], writes=[C.small])
        el = P.sb("hg_el", [128, 4, 8], F32)
        P.op("act", lambda e: e.activation(out=el[:], in_=C.small[:, 0:32].rearrange("p (l h) -> p l h", l=4), func=AF.Exp),
             reads=[C.small], writes=[el])
        den = P.sb("hg_den", [128, 8], F32)
        num = P.sb("hg_num", [128, 8], F32)
        P.op("dve", lambda e: e.tensor_tensor(out=den[:], in0=el[:, 0, :], in1=el[:, 1, :], op=ALU.add), reads=[el], writes=[den])
        P.op("dve", lambda e: e.tensor_tensor(out=den[:], in0=den[:], in1=el[:, 2, :], op=ALU.add), reads=[el, den], writes=[den])
        P.op("dve", lambda e: e.tensor_tensor(out=den[:], in0=den[:], in1=el[:, 3, :], op=ALU.add), reads=[el, den], writes=[den])
        P.op("dve", lambda e: e.memset(num[:], 0.0), writes=[num])
        for l in range(1, layer + 1):
            P.op("dve", lambda e: e.tensor_tensor(out=num[:], in0=num[:], in1=el[:, l, :], op=ALU.add), reads=[el, num], writes=[num])
        P.op("dve", lambda e: e.reciprocal(out=den[:], in_=den[:]), reads=[den], writes=[den])
        lb = P.sb("hg_lb", [128, 8], F32)
        oml = P.sb("hg_oml", [128, 8], F32)
        noml = P.sb("hg_noml", [128, 8], F32)
        P.op("dve", lambda e: e.tensor_tensor(out=lb[:], in0=num[:], in1=den[:], op=ALU.mult), reads=[num, den], writes=[lb])
        P.op("dve", lambda e: e.tensor_scalar(out=oml[:], in0=lb[:], scalar1=-1.0, scalar2=1.0, op0=ALU.mult, op1=ALU.add),
             reads=[lb], writes=[oml])
        P.op("dve", lambda e: e.tensor_scalar(out=noml[:], in0=oml[:], scalar1=-1.0, scalar2=None, op0=ALU.mult),
             reads=[oml], writes=[noml])
        mask01 = P.sb("hg_mask01", [128, ST], F32)
        P.op("pool", lambda e: e.memset(mask01[:], 1.0), writes=[mask01])
        for c in range(ST // 64):
            P.op("pool", lambda e: e.memset(mask01[:, c * 64:c * 64 + 1], 0.0), reads=[mask01], writes=[mask01])
        mc = P.sb("hg_mc", [64, ST], F32)
        P.op("pool", lambda e: e.memset(mc[:], 1.0), writes=[mc])
        P.op("pool", lambda e: e.affine_select(out=mc[:], in_=mc[:], pattern=[[0, ST // 64], [1, 64]], compare_op=ALU.is_ge,
                                               fill=0.0, base=0, channel_multiplier=-1), reads=[mc], writes=[mc])
        onesf = P.sb("hg_ones", [128, 128], F32)
        P.op("pool", lambda e: e.memset(onesf[:], 1.0), writes=[onesf])
        Sf = P.sb("hg_Sf", [128, 8, 128], F32)
        Sb = P.sb("hg_Sb", [128, 8, 128], BF16)
        P.op("pool", lambda e: e.memset(Sf[:], 0.0), writes=[Sf])
        P.op("pool", lambda e: e.memset(Sb[:], 0.0), writes=[Sb])
        Tt = P.sb("hg_T", [128, 128], F32)
        xnT = P.sb("hg_xnT", [128, 8, ST], BF16)
        v_sb = P.sb("hg_v", [64, ST // 64, D], BF16)
        f32t = {n: P.sb("hg_" + n, [128, ST], F32) for n in ["sg", "lf", "cum", "A", "Ai", "qs", "kk", "sgate", "sq", "rr"]}
        qt = P.sb("hg_qt", [128, ST], BF16)
        kt = P.sb("hg_kt", [128, ST], BF16)
        ktm = P.sb("hg_ktm", [64, ST // 64, 128], BF16)
        scm = P.sb("hg_scm", [64, ST], BF16)
        ogT = P.sb("hg_ogT", [128, 8, ST], BF16)
        obufs = [P.sb(f"hg_ob{i}", [128, D], F32) for i in range(2)]
        ocnt = [0]
        nt = NormT(P, C, "hg")
        oT, dS, ssps = C.trp[0], C.trp[1], C.small
        NCH = ST // 64
        for st in range(S // ST):
            t0 = st * ST
            for j in range(ST // 128):
                nt.emit(h_src[t0 + j * 128:t0 + (j + 1) * 128, :], h_deps, g_mix, xnT, xnT[:, :, j * 128:(j + 1) * 128], j)
            for c in range(NCH):
                for hh in range(2):
                    pv = C.bank()
                    for k in range(8):
                        P.op("pe", lambda e: e.matmul(out=pv[0:64, :], lhsT=xnT[:, k, c * 64:(c + 1) * 64],
                                                      rhs=Win[:, k, 2 * D + hh * 512:2 * D + (hh + 1) * 512],
                                                      start=(k == 0), stop=(k == 7)), reads=[xnT, (Win, 2)], writes=[pv])
                    if hh == 0:
                        P.op("act", lambda e: e.copy(out=v_sb[:, c, 0:512], in_=pv[0:64, :]), reads=[pv], writes=[(v_sb, (c, 0))])
                    else:
                        P.op("dve", lambda e: e.tensor_copy(out=v_sb[:, c, 512:1024], in_=pv[0:64, :]), reads=[pv],
                             writes=[(v_sb, (c, 1))])
            for H in range(8):
                Hs = slice(H * 128, (H + 1) * 128)
                pq, pz, pg = C.bank(), C.bank(), C.bank()
                for (pp, off, wk) in ((pq, 0, 0), (pz, D, 1), (pg, 3 * D, 3)):
                    for k in range(8):
                        P.op("pe", lambda e: e.matmul(out=pp[:], lhsT=Win[:, k, off + H * 128:off + (H + 1) * 128], rhs=xnT[:, k, :],
                                                      start=(k == 0), stop=(k == 7)), reads=[xnT, (Win, wk)], writes=[pp])
                T = f32t
                P.op("act", lambda e: e.activation(out=T["sg"][:], in_=pz[:], func=AF.Sigmoid), reads=[pz], writes=[T["sg"]])
                P.op("act", lambda e: e.activation(out=T["qs"][:], in_=pq[:], func=AF.Silu), reads=[pq], writes=[T["qs"]])
                P.op("act", lambda e: e.activation(out=T["sgate"][:], in_=pg[:], func=AF.Silu), reads=[pg], writes=[T["sgate"]])
                P.op("dve", lambda e: e.tensor_scalar(out=T["lf"][:], in0=T["sg"][:], scalar1=oml[:, H:H + 1], scalar2=lb[:, H:H + 1],
                                                      op0=ALU.mult, op1=ALU.add), reads=[T["sg"], oml, lb], writes=[T["lf"]])
                P.op("act", lambda e: e.activation(out=T["lf"][:], in_=T["lf"][:], func=AF.Ln), reads=[T["lf"]], writes=[T["lf"]])
                P.op("dve", lambda e: e.tensor_tensor_scan(out=T["cum"][:], data0=mask01[:], data1=T["lf"][:], initial=0.0,
                                                           op0=ALU.mult, op1=ALU.add), reads=[mask01, T["lf"]], writes=[T["cum"]])
                P.op("act", lambda e: e.activation(out=T["A"][:], in_=T["cum"][:], func=AF.Exp), reads=[T["cum"]], writes=[T["A"]])
                P.op("act", lambda e: e.activation(out=T["Ai"][:], in_=T["cum"][:], func=AF.Exp, scale=-1.0),
                     reads=[T["cum"]], writes=[T["Ai"]])
                P.op("dve", lambda e: e.scalar_tensor_tensor(out=qt[:], in0=T["qs"][:], scalar=128.0 ** -0.5, in1=T["A"][:],
                                                             op0=ALU.mult, op1=ALU.mult), reads=[T["qs"], T["A"]], writes=[qt])
                P.op("dve", lambda e: e.tensor_scalar(out=T["kk"][:], in0=T["sg"][:], scalar1=noml[:, H:H + 1], scalar2=oml[:, H:H + 1],
                                                      op0=ALU.mult, op1=ALU.add), reads=[T["sg"], noml, oml], writes=[T["kk"]])
                P.op("pool", lambda e: e.tensor_tensor(out=kt[:], in0=T["kk"][:], in1=T["Ai"][:], op=ALU.mult),
                     reads=[T["kk"], T["Ai"]], writes=[kt])
                for c in range(NCH):
                    P.op("pe", lambda e: e.transpose(out=C.trb[0:64, c * 128:(c + 1) * 128], in_=kt[:, c * 64:(c + 1) * 64],
                                                     identity=C.identb[:]), reads=[kt, C.identb], writes=[C.trb])
                P.op("act", lambda e: e.copy(out=ktm[:], in_=C.trb[0:64, :].rearrange("p (c d) -> p c d", c=NCH)),
                     reads=[C.trb], writes=[ktm])
                sc = C.bank()
                for c in range(NCH):
                    cs = slice(c * 64, (c + 1) * 64)
                    P.op("pe", lambda e: e.matmul(out=sc[0:64, cs], lhsT=kt[:, cs], rhs=qt[:, cs], start=True, stop=True),
                         reads=[kt, qt], writes=[sc])
                P.op("dve", lambda e: e.tensor_tensor(out=scm[:], in0=sc[0:64, :], in1=mc[:], op=ALU.mult),
                     reads=[sc, mc], writes=[scm])
                for c in range(NCH):
                    cs = slice(c * 64, (c + 1) * 64)
                    P.op("pe", lambda e: e.matmul(out=oT[:, cs], lhsT=v_sb[:, c, Hs], rhs=scm[:, cs], start=True, stop=False),
                         reads=[v_sb, scm], writes=[oT])
                    P.op("pe", lambda e: e.matmul(out=oT[:, cs], lhsT=Sb[:, H, :], rhs=qt[:, cs], start=False, stop=True),
                         reads=[(Sb, H), qt], writes=[oT])
                    P.op("pe", lambda e: e.matmul(out=dS[:, 0:128], lhsT=ktm[:, c, :], rhs=v_sb[:, c, Hs], start=True, stop=True),
                         reads=[ktm, v_sb], writes=[dS])
                    acol = T["A"][:, c * 64 + 63:c * 64 + 64]
                    P.op("dve", lambda e: e.tensor_tensor(out=Tt[:], in0=dS[:, 0:128], in1=Sf[:, H, :], op=ALU.add),
                         reads=[dS, (Sf, H)], writes=[Tt])
                    P.op("dve", lambda e: e.tensor_scalar(out=Sf[:, H, :], in0=Tt[:], scalar1=acol, scalar2=None, op0=ALU.mult),
                         reads=[Tt, T["A"]], writes=[(Sf, H)])
                    P.op("act", lambda e: e.activation(out=Sb[:, H, :], in_=Tt[:], func=AF.Copy, scale=acol),
                         reads=[Tt, T["A"]], writes=[(Sb, H)])
                P.op("act", lambda e: e.activation(out=T["sq"][:], in_=oT[:], func=AF.Square), reads=[oT], writes=[T["sq"]])
                P.op("pe", lambda e: e.matmul(out=ssps[:], lhsT=onesf[:], rhs=T["sq"][:], start=True, stop=True),
                     reads=[onesf, T["sq"]], writes=[ssps])
                emit_rstd(P, C, (ssps, ssps[:]), (T["rr"], T["rr"][:]), 128)
                P.op("dve", lambda e: e.tensor_tensor(out=T["sq"][:], in0=oT[:], in1=T["rr"][:], op=ALU.mult),
                     reads=[oT, T["rr"]], writes=[T["sq"]])
                P.op("dve", lambda e: e.scalar_tensor_tensor(out=ogT[:, H, :], in0=T["sq"][:], scalar=gn[:, 0:1], in1=T["sgate"][:],
                                                             op0=ALU.mult, op1=ALU.mult),
                     reads=[T["sq"], gn, T["sgate"]], writes=[(ogT, H)])
            emit_outproj(P, C, ogT, Wout, ST // 128, lambda j: mix_dst[t0 + j * 128:t0 + (j + 1) * 128, :], mix_tile, s_o,
                         obufs, ocnt)


def emit_rglru(P, C, S, h_src, h_deps, mix_dst, mix_tile, g_mix_ap, w_in, conv_w, conv_b, w_a, b_a, w_x, b_x, lam, w_out):
    ST = 512
    with Phase(P):
        s_w = P.dsem("d_rgw")
        s_c = P.dsem("d_rgc")
        s_o = P.dsem("d_rgo")
        Win = P.sb("rg_win", [128, 8, 2 * D], BF16)
        for q in range(2):
            load_w_bf16(P, Win, Win[:, :, q * D:(q + 1) * D], w_in[:, q * D:(q + 1) * D], s_w, key=q)
        Wout = P.sb("rg_wout", [128, 8, D], BF16)
        load_w_bf16(P, Wout, Wout[:], w_out, s_w)
        wa = P.sb("rg_wa_sb", [128, 8, 256], BF16)
        wx = P.sb("rg_wx_sb", [128, 8, 256], BF16)
        P.dma("pool", wa[:], w_a.rearrange("n (dh p) e -> p (n dh) e", p=128), s_w, writes=[wa])
        P.dma("pool", wx[:], w_x.rearrange("n (dh p) e -> p (n dh) e", p=128), s_w, writes=[wx])
        g_mix = bc_load(P, "sp", "rg_gmix", g_mix_ap, D, s_c)
        vrow = P.sb("rg_vrow", [64, 128], F32)
        P.dma("sp", vrow[0:32, :], conv_w.rearrange("t (k p) -> (t k) p", p=128), s_c, writes=[vrow])
        for i, v in enumerate((conv_b, b_a, b_x, lam)):
            P.dma("sp", vrow[32 + 8 * i:40 + 8 * i, :], v.rearrange("(k p) -> k p", p=128), s_c, writes=[vrow])
        P.op("pe", lambda e: e.transpose(out=C.small[:, 0:64], in_=vrow[:], identity=C.identf[0:64, 0:64]),
             reads=[vrow, C.identf], writes=[C.small])
        vT = P.sb("rg_vT", [128, 64], F32)
        P.op("dve", lambda e: e.tensor_copy(out=vT[:], in_=C.small[:, 0:64]), reads=[C.small], writes=[vT])
        cw = lambda t, k: vT[:, t * 8 + k:t * 8 + k + 1]
        cb = lambda k: vT[:, 32 + k:33 + k]
        ba = lambda k: vT[:, 40 + k:41 + k]
        bx = lambda k: vT[:, 48 + k:49 + k]
        cl = P.sb("rg_cl", [128, 8], F32)
        P.op("act", lambda e: e.activation(out=cl[:], in_=vT[:, 56:64], func=AF.Exp, scale=-1.0), reads=[vT], writes=[cl])
        P.op("act", lambda e: e.activation(out=cl[:], in_=cl[:], func=AF.Ln, bias=1.0), reads=[cl], writes=[cl])
        P.op("dve", lambda e: e.tensor_scalar(out=cl[:], in0=cl[:], scalar1=-8.0, scalar2=None, op0=ALU.mult),
             reads=[cl], writes=[cl])
        xnT = P.sb("rg_xnT", [128, 8, ST], BF16)
        gate = P.sb("rg_gate", [128, 8, ST], F32)
        ubuf = [P.sb(f"rg_ubuf{i}", [128, 8, ST + 3], F32) for i in range(2)]
        P.op("pool", lambda e: e.memset(ubuf[1][:], 0.0), writes=[ubuf[1]])
        ucf = P.sb("rg_ucf", [128, 8, ST], F32)
        ucb = P.sb("rg_ucb", [128, 8, ST], BF16)
        tt = [P.sb(f"rg_t{i}", [128, ST], F32) for i in range(6)]
        ti = [0]

        def tmp():
            t = tt[ti[0] % len(tt)]
            ti[0] += 1
            return t
        hlast = P.sb("rg_hlast", [128, 8], F32)
        P.op("pool", lambda e: e.memset(hlast[:], 0.0), writes=[hlast])
        yT = P.sb("rg_yT", [128, 8, ST], BF16)
        obufs = [P.sb(f"rg_ob{i}", [128, D], F32) for i in range(2)]
        ocnt = [0]
        nt = NormT(P, C, "rg")
        for st in range(S // ST):
            t0 = st * ST
            ub, ubp = ubuf[st % 2], ubuf[(st + 1) % 2]
            for j in range(ST // 128):
                nt.emit(h_src[t0 + j * 128:t0 + (j + 1) * 128, :], h_deps, g_mix, xnT, xnT[:, :, j * 128:(j + 1) * 128], j)
            for kc in range(8):
                ks = slice(kc * 128, (kc + 1) * 128)
                pg, pu = C.bank(), C.bank()
                for k in range(8):
                    P.op("pe", lambda e: e.matmul(out=pg[:], lhsT=Win[:, k, ks], rhs=xnT[:, k, :], start=(k == 0), stop=(k == 7)),
                         reads=[xnT, (Win, 0)], writes=[pg])
                for k in range(8):
                    P.op("pe", lambda e: e.matmul(out=pu[:], lhsT=Win[:, k, D + kc * 128:D + (kc + 1) * 128], rhs=xnT[:, k, :],
                                                  start=(k == 0), stop=(k == 7)), reads=[xnT, (Win, 1)], writes=[pu])
                t1, t2 = tmp(), tmp()
                P.op("act", lambda e: e.activation(out=t1[:], in_=pg[:], func=AF.Square), reads=[pg], writes=[t1])
                P.op("dve", lambda e: e.tensor_scalar(out=t1[:], in0=t1[:], scalar1=0.044715, scalar2=1.0, op0=ALU.mult, op1=ALU.add),
                     reads=[t1], writes=[t1])
                P.op("dve", lambda e: e.tensor_tensor(out=t1[:], in0=t1[:], in1=pg[:], op=ALU.mult), reads=[t1, pg], writes=[t1])
                P.op("act", lambda e: e.activation(out=t2[:], in_=t1[:], func=AF.Sigmoid, scale=1.5957691216),
                     reads=[t1], writes=[t2])
                P.op("dve", lambda e: e.tensor_tensor(out=gate[:, kc, :], in0=t2[:], in1=pg[:], op=ALU.mult),
                     reads=[t2, pg], writes=[(gate, kc)])
                P.op("pool", lambda e: e.tensor_copy(out=ub[:, kc, 0:3], in_=ubp[:, kc, ST:ST + 3]),
                     reads=[(ubp, kc)], writes=[(ub, kc)])
                P.op("act", lambda e: e.copy(out=ub[:, kc, 3:ST + 3], in_=pu[:]), reads=[pu], writes=[(ub, kc)])
                P.op("dve", lambda e: e.tensor_scalar(out=ucf[:, kc, :], in0=ub[:, kc, 0:ST], scalar1=cw(0, kc), scalar2=cb(kc),
                                                      op0=ALU.mult, op1=ALU.add), reads=[(ub, kc), vT], writes=[(ucf, kc)])
                for tap in range(1, 4):
                    P.op("dve", lambda e: e.scalar_tensor_tensor(out=ucf[:, kc, :], in0=ub[:, kc, tap:tap + ST], scalar=cw(tap, kc),
                                                                 in1=ucf[:, kc, :], op0=ALU.mult, op1=ALU.add),
                         reads=[(ub, kc), vT, (ucf, kc)], writes=[(ucf, kc)])
                P.op("pool", lambda e: e.tensor_copy(out=ucb[:, kc, :], in_=ucf[:, kc, :]), reads=[(ucf, kc)], writes=[(ucb, kc)])
            for oc in range(8):
                n, eh = oc // 2, oc % 2
                es_ = slice(eh * 128, (eh + 1) * 128)
                pr, pi = C.bank(), C.bank()
                for dh in range(2):
                    P.op("pe", lambda e: e.matmul(out=pr[:], lhsT=wa[:, 2 * n + dh, es_], rhs=ucb[:, 2 * n + dh, :],
                                                  start=(dh == 0), stop=(dh == 1)), reads=[wa, (ucb, 2 * n + dh)], writes=[pr])
                for dh in range(2):
                    P.op("pe", lambda e: e.matmul(out=pi[:], lhsT=wx[:, 2 * n + dh, es_], rhs=ucb[:, 2 * n + dh, :],
                                                  start=(dh == 0), stop=(dh == 1)), reads=[wx, (ucb, 2 * n + dh)], writes=[pi])
                r, ig, a, om = tmp(), tmp(), tmp(), tmp()
                P.op("act", lambda e: e.activation(out=r[:], in_=pr[:], func=AF.Sigmoid, bias=ba(oc)), reads=[pr, vT], writes=[r])
                P.op("act", lambda e: e.activation(out=ig[:], in_=pi[:], func=AF.Sigmoid, bias=bx(oc)), reads=[pi, vT], writes=[ig])
                P.op("act", lambda e: e.activation(out=a[:], in_=r[:], func=AF.Exp, scale=cl[:, oc:oc + 1]), reads=[r, cl], writes=[a])
                P.op("pool", lambda e: e.tensor_tensor(out=om[:], in0=a[:], in1=a[:], op=ALU.mult), reads=[a], writes=[om])
                P.op("dve", lambda e: e.tensor_scalar(out=om[:], in0=om[:], scalar1=-1.0, scalar2=1.0, op0=ALU.mult, op1=ALU.add),
                     reads=[om], writes=[om])
                P.op("act", lambda e: e.activation(out=om[:], in_=om[:], func=AF.Sqrt), reads=[om], writes=[om])
                P.op("pool", lambda e: e.tensor_tensor(out=ig[:], in0=ig[:], in1=ucf[:, oc, :], op=ALU.mult),
                     reads=[ig, (ucf, oc)], writes=[ig])
                P.op("dve", lambda e: e.tensor_tensor(out=ig[:], in0=ig[:], in1=om[:], op=ALU.mult), reads=[ig, om], writes=[ig])
                P.op("dve", lambda e: e.tensor_tensor_scan(out=r[:], data0=a[:], data1=ig[:], initial=hlast[:, oc:oc + 1],
                                                           op0=ALU.mult, op1=ALU.add), reads=[a, ig, hlast], writes=[r])
                P.op("pool", lambda e: e.tensor_copy(out=hlast[:, oc:oc + 1], in_=r[:, ST - 1:ST]), reads=[r], writes=[hlast])
                P.op("dve", lambda e: e.tensor_tensor(out=yT[:, oc, :], in0=r[:], in1=gate[:, oc, :], op=ALU.mult),
                     reads=[r, (gate, oc)], writes=[(yT, oc)])
            emit_outproj(P, C, yT, Wout, ST // 128, lambda j: mix_dst[t0 + j * 128:t0 + (j + 1) * 128, :], mix_tile, s_o,
                         obufs, ocnt)


def emit_fox(P, C, S, h_src, h_deps, mix_dst, mix_tile, g_mix_ap, w_in, f_bias, q_norm, k_norm, w_out):
    ST = 512
    NH, HD = 16, 64
    NT_ = S // 128
    NQ = S // ST
    nc = P.nc
    QTd = nc.dram_tensor("fox_QTd", [NH * HD, S], BF16, kind="Internal").ap()
    KTd = nc.dram_tensor("fox_KTd", [NH * HD, S], BF16, kind="Internal").ap()
    Vd = nc.dram_tensor("fox_Vd", [NH, 128, NT_ * 65], BF16, kind="Internal").ap()
    SGd = nc.dram_tensor("fox_SGd", [S, D], F32, kind="Internal").ap()
    Od = nc.dram_tensor("fox_Od", [S, D], F32, kind="Internal").ap()
    CUMd = nc.dram_tensor("fox_CUMd", [NH, S], F32, kind="Internal").ap()
    with Phase(P):
        s_w = P.dsem("d_fxw")
        s_c = P.dsem("d_fxc")
        s_o = [P.dsem(f"d_fxo{i}") for i in range(2)]
        Win = P.sb("fx_win", [128, 8, 4 * D + NH], BF16)
        for q in range(4):
            load_w_bf16(P, Win, Win[:, :, q * D:(q + 1) * D], w_in[:, q * D:(q + 1) * D], s_w, key=q)
        load_w_bf16(P, Win, Win[:, :, 4 * D:4 * D + NH], w_in[:, 4 * D:4 * D + NH], s_w, key=4)
        g_mix = bc_load(P, "sp", "fx_gmix", g_mix_ap, D, s_c)
        qn_bc = bc_load(P, "sp", "fx_qn", q_norm, HD, s_c)
        kn_bc = bc_load(P, "sp", "fx_kn", k_norm, HD, s_c)
        fb = P.sb("fx_fb", [NH, 1], F32)
        P.dma("sp", fb[:], f_bias.rearrange("(p o) -> p o", o=1), s_c, writes=[fb])
        ones16 = P.sb("fx_ones16", [NH, ST], F32)
        P.op("pool", lambda e: e.memset(ones16[:], 1.0), writes=[ones16])
        xnT = P.sb("fx_xnT", [128, 8, ST], BF16)
        cumst = [P.sb(f"fx_cumst{i}", [NH, ST], F32) for i in range(2)]
        P.op("pool", lambda e: e.memset(cumst[1][:], 0.0), writes=[cumst[1]])
        ls = P.sb("fx_ls", [NH, ST], F32)
        sq = P.sb("fx_sq", [128, ST], F32)
        qb = P.sb("fx_qb", [128, ST], BF16)
        ssq = P.sb("fx_ssq", [128, 8], F32)
        rs = P.sb("fx_rs", [128, 8], F32)
        Q2 = [P.sb(f"fx_Q2_{i}", [128, 8, 128], BF16) for i in range(2)]
        K2 = [P.sb(f"fx_K2_{i}", [128, 8, 128], BF16) for i in range(2)]
        vb = [P.sb(f"fx_vb{i}", [128, NH, 65], BF16) for i in range(2)]
        for i in range(2):
            P.op("pool", lambda e: e.memset(vb[i][:], 1.0), writes=[vb[i]])
        sgt = [P.sb(f"fx_sg{i}", [128, D], F32) for i in range(2)]
        nt = NormT(P, C, "fx")
        tix = 0
        for st in range(S // ST):
            t0 = st * ST
            for j in range(ST // 128):
                nt.emit(h_src[t0 + j * 128:t0 + (j + 1) * 128, :], h_deps, g_mix, xnT, xnT[:, :, j * 128:(j + 1) * 128], j)
            pf = C.bank()
            for k in range(8):
                P.op("pe", lambda e: e.matmul(out=pf[0:NH, :], lhsT=Win[:, k, 4 * D:4 * D + NH], rhs=xnT[:, k, :],
                                              start=(k == 0), stop=(k == 7)), reads=[xnT, (Win, 4)], writes=[pf])
            P.op("act", lambda e: e.activation(out=ls[:], in_=pf[0:NH, :], func=AF.Sigmoid, bias=fb[:, 0:1]),
                 reads=[pf, fb], writes=[ls])
            P.op("act", lambda e: e.activation(out=ls[:], in_=ls[:], func=AF.Ln), reads=[ls], writes=[ls])
            cs_, csp = cumst[st % 2], cumst[(st + 1) % 2]
            P.op("dve", lambda e: e.tensor_tensor_scan(out=cs_[:], data0=ones16[:], data1=ls[:], initial=csp[:, ST - 1:ST],
                                                       op0=ALU.mult, op1=ALU.add), reads=[ones16, ls, csp], writes=[cs_])
            P.dma("sp", CUMd[:, t0:t0 + ST], cs_[:], s_o[0], reads=[cs_])
            for j in range(ST // 128):
                r0 = t0 + j * 128
                tj = r0 // 128
                q2, k2, vbt, sg = Q2[tix % 2], K2[tix % 2], vb[tix % 2], sgt[tix % 2]
                osem = s_o[tix % 2]
                tix += 1
                for blk in range(8):
                    pp = C.bank()
                    for k in range(8):
                        P.op("pe", lambda e: e.matmul(out=pp[:], lhsT=xnT[:, k, j * 128:(j + 1) * 128],
                                                      rhs=Win[:, k, blk * 512:(blk + 1) * 512], start=(k == 0), stop=(k == 7)),
                             reads=[(xnT, j), (Win, blk // 2)], writes=[pp])
                    if blk < 4:
                        isq = blk < 2
                        P.op("act", lambda e: e.activation(out=sq[:], in_=pp[:], func=AF.Square), reads=[pp], writes=[sq])
                        P.op("dve", lambda e: e.tensor_reduce(out=ssq[:], in_=sq[:].rearrange("p (h d) -> p h d", d=HD), axis=AX.X,
                                                              op=ALU.add), reads=[sq], writes=[ssq])
                        emit_rstd(P, C, (ssq, ssq[:]), (rs, rs[:]), HD)
                        P.op("dve", lambda e: e.tensor_tensor(out=sq[:].rearrange("p (h d) -> p h d", d=HD),
                                                              in0=pp[:].rearrange("p (h d) -> p h d", d=HD),
                                                              in1=rs[:].unsqueeze(2).to_broadcast([128, 8, HD]), op=ALU.mult),
                             reads=[pp, rs], writes=[sq])
                        gbc = qn_bc if isq else kn_bc
                        P.op("dve", lambda e: e.scalar_tensor_tensor(out=qb[:].rearrange("p (h d) -> p h d", d=HD),
                                                                     in0=sq[:].rearrange("p (h d) -> p h d", d=HD),
                                                                     scalar=(1.0 if isq else HD ** -0.5),
                                                                     in1=gbc[:].unsqueeze(1).to_broadcast([128, 8, HD]),
                                                                     op0=ALU.mult, op1=ALU.mult), reads=[sq, gbc], writes=[qb])
                        for r in range(4):
                            P.op("pe", lambda e: e.transpose(out=C.trb[:, r * 128:(r + 1) * 128], in_=qb[:, r * 128:(r + 1) * 128],
                                                             identity=C.identb[:]), reads=[qb, C.identb], writes=[C.trb])
                        dst = q2 if isq else k2
                        P.op("act", lambda e: e.copy(out=dst[:, (blk % 2) * 4:(blk % 2) * 4 + 4, :],
                                                     in_=C.trb[:, 0:512].rearrange("p (r t) -> p r t", r=4)),
                             reads=[C.trb], writes=[(dst, blk % 2)])
                    elif blk < 6:
                        P.op("act", lambda e: e.copy(out=vbt[:, (blk - 4) * 8:(blk - 4) * 8 + 8, 0:HD],
                                                     in_=pp[:].rearrange("p (h d) -> p h d", d=HD)),
                             reads=[pp], writes=[(vbt, blk - 4)])
                    else:
                        P.op("act", lambda e: e.activation(out=sg[:, (blk - 6) * 512:(blk - 5) * 512], in_=pp[:], func=AF.Sigmoid),
                             reads=[pp], writes=[(sg, blk - 6)])
                P.dma("sp", QTd[:, r0:r0 + 128].rearrange("(pr p) t -> p pr t", p=128), q2[:], osem, reads=[q2])
                P.dma("sp", KTd[:, r0:r0 + 128].rearrange("(pr p) t -> p pr t", p=128), k2[:], osem, reads=[k2])
                P.dma("sp", Vd[:, :, tj * 65:(tj + 1) * 65].rearrange("h p c -> p h c"), vbt[:], osem, reads=[vbt])
                P.dma("sp", SGd[r0:r0 + 128, :], sg[:], osem, reads=[sg])
    with Phase(P):
        s_l = [P.dsem(f"d_fxl{i}") for i in range(2)]
        s_c2 = P.dsem("d_fxc2")
        s_o2 = P.dsem("d_fxo2")
        cumT = P.sb("fx_cumT", [NH, S], F32)
        P.dma("sp", cumT[:], CUMd, s_c2, writes=[cumT])
        cumtm = P.sb("fx_cumtm", [128, NT_, NH], F32)
        for t in range(NT_):
            P.op("pe", lambda e: e.transpose(out=C.small[:, 0:NH], in_=cumT[:, t * 128:(t + 1) * 128], identity=C.identf[0:NH, 0:NH]),
                 reads=[cumT, C.identf], writes=[C.small])
            P.op("dve", lambda e: e.tensor_copy(out=cumtm[:, t, :], in_=C.small[:, 0:NH]), reads=[C.small], writes=[(cumtm, t)])
        E0 = P.sb("fx_E0", [128, 128], F32)
        P.op("pool", lambda e: e.memset(E0[:], 0.0), writes=[E0])
        P.op("pool", lambda e: e.memset(E0[0:1, :], 1.0), reads=[E0], writes=[E0])
        Cbc = P.sb("fx_Cbc", [128, NQ, NH], F32)
        nCbc = P.sb("fx_nCbc", [128, NQ, NH], F32)
        for qt_ in range(NQ):
            P.op("pe", lambda e: e.matmul(out=C.small[:, 0:NH], lhsT=E0[:], rhs=cumtm[:, 4 * qt_, :], start=True, stop=True),
                 reads=[E0, (cumtm, 4 * qt_)], writes=[C.small])
            P.op("dve", lambda e: e.tensor_copy(out=Cbc[:, qt_, :], in_=C.small[:, 0:NH]), reads=[C.small], writes=[(Cbc, qt_)])
        P.op("dve", lambda e: e.tensor_scalar(out=nCbc[:], in0=Cbc[:], scalar1=-1.0, scalar2=None, op0=ALU.mult),
             reads=[Cbc], writes=[nCbc])
        Sel = P.sb("fx_Sel", [NH, NH, 128], F32)
        P.op("pool", lambda e: e.memset(Sel[:], 0.0), writes=[Sel])
        for col in (64, 96):
            P.op("dve", lambda e: e.tensor_copy(out=Sel[:, :, col], in_=C.identf[0:NH, 0:NH]), reads=[Sel, C.identf], writes=[Sel])
        mneg = P.sb("fx_mneg", [128, 4, ST], F32)
        P.op("pool", lambda e: e.memset(mneg[:], 0.0), writes=[mneg])
        for r in range(4):
            P.op("pool", lambda e: e.affine_select(out=mneg[:, r, :], in_=mneg[:, r, :], pattern=[[1, ST]], compare_op=ALU.is_ge,
                                                   fill=-30000.0, base=-r * 128, channel_multiplier=-1),
                 reads=[mneg], writes=[mneg])
        KT = [P.sb(f"fx_KT{i}", [128, S], BF16) for i in range(2)]
        QT = [P.sb(f"fx_QT{i}", [128, S], BF16) for i in range(2)]
        VV = [P.sb(f"fx_VV{i}", [128, NT_ * 65], BF16) for i in range(2)]
        for i in range(2):
            P.op("pool", lambda e: e.memset(KT[i][64:128, :], 0.0), writes=[KT[i]])
            P.op("pool", lambda e: e.memset(KT[i][64:65, :], 1.0), reads=[KT[i]], writes=[KT[i]])
            P.op("pool", lambda e: e.memset(KT[i][96:97, :], 1.0), reads=[KT[i]], writes=[KT[i]])
            P.op("pool", lambda e: e.memset(QT[i][64:128, :], 0.0), writes=[QT[i]])
        bias = P.sb("fx_bias", [128, NT_], F32)
        hi96 = P.sb("fx_hi96", [128, ST], BF16)
        smk = [P.sb(f"fx_smk{i}", [128, ST], F32) for i in range(2)]
        PT = [P.sb(f"fx_PT{i}", [128, ST], BF16) for i in range(3)]
        OTs = P.sb("fx_OTs", [65, ST], F32)
        rden = P.sb("fx_rden", [128, 4], F32)
        otm = [P.sb(f"fx_otm{i}", [128, 4, HD], F32) for i in range(2)]
        OTp, TRp = C.trp[0], C.trp[1]
        pti = 0
        for h in range(NH):
            kt_, qt2, vv, sl = KT[h % 2], QT[h % 2], VV[h % 2], s_l[h % 2]
            P.dma("sp", kt_[0:HD, :], KTd[h * HD:(h + 1) * HD, :], sl, writes=[kt_])
            P.dma("sp", qt2[0:HD, :], QTd[h * HD:(h + 1) * HD, :], sl, writes=[qt2])
            P.dma("sp", vv[:], Vd[h], sl, writes=[vv])
            for Q in range(NQ):
                qs_ = slice(Q * ST, (Q + 1) * ST)
                nkt = 4 * (Q + 1)
                pgm = C.bank()
                P.op("pe", lambda e: e.matmul(out=pgm[:], lhsT=Sel[:, h, :], rhs=cumT[:, qs_], start=True, stop=True),
                     reads=[Sel, cumT], writes=[pgm])
                P.op("act", lambda e: e.activation(out=qt2[64:65, qs_], in_=pgm[64:65, :], func=AF.Identity,
                                                   bias=nCbc[64:65, Q, h:h + 1]), reads=[pgm, nCbc], writes=[qt2])
                P.op("act", lambda e: e.activation(out=hi96[96:97, :], in_=pgm[96:97, :], func=AF.Identity,
                                                   bias=nCbc[96:97, Q, h:h + 1]), reads=[pgm, nCbc], writes=[hi96])
                P.op("dve", lambda e: e.scalar_tensor_tensor(out=qt2[96:97, qs_], in0=pgm[96:97, :], scalar=Cbc[96:97, Q, h:h + 1],
                                                             in1=hi96[96:97, :], op0=ALU.subtract, op1=ALU.subtract),
                     reads=[pgm, Cbc, hi96], writes=[qt2])
                P.op("dve", lambda e: e.tensor_scalar(out=bias[:, 0:nkt], in0=cumtm[:, 0:nkt, h], scalar1=-1.0,
                                                      scalar2=Cbc[:, Q, h:h + 1], op0=ALU.mult, op1=ALU.add),
                     reads=[cumtm, Cbc], writes=[bias])
                for kt in range(nkt):
                    ps_ = C.bank()
                    P.op("pe", lambda e: e.matmul(out=ps_[:], lhsT=kt_[:, kt * 128:(kt + 1) * 128], rhs=qt2[:, qs_],
                                                  start=True, stop=True), reads=[kt_, qt2], writes=[ps_])
                    pt = PT[pti % 3]
                    pti += 1
                    r = kt - 4 * Q
                    if r >= 0:
                        sm = smk[r % 2]
                        P.op("dve", lambda e: e.tensor_tensor(out=sm[:], in0=ps_[:], in1=mneg[:, r, :], op=ALU.add),
                             reads=[ps_, mneg], writes=[sm])
                        P.op("act", lambda e: e.activation(out=pt[:], in_=sm[:], func=AF.Exp, bias=bias[:, kt:kt + 1]),
                             reads=[sm, bias], writes=[pt])
                    else:
                        P.op("act", lambda e: e.activation(out=pt[:], in_=ps_[:], func=AF.Exp, bias=bias[:, kt:kt + 1]),
                             reads=[ps_, bias], writes=[pt])
                    P.op("pe", lambda e: e.matmul(out=OTp[0:65, :], lhsT=vv[:, kt * 65:(kt + 1) * 65], rhs=pt[:],
                                                  start=(kt == 0), stop=(kt == nkt - 1)), reads=[vv, pt], writes=[OTp])
                P.op("act", lambda e: e.copy(out=OTs[:], in_=OTp[0:65, :]), reads=[OTp], writes=[OTs])
                for r in range(4):
                    P.op("pe", lambda e: e.transpose(out=TRp[:, r * 128:r * 128 + 65], in_=OTs[:, r * 128:(r + 1) * 128],
                                                     identity=C.identf[0:65, 0:65]), reads=[OTs, C.identf], writes=[TRp])
                ot = otm[(h * NQ + Q) % 2]
                trv = TRp[:].rearrange("p (r c) -> p r c", r=4)
                P.op("dve", lambda e: e.reciprocal(out=rden[:], in_=trv[:, :, 64]), reads=[TRp], writes=[rden])
                P.op("dve", lambda e: e.tensor_tensor(out=ot[:], in0=trv[:, :, 0:HD], in1=rden[:].unsqueeze(2).to_broadcast([128, 4, HD]),
                                                      op=ALU.mult), reads=[TRp, rden], writes=[ot])
                P.dma("sp", Od[Q * ST:(Q + 1) * ST, h * HD:(h + 1) * HD].rearrange("(t p) d -> p t d", p=128), ot[:], s_o2, reads=[ot])
    with Phase(P):
        s_w3 = P.dsem("d_fxw3")
        s_l3 = [P.dsem(f"d_fxl3{i}") for i in range(2)]
        s_o3 = P.dsem("d_fxo3")
        Wout = P.sb("fx_wout", [128, 8, D], BF16)
        load_w_bf16(P, Wout, Wout[:], w_out, s_w3)
        ob_ = [P.sb(f"fx_o{i}", [128, D], F32) for i in range(2)]
        sb_ = [P.sb(f"fx_s{i}", [128, D], F32) for i in range(2)]
        yb = P.sb("fx_yb", [128, D], BF16)
        yT = P.sb("fx_yT", [128, 8, ST], BF16)
        obufs = [P.sb(f"fx_ob{i}", [128, D], F32) for i in range(2)]
        ocnt = [0]
        ti = 0
        for st in range(S // ST):
            t0 = st * ST
            for j in range(ST // 128):
                r0 = t0 + j * 128
                o_, s_, sl = ob_[ti % 2], sb_[ti % 2], s_l3[ti % 2]
                ti += 1
                P.dma("sp", o_[:], Od[r0:r0 + 128, :], sl, writes=[o_])
                P.dma("sp", s_[:], SGd[r0:r0 + 128, :], sl, writes=[s_])
                P.op("dve", lambda e: e.tensor_tensor(out=yb[:], in0=o_[:], in1=s_[:], op=ALU.mult), reads=[o_, s_], writes=[yb])
                for k in range(8):
                    P.op("pe", lambda e: e.transpose(out=C.trb[:, k * 128:(k + 1) * 128], in_=yb[:, k * 128:(k + 1) * 128],
                                                     identity=C.identb[:]), reads=[yb, C.identb], writes=[C.trb])
                P.op("act", lambda e: e.copy(out=yT[:, :, j * 128:(j + 1) * 128], in_=C.trb[:].rearrange("p (k t) -> p k t", k=8)),
                     reads=[C.trb], writes=[(yT, j)])
            emit_outproj(P, C, yT, Wout, ST // 128, lambda j: mix_dst[t0 + j * 128:t0 + (j + 1) * 128, :], mix_tile, s_o3,
                         obufs, ocnt)


DEPTH = 4
IN_SHAPES = {
    "norm_mix": [4, D], "norm_ffn": [4, D], "hg_w_in": [2, D, 4 * D], "hg_w_out": [2, D, D], "hg_gnorm": [2, 128],
    "hg_lb_param": [4, D], "fox_w_in": [1, D, 4 * D + 16], "fox_f_bias": [1, 16], "fox_qnorm": [1, 64], "fox_knorm": [1, 64],
    "fox_w_out": [1, D, D], "rg_w_in": [1, D, 2 * D], "rg_conv_w": [1, 4, D], "rg_conv_b": [1, D], "rg_wa": [1, 4, 256, 256],
    "rg_ba": [1, D], "rg_wx": [1, 4, 256, 256], "rg_bx": [1, D], "rg_lambda": [1, D], "rg_w_out": [1, D, D],
    "router_w": [4, D, NE], "router_b": [4, NE], "moe_w_gu": [4, NE, D, 2 * D], "moe_b_gu": [4, NE, 2 * D],
    "moe_w_dn": [4, NE, D, D], "moe_b_dn": [4, NE, D], "ple_w": [4, 256, D], "ple_norm": [4, D], "ple_gate_norm": [4, D],
    "ple_gate_w": [4, D, D],
}


def build_full(S, depth=DEPTH):
    P = Prog()
    C = Ctx(P)
    nc = P.nc
    a = {}
    x = P.dram("i_x", [S, D], F32, "ExternalInput").ap()
    p = P.dram("i_p", [4, S, 256], F32, "ExternalInput").ap()
    for n, shp in IN_SHAPES.items():
        a[n] = P.dram("i_" + n, shp, F32, "ExternalInput").ap()
    out = P.dram("o_h", [S, D], F32, "ExternalOutput").ap()
    hb = [nc.dram_tensor(f"hbuf{i}", [S, D], F32, kind="Internal").ap() for i in range(2)]
    mixd = nc.dram_tensor("mixd", [S, D], F32, kind="Internal").ap()
    hsrc = x
    for i in range(depth):
        kind, j = i % 3, i // 3
        if kind == 0:
            emit_hgrn2(P, C, S, i, hsrc, None, mixd, None, a["norm_mix"][i], a["hg_w_in"][j], a["hg_w_out"][j], a["hg_gnorm"][j],
                       a["hg_lb_param"])
        elif kind == 1:
            emit_fox(P, C, S, hsrc, None, mixd, None, a["norm_mix"][i], a["fox_w_in"][j], a["fox_f_bias"][j], a["fox_qnorm"][j],
                     a["fox_knorm"][j], a["fox_w_out"][j])
        else:
            emit_rglru(P, C, S, hsrc, None, mixd, None, a["norm_mix"][i], a["rg_w_in"][j], a["rg_conv_w"][j], a["rg_conv_b"][j],
                       a["rg_wa"][j], a["rg_ba"][j], a["rg_wx"][j], a["rg_bx"][j], a["rg_lambda"][j], a["rg_w_out"][j])
        dst = out if i == depth - 1 else hb[i % 2]
        emit_ffn(P, C, S, hsrc, [mixd], p[i], a["norm_ffn"][i], a["router_w"][i], a["router_b"][i], a["moe_w_gu"][i], a["moe_b_gu"][i],
                 a["moe_w_dn"][i], a["moe_b_dn"][i], a["ple_w"][i], a["ple_norm"][i], a["ple_gate_norm"][i], a["ple_gate_w"][i], dst)
        hsrc = dst
    n_inst = P.n_inst
    ncf = P.finish(P.dsems)
    return ncf, n_inst


_NC_CACHE = {}


def kernel(**inputs):
    x = np.ascontiguousarray(np.asarray(inputs["x"], dtype=np.float32))
    p = np.asarray(inputs["p"], dtype=np.float32)
    B, S, _ = x.shape
    if S not in _NC_CACHE:
        _NC_CACHE[S] = build_full(S)[0]
    nc = _NC_CACHE[S]
    shared = {"i_" + n: np.ascontiguousarray(np.asarray(inputs[n], dtype=np.float32)) for n in IN_SHAPES}
    in_maps = []
    for c in range(B):
        m = dict(shared)
        m["i_x"] = np.ascontiguousarray(x[c])
        m["i_p"] = np.ascontiguousarray(p[:, c])
        in_maps.append(m)
    res = run_bass_kernel_spmd(nc, in_maps, core_ids=list(range(B)))
    return np.stack([np.asarray(res.results[c]["o_h"], dtype=np.float32) for c in range(B)], axis=0)
```

```python
import numpy as np
import concourse.bass as bass
import concourse.mybir as mybir
from concourse.bass_utils import run_bass_kernel_spmd
from contextlib import ExitStack

F32 = mybir.dt.float32
BF16 = mybir.dt.bfloat16
I32 = mybir.dt.int32
AF = mybir.ActivationFunctionType
ALU = mybir.AluOpType
AX = mybir.AxisListType


class _St:
    __slots__ = ("w", "r")

    def __init__(self):
        self.w = []
        self.r = {}


class Tile:
    def __init__(self, t, name):
        self.t = t
        self.name = name
        self.whole = _St()
        self.parts = {}
        self.psum = False

    def __getitem__(self, idx):
        return self.t[idx]


class Sem:
    def __init__(self, h, name, is_dma=False):
        self.h = h
        self.name = name
        self.is_dma = is_dma
        self.issued = 0
        self.last_wait = 0


class Eng:
    def __init__(self, P, name, e, sem):
        self.P = P
        self.name = name
        self.e = e
        self.sem = sem
        self.known = {}


class Prog:
    def __init__(self):
        self.nc = bass.Bass("TRN2", target_bir_lowering=False)
        self.es = ExitStack()
        self.gstack = self.es
        self.dsems = []
        nc = self.nc
        self.engs = {}
        for nm, e in (("pe", nc.tensor), ("dve", nc.vector), ("act", nc.scalar),
                      ("pool", nc.gpsimd), ("sp", nc.sync)):
            s = Sem(self.es.enter_context(nc.semaphore("s_" + nm)), "s_" + nm)
            self.engs[nm] = Eng(self, nm, e, s)
        self.n_inst = 0

    def dram(self, name, shape, dt, kind):
        return self.nc.dram_tensor(name, list(shape), dt, kind=kind)

    def sb(self, name, shape, dt):
        self.uid = getattr(self, "uid", 0) + 1
        name = f"{name}_u{self.uid}"
        t = self.es.enter_context(self.nc.sbuf_tensor(name, list(shape), dt))
        return Tile(t, name)

    def ps(self, name, shape, dt=F32):
        t = self.es.enter_context(self.nc.psum_tensor(name, list(shape), dt))
        tl = Tile(t, name)
        tl.psum = True
        return tl

    def dsem(self, name):
        return Sem(self.es.enter_context(self.nc.semaphore(name)), name, is_dma=True)

    @staticmethod
    def _norm(lst):
        out = []
        for x in lst or []:
            if x is None:
                continue
            if isinstance(x, tuple):
                out.append(x)
            else:
                out.append((x, None))
        return out

    def _deps(self, reads, writes):
        deps = []
        for (t, k) in reads:
            deps += t.whole.w
            if k is None:
                for st in t.parts.values():
                    deps += st.w
            elif k in t.parts:
                deps += t.parts[k].w
            if t.psum:
                deps += list(t.whole.r.items())
                for st in t.parts.values():
                    deps += list(st.r.items())
        for (t, k) in writes:
            deps += t.whole.w + list(t.whole.r.items())
            if k is None:
                for st in t.parts.values():
                    deps += st.w + list(st.r.items())
            elif k in t.parts:
                deps += t.parts[k].w + list(t.parts[k].r.items())
        return deps

    def _mark(self, reads, writes, tok):
        for (t, k) in reads:
            st = t.whole if k is None else t.parts.setdefault(k, _St())
            if st.r.get(tok[0], 0) < tok[1]:
                st.r[tok[0]] = tok[1]
        for (t, k) in writes:
            if k is None:
                t.whole.w = [tok]
                t.whole.r = {}
                t.parts = {}
            else:
                st = t.parts.setdefault(k, _St())
                st.w = [tok]
                st.r = {}

    def _wait(self, E, deps, skip_self=False):
        need = {}
        for (s, v) in deps:
            if skip_self and s is E.sem:
                continue
            if s.is_dma:
                assert v == s.issued or E.known.get(s, 0) >= v or True
                v = s.issued
            if v > need.get(s, 0):
                need[s] = v
        for s, v in need.items():
            if E.known.get(s, 0) < v:
                E.e.wait_ge(s.h, v)
                E.known[s] = v
                if s.is_dma and v > s.last_wait:
                    s.last_wait = v

    def op(self, eng, fn, reads=None, writes=None):
        E = self.engs[eng]
        reads = self._norm(reads)
        writes = self._norm(writes)
        deps = self._deps(reads, writes)
        self._wait(E, deps, skip_self=(eng == "pe"))
        inst = fn(E.e)
        E.sem.issued += 1
        inst.then_inc(E.sem.h, 1)
        self._mark(reads, writes, (E.sem, E.sem.issued))
        self.n_inst += 1
        return inst

    def dma(self, q, out, in_, sem, reads=None, writes=None, **kw):
        E = self.engs[q]
        reads = self._norm(reads)
        writes = self._norm(writes)
        deps = self._deps(reads, writes)
        self._wait(E, deps)
        if sem.last_wait > E.known.get(sem, 0):
            E.e.wait_ge(sem.h, sem.issued)
            E.known[sem] = sem.issued
        kind = "sw" if q == "pool" else "hw"
        assert getattr(sem, "qkind", kind) == kind, f"sem {sem.name} mixes SW and HW DGE"
        sem.qkind = kind
        inst = E.e.dma_start(out=out, in_=in_, **kw)
        sem.issued += 16
        inst.then_inc(sem.h, 16)
        self._mark(reads, writes, (sem, sem.issued))
        self.n_inst += 1
        return inst

    def finish(self, out_sems):
        E = self.engs["sp"]
        for s in out_sems:
            if E.known.get(s, 0) < s.issued:
                E.e.wait_ge(s.h, s.issued)
                E.known[s] = s.issued
        self.es.close()
        return self.nc


D = 1024
EPS = 1e-6


class Ctx:
    def __init__(self, P):
        self.P = P
        self.identf = P.sb("identf", [128, 128], F32)
        self.identb = P.sb("identb", [128, 128], BF16)
        P.op("pool", lambda e: e.memset(self.identf[:], 0.0), writes=[self.identf])
        P.op("pool", lambda e: e.affine_select(out=self.identf[:], in_=self.identf[:], pattern=[[-1, 128]],
                                               compare_op=ALU.not_equal, fill=1.0, base=0, channel_multiplier=1),
             reads=[self.identf], writes=[self.identf])
        P.op("dve", lambda e: e.tensor_copy(out=self.identb[:], in_=self.identf[:]),
             reads=[self.identf], writes=[self.identb])
        self.mm = [P.ps(f"mm{i}", [128, 512], F32) for i in range(4)]
        self.mm_i = 0
        self.trp = [P.ps(f"trp{i}", [128, 512], F32) for i in range(2)]
        self.trb = P.ps("trb", [128, 1024], BF16)
        self.small = P.ps("smallps", [128, 512], F32)
        self.junk = P.sb("junk", [128, 1024], F32)
        self.st = {}
        for nm, w in (("ss", 1), ("rstd", 1), ("negm", 1), ("ssum", 1), ("ssa", 2), ("ssa1", 1), ("rstda", 1), ("ss2", 1),
                      ("rstd2", 1)):
            self.st[nm] = P.sb("st_" + nm, [128, w], F32)

    def bank(self):
        b = self.mm[self.mm_i % 4]
        self.mm_i += 1
        return b

    def stat(self, name, w=1):
        return self.st[name]


def bc_load(P, q, name, src_ap, n, sem):
    t = P.sb(name, [128, n], F32)
    P.dma(q, t[:], src_ap.partition_broadcast(128), sem, writes=[t])
    return t


def emit_rstd(P, C, ss, rstd, n, p=128):
    (sst, ssa), (rt, ra) = ss, rstd
    P.op("dve", lambda e: e.tensor_scalar(out=ra, in0=ssa, scalar1=1.0 / n, scalar2=EPS,
                                          op0=ALU.mult, op1=ALU.add), reads=[sst], writes=[rt])
    P.op("act", lambda e: e.activation(out=ra, in_=ra, func=AF.Sqrt), reads=[rt], writes=[rt])
    P.op("dve", lambda e: e.reciprocal(out=ra, in_=ra), reads=[rt], writes=[rt])


def load_w_bf16(P, dst, dst_ap, src_ap, sem, key=None):
    P.dma("pool", dst_ap, src_ap.rearrange("(k p) n -> p k n", p=128), sem, writes=[(dst, key)])


GT = 512
NE = 32
SPARSE = True
CAP = 128


def emit_ffn(P, C, NT, h_in, parts, p_in, norm_ffn, router_w, router_b, w_gu, b_gu, w_dn, b_dn, ple_w, ple_norm,
             ple_gate_norm, ple_gate_w, h_out, n_exp=NE, stop=None):
    n_part = len(parts)
    with Phase(P):
        return _emit_ffn(P, C, NT, h_in, parts, p_in, norm_ffn, router_w, router_b, w_gu, b_gu, w_dn, b_dn, ple_w, ple_norm,
                         ple_gate_norm, ple_gate_w, h_out, n_exp, stop, n_part)


def _emit_ffn(P, C, NT, h_in, parts, p_in, norm_ffn, router_w, router_b, w_gu, b_gu, w_dn, b_dn, ple_w, ple_norm,
              ple_gate_norm, ple_gate_w, h_out, n_exp, stop, n_part):
    s_c = P.dsem("d_const")
    s_out = P.dsem("d_out")
    g_ffn = bc_load(P, "sp", "g_ffn", norm_ffn, D, s_c)
    g_ple = bc_load(P, "sp", "g_ple", ple_norm, D, s_c)
    g_gate = bc_load(P, "sp", "g_gate", ple_gate_norm, D, s_c)
    br_bc = bc_load(P, "sp", "br_bc", router_b, NE, s_c)
    wr_sb = P.sb("wr_sb", [128, 8, NE], F32)
    P.dma("sp", wr_sb[:], router_w.rearrange("(k p) n -> p k n", p=128), s_c, writes=[wr_sb])
    s_cw = P.dsem("d_constw")
    plew = P.sb("plew", [128, 2, D], BF16)
    load_w_bf16(P, plew, plew[:], ple_w, s_cw)
    plegw = P.sb("plegw", [128, 8, D], BF16)
    load_w_bf16(P, plegw, plegw[:], ple_gate_w, s_cw)
    big = [P.sb(f"big{i}", [128, D], F32) for i in range(3)]
    s_b = P.dsem("d_brow")
    P.dma("sp", big[0][0:NE, :], b_gu[:, 0:D], s_b, writes=[big[0]])
    P.dma("sp", big[1][0:NE, :], b_gu[:, D:2 * D], s_b, writes=[big[1]])
    P.dma("sp", big[2][0:NE, :], b_dn, s_b, writes=[big[2]])
    bfm = P.sb("bfm", [128, 24, NE], F32)
    for m in range(24):
        brow = big[m // 8]
        P.op("pe", lambda e: e.transpose(out=C.small[:, 0:NE], in_=brow[0:NE, (m % 8) * 128:(m % 8 + 1) * 128],
                                         identity=C.identf[0:NE, 0:NE]),
             reads=[brow, C.identf], writes=[C.small])
        P.op("dve", lambda e: e.tensor_copy(out=bfm[:, m, :], in_=C.small[:, 0:NE]),
             reads=[C.small], writes=[(bfm, m)])

    if stop == "setup":
        P.dma("sp", h_out[0:128, 0:24 * NE], bfm[:].rearrange("p m e -> p (m e)"), s_out, reads=[bfm])
        return None
    NS = 8
    wslot = [P.sb(f"wslot{i}", [128, 8, 512], BF16) for i in range(NS)]
    wsem = [P.dsem(f"d_w{i}") for i in range(NS)]
    hsem = [P.dsem(f"d_h{i}") for i in range(2)]
    pbuf = [P.sb(f"pb{i}", [128, 256], F32) for i in range(2)]
    psem = [P.dsem(f"d_p{i}") for i in range(2)]
    h1 = P.sb("h1", [128, 1, D], F32)
    xn = P.sb("xn", [128, D], F32)
    xnb = P.sb("xnb", [128, D], BF16)
    xnTf = P.sb("xnTf", [128, 8, 128], F32)
    G_l = [P.sb(f"G{u}", [NE, GT], F32) for u in range(2)]
    G = G_l[0]
    if not SPARSE:
        xnT = P.sb("xnT", [128, 8, GT], BF16)
        selt = P.sb("selt", [NE, 128], F32)
        Gbc = P.sb("Gbc", [128, GT], F32)
        actT = P.sb("actT", [128, 8, GT], BF16)
        yacc = P.sb("yacc", [128, 8, GT], F32)
    else:
        xnT = actT = yacc = None
        xn_tm_l = [P.sb(f"xn_tm{u}", [128, GT // 128, D], BF16) for u in range(2)]
        gates_tm_l = [P.sb(f"gates_tm{u}", [128, GT // 128, NE], F32) for u in range(2)]
        Rk_l = [P.sb(f"Rk{u}", [NE, GT], BF16) for u in range(2)]
        rks = P.sb("rks", [128, NE], F32)
        cnt_bc_l = [P.sb(f"cnt_bc{u}", [128, NE], F32) for u in range(2)]
        Ltri = P.sb("Ltri", [128, 128], F32)
        onesf = P.sb("onesf", [128, 128], F32)
        pidx = P.sb("pidx", [128, 1], F32)
        selb = P.sb("selb", [NE, 128], BF16)
        SelT = P.sb("SelT", [128, GT], BF16)
        Selsb = P.sb("Selsb", [128, GT // 128, 128], BF16)
        XcT = P.sb("XcT", [128, 8, CAP], BF16)
        actS = P.sb("actS", [128, 8, CAP], BF16)
        Ycs = P.sb("Ycs", [128, D], BF16)
        yacc_tm_l = [P.sb(f"yacc_tm{u}", [128, GT // 128, D], F32) for u in range(2)]
        bdnr = P.sb("bdnr", [NE, D], F32)
        P.dma("sp", bdnr[:], b_dn, s_b, writes=[bdnr])
        P.op("pool", lambda e: e.memset(onesf[:], 1.0), writes=[onesf])
        P.op("pool", lambda e: e.memset(Ltri[:], 1.0), writes=[Ltri])
        P.op("pool", lambda e: e.affine_select(out=Ltri[:], in_=Ltri[:], pattern=[[1, 128]], compare_op=ALU.is_ge, fill=0.0,
                                               base=-1, channel_multiplier=-1), reads=[Ltri], writes=[Ltri])
        P.op("pool", lambda e: e.iota(pidx[:], pattern=[[0, 1]], base=0, channel_multiplier=1,
                                      allow_small_or_imprecise_dtypes=True), writes=[pidx])
    NEW = 6
    ew = [P.sb(f"ew{i}", [128, CAP if SPARSE else GT], F32) for i in range(NEW)]
    ew_i = [0]

    def tmp():
        t = ew[ew_i[0] % NEW]
        ew_i[0] += 1
        return t
    lg = P.sb("lg", [128, NE], F32)
    top8 = P.sb("top8", [128, 8], F32)
    msk = P.sb("msk", [128, NE], F32)
    exs = P.sb("exs", [128, NE], F32)
    pT = P.sb("pT", [128, 2, 128], BF16)
    hnT = P.sb("hnT", [128, 8, 128], BF16)
    otile = [big[1]]

    NG = NT // GT
    JT = GT // 128
    chunks = []
    for g in range(NG // (2 if NG % 2 == 0 else 1)):
        for e in range(n_exp):
            chunks.append(w_gu[e][:, 0:512])
            chunks.append(w_gu[e][:, D:D + 512])
            chunks.append(w_gu[e][:, 512:D])
            chunks.append(w_gu[e][:, D + 512:2 * D])
            chunks.append(w_dn[e][:, 0:512])
            chunks.append(w_dn[e][:, 512:D])
    issued = [0]

    def ensure(ci):
        while issued[0] <= min(ci, len(chunks) - 1):
            c = issued[0]
            load_w_bf16(P, wslot[c % NS], wslot[c % NS][:], chunks[c], wsem[c % NS])
            issued[0] += 1

    assert SPARSE
    SUB = 2 if NG % 2 == 0 else 1

    def secA(g, u):
        t0 = g * GT
        xn_tm, gates_tm, Rk, G, yacc_tm, cnt_bc = xn_tm_l[u], gates_tm_l[u], Rk_l[u], G_l[u], yacc_tm_l[u], cnt_bc_l[u]
        for j in range(JT):
            r0 = t0 + j * 128
            P.dma("sp", h1[:, 0, :], h_in[r0:r0 + 128, :], hsem[0], writes=[(h1, 0)])
            for i in range(n_part):
                stg = big[1 + i % 2]
                P.dma("sp", stg[:], parts[i][r0:r0 + 128, :], hsem[1], writes=[stg])
                P.op("pool", lambda e: e.tensor_tensor(out=h1[:, 0, :], in0=h1[:, 0, :], in1=stg[:], op=ALU.add),
                     reads=[(h1, 0), stg], writes=[(h1, 0)])
            def bail(ap, rd, w):
                P.dma("sp", h_out[0:ap.shape[0], 0:w], ap, s_out, reads=rd)
                return None
            if stop == "A1":
                return bail(h1[:, 0, :], [h1], 1024)
            ss = C.stat("ss")
            rstd = C.stat("rstd")
            P.op("act", lambda e: e.activation(out=C.junk[:], in_=h1[:, 0, :], func=AF.Square, accum_out=ss[:]),
                 reads=[(h1, 0)], writes=[C.junk, ss])
            emit_rstd(P, C, (ss, ss[:]), (rstd, rstd[:]), D)
            P.op("dve", lambda e: e.scalar_tensor_tensor(out=xn[:], in0=h1[:, 0, :], scalar=rstd[:, 0:1], in1=g_ffn[:],
                                                         op0=ALU.mult, op1=ALU.mult),
                 reads=[(h1, 0), rstd, g_ffn], writes=[xn])
            if stop == "A2":
                return bail(xn[:], [xn], 1024)
            for k in range(8):
                tr = C.trp[k // 4]
                P.op("pe", lambda e: e.transpose(out=tr[:, (k % 4) * 128:(k % 4 + 1) * 128],
                                                 in_=xn[:, k * 128:(k + 1) * 128], identity=C.identf[:]),
                     reads=[xn, C.identf], writes=[(tr, k % 4)])
            import os as _os
            VAR = _os.environ.get("VAR", "")
            for hh in range(2):
                tr = C.trp[hh]
                if VAR != "1" and not SPARSE:
                    P.op("act", lambda e: e.copy(out=xnT[:, hh * 4:(hh + 1) * 4, j * 128:(j + 1) * 128],
                                                 in_=tr[:].rearrange("p (k t) -> p k t", k=4)),
                         reads=[tr], writes=[(xnT, (j, hh))])
                if VAR != "2":
                    P.op("dve", lambda e: e.tensor_copy(out=xnTf[:, hh * 4:(hh + 1) * 4, :],
                                                        in_=tr[:].rearrange("p (k t) -> p k t", k=4)),
                         reads=[tr], writes=[(xnTf, hh)])
            if SPARSE:
                P.op("pool", lambda e: e.tensor_copy(out=xn_tm[:, j, :], in_=xn[:]), reads=[xn], writes=[(xn_tm, j)])
            if stop == "A3":
                return bail(xnTf[:].rearrange("p k t -> p (k t)"), [xnTf], 1024)
            for k in range(8):
                P.op("pe", lambda e: e.matmul(out=C.small[:, 0:NE], lhsT=xnTf[:, k, :], rhs=wr_sb[:, k, :],
                                              start=(k == 0), stop=(k == 7)),
                     reads=[(xnTf, k // 4), wr_sb], writes=[C.small])
            P.op("dve", lambda e: e.tensor_tensor(out=lg[:], in0=C.small[:, 0:NE], in1=br_bc[:], op=ALU.add),
                 reads=[C.small, br_bc], writes=[lg])
            if stop == "A4":
                return bail(lg[:], [lg], NE)
            P.op("dve", lambda e: e.max(out=top8[:], in_=lg[:]), reads=[lg], writes=[top8])
            P.op("dve", lambda e: e.tensor_scalar(out=msk[:], in0=lg[:], scalar1=top8[:, 3:4], scalar2=None,
                                                  op0=ALU.is_ge), reads=[lg, top8], writes=[msk])
            negm = C.stat("negm")
            P.op("dve", lambda e: e.tensor_scalar(out=negm[:], in0=top8[:, 0:1], scalar1=-1.0, scalar2=None,
                                                  op0=ALU.mult), reads=[top8], writes=[negm])
            P.op("act", lambda e: e.activation(out=exs[:], in_=lg[:], func=AF.Exp, bias=negm[:, 0:1], scale=1.0),
                 reads=[lg, negm], writes=[exs])
            P.op("dve", lambda e: e.tensor_tensor(out=exs[:], in0=exs[:], in1=msk[:], op=ALU.mult),
                 reads=[exs, msk], writes=[exs])
            ssum = C.stat("ssum")
            P.op("dve", lambda e: e.tensor_reduce(out=ssum[:], in_=exs[:], axis=AX.X, op=ALU.add),
                 reads=[exs], writes=[ssum])
            P.op("dve", lambda e: e.reciprocal(out=ssum[:], in_=ssum[:]), reads=[ssum], writes=[ssum])
            P.op("dve", lambda e: e.tensor_scalar(out=exs[:], in0=exs[:], scalar1=ssum[:, 0:1], scalar2=None,
                                                  op0=ALU.mult), reads=[exs, ssum], writes=[exs])
            if stop == "A5":
                return bail(exs[:], [exs], NE)
            P.op("pe", lambda e: e.transpose(out=C.small[0:NE, 128:256], in_=exs[:], identity=C.identf[:]),
                 reads=[exs, C.identf], writes=[C.small])
            P.op("dve", lambda e: e.tensor_copy(out=G[:, j * 128:(j + 1) * 128], in_=C.small[0:NE, 128:256]),
                 reads=[C.small], writes=[(G, j)])
            if SPARSE:
                P.op("pool", lambda e: e.tensor_copy(out=gates_tm[:, j, :], in_=exs[:]), reads=[exs], writes=[(gates_tm, j)])
                if j == 0:
                    P.op("pool", lambda e: e.memset(cnt_bc[:], 0.0), writes=[cnt_bc])
                P.op("pe", lambda e: e.matmul(out=C.small[:, 256:256 + NE], lhsT=Ltri[:], rhs=msk[:], start=True, stop=True),
                     reads=[Ltri, msk], writes=[C.small])
                P.op("dve", lambda e: e.tensor_tensor(out=rks[:], in0=C.small[:, 256:256 + NE], in1=cnt_bc[:], op=ALU.add),
                     reads=[C.small, cnt_bc], writes=[rks])
                P.op("pe", lambda e: e.matmul(out=C.small[:, 320:320 + NE], lhsT=onesf[:], rhs=msk[:], start=True, stop=True),
                     reads=[onesf, msk], writes=[C.small])
                P.op("dve", lambda e: e.tensor_tensor(out=cnt_bc[:], in0=C.small[:, 320:320 + NE], in1=cnt_bc[:], op=ALU.add),
                     reads=[C.small, cnt_bc], writes=[cnt_bc])
                P.op("dve", lambda e: e.scalar_tensor_tensor(out=rks[:], in0=rks[:], scalar=-255.0, in1=msk[:], op0=ALU.add,
                                                             op1=ALU.mult), reads=[rks, msk], writes=[rks])
                P.op("dve", lambda e: e.tensor_scalar(out=rks[:], in0=rks[:], scalar1=255.0, scalar2=None, op0=ALU.add),
                     reads=[rks], writes=[rks])
                P.op("pe", lambda e: e.transpose(out=C.small[0:NE, 128:256], in_=rks[:], identity=C.identf[:]),
                     reads=[rks, C.identf], writes=[C.small])
                P.op("dve", lambda e: e.tensor_copy(out=Rk[:, j * 128:(j + 1) * 128], in_=C.small[0:NE, 128:256]),
                     reads=[C.small], writes=[(Rk, j)])


    def secE(g, u, ei, ci):
        t0 = g * GT
        xn_tm, gates_tm, Rk, G, yacc_tm, cnt_bc = xn_tm_l[u], gates_tm_l[u], Rk_l[u], G_l[u], yacc_tm_l[u], cnt_bc_l[u]
        P.op("dve", lambda e: e.tensor_copy(out=selb[:], in_=C.identb[0:NE, ei:ei + 1].to_broadcast([NE, 128])),
             reads=[C.identb], writes=[selb])
        pr = C.bank()
        P.op("pe", lambda e: e.matmul(out=pr[:], lhsT=selb[:], rhs=Rk[:], start=True, stop=True), reads=[selb, Rk], writes=[pr])
        P.op("dve", lambda e: e.tensor_scalar(out=SelT[:], in0=pr[:], scalar1=pidx[:, 0:1], scalar2=None, op0=ALU.is_equal),
             reads=[pr, pidx], writes=[SelT])
        for tc in range(JT):
            P.op("pe", lambda e: e.transpose(out=C.trb[:, tc * 128:(tc + 1) * 128], in_=SelT[:, tc * 128:(tc + 1) * 128],
                                             identity=C.identb[:]), reads=[SelT, C.identb], writes=[C.trb])
        P.op("act", lambda e: e.copy(out=Selsb[:], in_=C.trb[:, 0:GT].rearrange("p (c s) -> p c s", c=JT)),
             reads=[C.trb], writes=[Selsb])
        for hf in range(2):
            pgx = C.bank()
            for kk in range(4):
                k = hf * 4 + kk
                for tc in range(JT):
                    P.op("pe", lambda e: e.matmul(out=pgx[:, kk * 128:(kk + 1) * 128], lhsT=xn_tm[:, tc, k * 128:(k + 1) * 128],
                                                  rhs=Selsb[:, tc, :], start=(tc == 0), stop=(tc == JT - 1)),
                         reads=[xn_tm, Selsb], writes=[pgx])
            if hf == 0:
                P.op("act", lambda e: e.copy(out=XcT[:, 0:4, :], in_=pgx[:].rearrange("p (k s) -> p k s", k=4)),
                     reads=[pgx], writes=[(XcT, 0)])
            else:
                P.op("dve", lambda e: e.tensor_copy(out=XcT[:, 4:8, :], in_=pgx[:].rearrange("p (k s) -> p k s", k=4)),
                     reads=[pgx], writes=[(XcT, 1)])
        for m in range(8):
            ms = slice((m % 4) * 128, (m % 4 + 1) * 128)
            wa, wb = wslot[(ci + 2 * (m // 4)) % NS], wslot[(ci + 2 * (m // 4) + 1) % NS]
            pgl = C.bank()
            for k in range(8):
                P.op("pe", lambda e: e.matmul(out=pgl[:, 0:CAP], lhsT=wa[:, k, ms], rhs=XcT[:, k, :], start=(k == 0), stop=(k == 7)),
                     reads=[wa, XcT], writes=[pgl])
            pli = C.bank()
            for k in range(8):
                P.op("pe", lambda e: e.matmul(out=pli[:, 0:CAP], lhsT=wb[:, k, ms], rhs=XcT[:, k, :], start=(k == 0), stop=(k == 7)),
                     reads=[wb, XcT], writes=[pli])
            g1, sg, l1 = tmp(), tmp(), tmp()
            P.op("dve", lambda e: e.tensor_scalar(out=g1[:, 0:CAP], in0=pgl[:, 0:CAP], scalar1=bfm[:, m, ei:ei + 1], scalar2=7.0,
                                                  op0=ALU.add, op1=ALU.min), reads=[pgl, bfm], writes=[g1])
            P.op("act", lambda e: e.activation(out=sg[:, 0:CAP], in_=g1[:, 0:CAP], func=AF.Sigmoid, scale=1.702),
                 reads=[g1], writes=[sg])
            P.op("dve", lambda e: e.tensor_scalar(out=l1[:, 0:CAP], in0=pli[:, 0:CAP], scalar1=bfm[:, 8 + m, ei:ei + 1], scalar2=7.0,
                                                  op0=ALU.add, op1=ALU.min), reads=[pli, bfm], writes=[l1])
            P.op("dve", lambda e: e.tensor_scalar(out=l1[:, 0:CAP], in0=l1[:, 0:CAP], scalar1=-7.0, scalar2=1.0,
                                                  op0=ALU.max, op1=ALU.add), reads=[l1], writes=[l1])
            P.op("pool", lambda e: e.tensor_tensor(out=g1[:, 0:CAP], in0=g1[:, 0:CAP], in1=sg[:, 0:CAP], op=ALU.mult),
                 reads=[g1, sg], writes=[g1])
            P.op("pool", lambda e: e.tensor_tensor(out=actS[:, m, :], in0=g1[:, 0:CAP], in1=l1[:, 0:CAP], op=ALU.mult),
                 reads=[g1, l1], writes=[(actS, m)])
        for hf in range(2):
            wd = wslot[(ci + 4 + hf) % NS]
            py = C.bank()
            for k in range(8):
                P.op("pe", lambda e: e.matmul(out=py[:], lhsT=actS[:, k, :], rhs=wd[:, k, :], start=(k == 0), stop=(k == 7)),
                     reads=[wd, (actS, k)], writes=[py])
            if hf == 0:
                P.op("act", lambda e: e.copy(out=Ycs[:, 0:512], in_=py[:]), reads=[py], writes=[(Ycs, 0)])
            else:
                P.op("dve", lambda e: e.tensor_copy(out=Ycs[:, 512:1024], in_=py[:]), reads=[py], writes=[(Ycs, 1)])
        for tc in range(JT):
            for hf in range(2):
                hs = slice(hf * 512, (hf + 1) * 512)
                psc = C.bank()
                P.op("pe", lambda e: e.matmul(out=psc[:], lhsT=SelT[:, tc * 128:(tc + 1) * 128], rhs=Ycs[:, hs], start=True, stop=True),
                     reads=[SelT, (Ycs, hf)], writes=[psc])
                if ei == 0:
                    P.op("dve", lambda e: e.tensor_scalar(out=yacc_tm[:, tc, hs], in0=psc[:], scalar1=gates_tm[:, tc, ei:ei + 1],
                                                          scalar2=None, op0=ALU.mult),
                         reads=[psc, (gates_tm, tc)], writes=[(yacc_tm, (tc, hf))])
                else:
                    P.op("dve", lambda e: e.scalar_tensor_tensor(out=yacc_tm[:, tc, hs], in0=psc[:], scalar=gates_tm[:, tc, ei:ei + 1],
                                                                 in1=yacc_tm[:, tc, hs], op0=ALU.mult, op1=ALU.add),
                         reads=[psc, (gates_tm, tc), (yacc_tm, (tc, hf))], writes=[(yacc_tm, (tc, hf))])

    def secBD(g, u):
        t0 = g * GT
        xn_tm, gates_tm, Rk, G, yacc_tm, cnt_bc = xn_tm_l[u], gates_tm_l[u], Rk_l[u], G_l[u], yacc_tm_l[u], cnt_bc_l[u]
        for tc in range(JT):
            for hf in range(2):
                hs = slice(hf * 512, (hf + 1) * 512)
                pbd = C.bank()
                P.op("pe", lambda e: e.matmul(out=pbd[:], lhsT=G[:, tc * 128:(tc + 1) * 128], rhs=bdnr[:, hs], start=True, stop=True),
                     reads=[G, bdnr], writes=[pbd])
                P.op("dve", lambda e: e.tensor_tensor(out=yacc_tm[:, tc, hs], in0=pbd[:], in1=yacc_tm[:, tc, hs], op=ALU.add),
                     reads=[pbd, (yacc_tm, (tc, hf))], writes=[(yacc_tm, (tc, hf))])

    def secC(g, u):
        t0 = g * GT
        xn_tm, gates_tm, Rk, G, yacc_tm, cnt_bc = xn_tm_l[u], gates_tm_l[u], Rk_l[u], G_l[u], yacc_tm_l[u], cnt_bc_l[u]
        for j in range(JT):
            r0 = t0 + j * 128
            js = slice(j * 128, (j + 1) * 128)
            pb_ = pbuf[j % 2]
            P.dma("sp", pb_[:], p_in[r0:r0 + 128, :], psem[j % 2], writes=[pb_])
            P.dma("sp", h1[:, 0, :], h_in[r0:r0 + 128, :], hsem[0], writes=[(h1, 0)])
            for i in range(n_part):
                stg = big[1 + i % 2]
                P.dma("sp", stg[:], parts[i][r0:r0 + 128, :], hsem[1], writes=[stg])
                P.op("pool", lambda e: e.tensor_tensor(out=h1[:, 0, :], in0=h1[:, 0, :], in1=stg[:], op=ALU.add),
                     reads=[(h1, 0), stg], writes=[(h1, 0)])
            for m in (range(8) if not SPARSE else []):
                tr = C.trp[m // 4]
                P.op("pe", lambda e: e.transpose(out=tr[:, (m % 4) * 128:(m % 4 + 1) * 128],
                                                 in_=yacc[:, m, js], identity=C.identf[:]),
                     reads=[(yacc, m), C.identf], writes=[(tr, m % 4)])
            h2 = big[0]
            for hh in range(2):
                if SPARSE:
                    P.op("pool", lambda e: e.tensor_tensor(out=h2[:, hh * 512:(hh + 1) * 512], in0=yacc_tm[:, j, hh * 512:(hh + 1) * 512],
                                                           in1=h1[:, 0, hh * 512:(hh + 1) * 512], op=ALU.add),
                         reads=[(yacc_tm, (j, hh)), (h1, 0)], writes=[(h2, hh)])
                    continue
                P.op("dve", lambda e: e.tensor_tensor(out=h2[:, hh * 512:(hh + 1) * 512], in0=C.trp[hh][:],
                                                      in1=h1[:, 0, hh * 512:(hh + 1) * 512], op=ALU.add),
                     reads=[C.trp[hh], (h1, 0)], writes=[(h2, hh)])
            for k in range(2):
                P.op("pe", lambda e: e.transpose(out=C.trp[0][:, k * 128:(k + 1) * 128], in_=pb_[:, k * 128:(k + 1) * 128],
                                                 identity=C.identf[:]), reads=[pb_, C.identf], writes=[(C.trp[0], k)])
            P.op("act", lambda e: e.copy(out=pT[:], in_=C.trp[0][:, 0:256].rearrange("p (k t) -> p k t", k=2)),
                 reads=[C.trp[0]], writes=[pT])
            pA = [C.bank(), C.bank()]
            for hh in range(2):
                for k in range(2):
                    P.op("pe", lambda e: e.matmul(out=pA[hh][:], lhsT=pT[:, k, :], rhs=plew[:, k, hh * 512:(hh + 1) * 512],
                                                  start=(k == 0), stop=(k == 1)),
                         reads=[pT, plew], writes=[pA[hh]])
            ssa = C.stat("ssa", 2)
            for hh in range(2):
                P.op("act", lambda e: e.activation(out=C.junk[:, hh * 512:(hh + 1) * 512], in_=pA[hh][:], func=AF.Square,
                                                   accum_out=ssa[:, hh:hh + 1]),
                     reads=[pA[hh]], writes=[(C.junk, hh), (ssa, hh)])
            ssa1 = C.stat("ssa1")
            P.op("dve", lambda e: e.tensor_tensor(out=ssa1[:], in0=ssa[:, 0:1], in1=ssa[:, 1:2], op=ALU.add),
                 reads=[ssa], writes=[ssa1])
            rstda = C.stat("rstda")
            emit_rstd(P, C, (ssa1, ssa1[:]), (rstda, rstda[:]), D)
            An = big[1]
            for hh in range(2):
                hs = slice(hh * 512, (hh + 1) * 512)
                P.op("dve", lambda e: e.scalar_tensor_tensor(out=An[:, hs], in0=pA[hh][:], scalar=rstda[:, 0:1],
                                                             in1=g_ple[:, hs], op0=ALU.mult, op1=ALU.mult),
                     reads=[pA[hh], rstda, g_ple], writes=[(An, hh)])
            ss2 = C.stat("ss2")
            rstd2 = C.stat("rstd2")
            P.op("act", lambda e: e.activation(out=C.junk[:], in_=h2[:], func=AF.Square, accum_out=ss2[:]),
                 reads=[h2], writes=[C.junk, ss2])
            emit_rstd(P, C, (ss2, ss2[:]), (rstd2, rstd2[:]), D)
            P.op("dve", lambda e: e.scalar_tensor_tensor(out=xnb[:], in0=h2[:], scalar=rstd2[:, 0:1], in1=g_gate[:],
                                                         op0=ALU.mult, op1=ALU.mult),
                 reads=[h2, rstd2, g_gate], writes=[xnb])
            for k in range(8):
                P.op("pe", lambda e: e.transpose(out=C.trb[:, k * 128:(k + 1) * 128], in_=xnb[:, k * 128:(k + 1) * 128],
                                                 identity=C.identb[:]), reads=[xnb, C.identb], writes=[(C.trb, k)])
            P.op("act", lambda e: e.copy(out=hnT[:], in_=C.trb[:].rearrange("p (k t) -> p k t", k=8)),
                 reads=[C.trb], writes=[hnT])
            pB = [C.bank(), C.bank()]
            for hh in range(2):
                for k in range(8):
                    P.op("pe", lambda e: e.matmul(out=pB[hh][:], lhsT=hnT[:, k, :], rhs=plegw[:, k, hh * 512:(hh + 1) * 512],
                                                  start=(k == 0), stop=(k == 7)),
                         reads=[hnT, plegw], writes=[pB[hh]])
            sB = big[2]
            ot = otile[0]
            for hh in range(2):
                hs = slice(hh * 512, (hh + 1) * 512)
                P.op("act", lambda e: e.activation(out=sB[:, hs], in_=pB[hh][:], func=AF.Sigmoid),
                     reads=[pB[hh]], writes=[(sB, hh)])
                P.op("pool", lambda e: e.tensor_tensor(out=sB[:, hs], in0=sB[:, hs], in1=An[:, hs], op=ALU.mult),
                     reads=[(sB, hh), (An, hh)], writes=[(sB, hh)])
                P.op("pool", lambda e: e.tensor_tensor(out=ot[:, hs], in0=sB[:, hs], in1=h2[:, hs], op=ALU.add),
                     reads=[(sB, hh), (h2, hh)], writes=[(ot, hh)])
            P.dma("sp", h_out[r0:r0 + 128, :], ot[:], s_out, reads=[ot])

    for sg in range(NG // SUB):
        for u in range(SUB):
            secA(sg * SUB + u, u)
        for ei in range(n_exp):
            ci = (sg * n_exp + ei) * 6
            ensure(ci + 7)
            for u in range(SUB):
                secE(sg * SUB + u, u, ei, ci)
        for u in range(SUB):
            secBD(sg * SUB + u, u)
        for u in range(SUB):
            secC(sg * SUB + u, u)
    return None


def build_ffn(NT, n_part=2, n_exp=NE, stop=None):
    P = Prog()
    C = Ctx(P)
    din = lambda n, s: P.dram(n, s, F32, "ExternalInput")
    h_in = din("h_in", [NT, D])
    parts = [din(f"part{i}", [NT, D]) for i in range(n_part)]
    p_in = din("p_in", [NT, 256])
    a = {}
    for n, shp in (("norm_ffn", [D]), ("router_w", [D, NE]), ("router_b", [NE]), ("w_gu", [n_exp, D, 2 * D]), ("b_gu", [NE, 2 * D]),
                   ("w_dn", [n_exp, D, D]), ("b_dn", [NE, D]), ("ple_w", [256, D]), ("ple_norm", [D]), ("ple_gate_norm", [D]),
                   ("ple_gate_w", [D, D])):
        a[n] = din(n, shp).ap()
    h_out = P.dram("h_out", [NT, D], F32, "ExternalOutput")
    emit_ffn(P, C, NT, h_in.ap(), [x.ap() for x in parts], p_in.ap(), a["norm_ffn"], a["router_w"], a["router_b"], a["w_gu"], a["b_gu"],
             a["w_dn"], a["b_dn"], a["ple_w"], a["ple_norm"], a["ple_gate_norm"], a["ple_gate_w"], h_out.ap(), n_exp=n_exp, stop=stop)
    print("ffn program: n_inst", P.n_inst)
    return P.finish(P.dsems)


class Phase:
    def __init__(self, P):
        self.P = P

    def __enter__(self):
        self.saved = self.P.es
        self.P.es = ExitStack()
        return self

    def __exit__(self, *a):
        P = self.P
        P.barrier()
        P.es.close()
        P.es = self.saved
        return False


def _barrier(self):
    sems = [E.sem for E in self.engs.values()] + list(self.dsems)
    for E in self.engs.values():
        for s in sems:
            if s is E.sem:
                continue
            if s.issued > 0 and E.known.get(s, 0) < s.issued:
                E.e.wait_ge(s.h, s.issued)
                E.known[s] = s.issued
                if s.is_dma and s.issued > s.last_wait:
                    s.last_wait = s.issued


Prog.barrier = _barrier
_old_dsem = Prog.dsem


def _dsem(self, name):
    if not hasattr(self, "dsem_by_name"):
        self.dsem_by_name = {}
    if name in self.dsem_by_name:
        return self.dsem_by_name[name]
    s = self.dsem_by_name[name] = Sem(self.gstack.enter_context(self.nc.semaphore(name)), name, is_dma=True)
    self.dsems.append(s)
    return s


Prog.dsem = _dsem


class NormT:
    def __init__(self, P, C, tag):
        self.P, self.C = P, C
        self.hb = [P.sb(f"nt_hb{i}_{tag}", [128, D], F32) for i in range(2)]
        self.sem = [P.dsem(f"d_nt{i}_{tag}") for i in range(2)]
        self.xnb = P.sb(f"nt_xnb_{tag}", [128, D], BF16)
        self.i = 0

    def emit(self, src_ap, src_deps, gain, dst, dst_ap, dst_key):
        P, C = self.P, self.C
        hb, sem = self.hb[self.i % 2], self.sem[self.i % 2]
        self.i += 1
        P.dma("sp", hb[:], src_ap, sem, reads=src_deps, writes=[hb])
        ss, rstd = C.stat("ss"), C.stat("rstd")
        P.op("act", lambda e: e.activation(out=C.junk[:], in_=hb[:], func=AF.Square, accum_out=ss[:]),
             reads=[hb], writes=[C.junk, ss])
        emit_rstd(P, C, (ss, ss[:]), (rstd, rstd[:]), D)
        P.op("dve", lambda e: e.scalar_tensor_tensor(out=self.xnb[:], in0=hb[:], scalar=rstd[:, 0:1], in1=gain[:],
                                                     op0=ALU.mult, op1=ALU.mult),
             reads=[hb, rstd, gain], writes=[self.xnb])
        for k in range(8):
            P.op("pe", lambda e: e.transpose(out=C.trb[:, k * 128:(k + 1) * 128], in_=self.xnb[:, k * 128:(k + 1) * 128],
                                             identity=C.identb[:]), reads=[self.xnb, C.identb], writes=[C.trb])
        P.op("act", lambda e: e.copy(out=dst_ap, in_=C.trb[:].rearrange("p (k t) -> p k t", k=8)),
             reads=[C.trb], writes=[(dst, dst_key)])


def emit_outproj(P, C, yT, Wout, ntile, dst_rows_fn, dst_tile, osem, obufs, cnt):
    for j in range(ntile):
        ot = obufs[cnt[0] % len(obufs)]
        cnt[0] += 1
        for hh in range(2):
            po = C.bank()
            for k in range(8):
                P.op("pe", lambda e: e.matmul(out=po[:], lhsT=yT[:, k, j * 128:(j + 1) * 128],
                                              rhs=Wout[:, k, hh * 512:(hh + 1) * 512], start=(k == 0), stop=(k == 7)),
                     reads=[yT, Wout], writes=[po])
            if hh == 0:
                P.op("act", lambda e: e.copy(out=ot[:, 0:512], in_=po[:]), reads=[po], writes=[(ot, 0)])
            else:
                P.op("dve", lambda e: e.tensor_copy(out=ot[:, 512:1024], in_=po[:]), reads=[po], writes=[(ot, 1)])
        P.dma("sp", dst_rows_fn(j), ot[:], osem, reads=[ot], writes=[dst_tile] if dst_tile is not None else None)


def emit_hgrn2(P, C, S, layer, h_src, h_deps, mix_dst, mix_tile, g_mix_ap, w_in, w_out, gnorm_ap, lb_param):
    ST = 512
    with Phase(P):
        s_w = P.dsem("d_hgw")
        s_c = P.dsem("d_hgc")
        s_o = P.dsem("d_hgo")
        Win = P.sb("hg_win", [128, 8, 4 * D], BF16)
        for q in range(4):
            load_w_bf16(P, Win, Win[:, :, q * D:(q + 1) * D], w_in[:, q * D:(q + 1) * D], s_w, key=q)
        Wout = P.sb("hg_wout", [128, 8, D], BF16)
        load_w_bf16(P, Wout, Wout[:], w_out, s_w)
        g_mix = bc_load(P, "sp", "hg_gmix", g_mix_ap, D, s_c)
        gn = P.sb("hg_gn", [128, 1], F32)
        P.dma("sp", gn[:], gnorm_ap.rearrange("(p o) -> p o", o=1), s_c, writes=[gn])
        lbrow = P.sb("hg_lbrow", [32, 128], F32)
        P.dma("sp", lbrow[:], lb_param.rearrange("l (h p) -> (l h) p", p=128), s_c, writes=[lbrow])
        P.op("pe", lambda e: e.transpose(out=C.small[:, 0:32], in_=lbrow[:], identity=C.identf[0:32, 0:32]),
             reads=[lbrow, C.identf], writes=[C.small])
        el = P.sb("hg_el", [128, 4, 8], F32)
        P.op("act", lambda e: e.activation(out=el[:], in_=C.small[:, 0:32].rearrange("p (l h) -> p l h", l=4), func=AF.Exp),
             reads=[C.small], writes=[el])
        den = P.sb("hg_den", [128, 8], F32)
        num = P.sb("hg_num", [128, 8], F32)
        P.op("dve", lambda e: e.tensor_tensor(out=den[:], in0=el[:, 0, :], in1=el[:, 1, :], op=ALU.add), reads=[el], writes=[den])
        P.op("dve", lambda e: e.tensor_tensor(out=den[:], in0=den[:], in1=el[:, 2, :], op=ALU.add), reads=[el, den], writes=[den])
        P.op("dve", lambda e: e.tensor_tensor(out=den[:], in0=den[:], in1=el[:, 3, :], op=ALU.add), reads=[el, den], writes=[den])
        P.op("dve", lambda e: e.memset(num[:], 0.0), writes=[num])
        for l in range(1, layer + 1):
            P.op("dve", lambda e: e.tensor_tensor(out=num[:], in0=num[:], in1=el[:, l, :], op=ALU.add), reads=[el, num], writes=[num])
        P.op("dve", lambda e: e.reciprocal(out=den[:], in_=den[:]), reads=[den], writes=[den])
        lb = P.sb("hg_lb", [128, 8], F32)
        oml = P.sb("hg_oml", [128, 8], F32)
        noml = P.sb("hg_noml", [128, 8], F32)
        P.op("dve", lambda e: e.tensor_tensor(out=lb[:], in0=num[:], in1=den[:], op=ALU.mult), reads=[num, den], writes=[lb])
        P.op("dve", lambda e: e.tensor_scalar(out=oml[:], in0=lb[:], scalar1=-1.0, scalar2=1.0, op0=ALU.mult, op1=ALU.add),
             reads=[lb], writes=[oml])
        P.op("dve", lambda e: e.tensor_scalar(out=noml[:], in0=oml[:], scalar1=-1.0, scalar2=None, op0=ALU.mult),
             reads=[oml], writes=[noml])
        mask01 = P.sb("hg_mask01", [128, ST], F32)
        P.op("pool", lambda e: e.memset(mask01[:], 1.0), writes=[mask01])
        for c in range(ST // 64):
            P.op("pool", lambda e: e.memset(mask01[:, c * 64:c * 64 + 1], 0.0), reads=[mask01], writes=[mask01])
        mc = P.sb("hg_mc", [64, ST], F32)
        P.op("pool", lambda e: e.memset(mc[:], 1.0), writes=[mc])
        P.op("pool", lambda e: e.affine_select(out=mc[:], in_=mc[:], pattern=[[0, ST // 64], [1, 64]], compare_op=ALU.is_ge,
                                               fill=0.0, base=0, channel_multiplier=-1), reads=[mc], writes=[mc])
        onesf = P.sb("hg_ones", [128, 128], F32)
        P.op("pool", lambda e: e.memset(onesf[:], 1.0), writes=[onesf])
        Sf = P.sb("hg_Sf", [128, 8, 128], F32)
        Sb = P.sb("hg_Sb", [128, 8, 128], BF16)
        P.op("pool", lambda e: e.memset(Sf[:], 0.0), writes=[Sf])
        P.op("pool", lambda e: e.memset(Sb[:], 0.0), writes=[Sb])
        Tt = P.sb("hg_T", [128, 128], F32)
        xnT = P.sb("hg_xnT", [128, 8, ST], BF16)
        v_sb = P.sb("hg_v", [64, ST // 64, D], BF16)
        f32t = {n: P.sb("hg_" + n, [128, ST], F32) for n in ["sg", "lf", "cum", "A", "Ai", "qs", "kk", "sgate", "sq", "rr"]}
        qt = P.sb("hg_qt", [128, ST], BF16)
        kt = P.sb("hg_kt", [128, ST], BF16)
        ktm = P.sb("hg_ktm", [64, ST // 64, 128], BF16)
        scm = P.sb("hg_scm", [64, ST], BF16)
        ogT = P.sb("hg_ogT", [128, 8, ST], BF16)
        obufs = [P.sb(f"hg_ob{i}", [128, D], F32) for i in range(2)]
        ocnt = [0]
        nt = NormT(P, C, "hg")
        oT, dS, ssps = C.trp[0], C.trp[1], C.small
        NCH = ST // 64
        for st in range(S // ST):
            t0 = st * ST
            for j in range(ST // 128):
                nt.emit(h_src[t0 + j * 128:t0 + (j + 1) * 128, :], h_deps, g_mix, xnT, xnT[:, :, j * 128:(j + 1) * 128], j)
            for c in range(NCH):
                for hh in range(2):
                    pv = C.bank()
                    for k in range(8):
                        P.op("pe", lambda e: e.matmul(out=pv[0:64, :], lhsT=xnT[:, k, c * 64:(c + 1) * 64],
                                                      rhs=Win[:, k, 2 * D + hh * 512:2 * D + (hh + 1) * 512],
                                                      start=(k == 0), stop=(k == 7)), reads=[xnT, (Win, 2)], writes=[pv])
                    if hh == 0:
                        P.op("act", lambda e: e.copy(out=v_sb[:, c, 0:512], in_=pv[0:64, :]), reads=[pv], writes=[(v_sb, (c, 0))])
                    else:
                        P.op("dve", lambda e: e.tensor_copy(out=v_sb[:, c, 512:1024], in_=pv[0:64, :]), reads=[pv],
                             writes=[(v_sb, (c, 1))])
            for H in range(8):
                Hs = slice(H * 128, (H + 1) * 128)
                pq, pz, pg = C.bank(), C.bank(), C.bank()
                for (pp, off, wk) in ((pq, 0, 0), (pz, D, 1), (pg, 3 * D, 3)):
                    for k in range(8):
                        P.op("pe", lambda e: e.matmul(out=pp[:], lhsT=Win[:, k, off + H * 128:off + (H + 1) * 128], rhs=xnT[:, k, :],
                                                      start=(k == 0), stop=(k == 7)), reads=[xnT, (Win, wk)], writes=[pp])
                T = f32t
                P.op("act", lambda e: e.activation(out=T["sg"][:], in_=pz[:], func=AF.Sigmoid), reads=[pz], writes=[T["sg"]])
                P.op("act", lambda e: e.activation(out=T["qs"][:], in_=pq[:], func=AF.Silu), reads=[pq], writes=[T["qs"]])
                P.op("act", lambda e: e.activation(out=T["sgate"][:], in_=pg[:], func=AF.Silu), reads=[pg], writes=[T["sgate"]])
                P.op("dve", lambda e: e.tensor_scalar(out=T["lf"][:], in0=T["sg"][:], scalar1=oml[:, H:H + 1], scalar2=lb[:, H:H + 1],
                                                      op0=ALU.mult, op1=ALU.add), reads=[T["sg"], oml, lb], writes=[T["lf"]])
                P.op("act", lambda e: e.activation(out=T["lf"][:], in_=T["lf"][:], func=AF.Ln), reads=[T["lf"]], writes=[T["lf"]])
                P.op("dve", lambda e: e.tensor_tensor_scan(out=T["cum"][:], data0=mask01[:], data1=T["lf"][:], initial=0.0,
                                                           op0=ALU.mult, op1=ALU.add), reads=[mask01, T["lf"]], writes=[T["cum"]])
                P.op("act", lambda e: e.activation(out=T["A"][:], in_=T["cum"][:], func=AF.Exp), reads=[T["cum"]], writes=[T["A"]])
                P.op("act", lambda e: e.activation(out=T["Ai"][:], in_=T["cum"][:], func=AF.Exp, scale=-1.0),
                     reads=[T["cum"]], writes=[T["Ai"]])
                P.op("dve", lambda e: e.scalar_tensor_tensor(out=qt[:], in0=T["qs"][:], scalar=128.0 ** -0.5, in1=T["A"][:],
                                                             op0=ALU.mult, op1=ALU.mult), reads=[T["qs"], T["A"]], writes=[qt])
                P.op("dve", lambda e: e.tensor_scalar(out=T["kk"][:], in0=T["sg"][:], scalar1=noml[:, H:H + 1], scalar2=oml[:, H:H + 1],
                                                      op0=ALU.mult, op1=ALU.add), reads=[T["sg"], noml, oml], writes=[T["kk"]])
                P.op("pool", lambda e: e.tensor_tensor(out=kt[:], in0=T["kk"][:], in1=T["Ai"][:], op=ALU.mult),
                     reads=[T["kk"], T["Ai"]], writes=[kt])
                for c in range(NCH):
                    P.op("pe", lambda e: e.transpose(out=C.trb[0:64, c * 128:(c + 1) * 128], in_=kt[:, c * 64:(c + 1) * 64],
                                                     identity=C.identb[:]), reads=[kt, C.identb], writes=[C.trb])
                P.op("act", lambda e: e.copy(out=ktm[:], in_=C.trb[0:64, :].rearrange("p (c d) -> p c d", c=NCH)),
                     reads=[C.trb], writes=[ktm])
                sc = C.bank()
                for c in range(NCH):
                    cs = slice(c * 64, (c + 1) * 64)
                    P.op("pe", lambda e: e.matmul(out=sc[0:64, cs], lhsT=kt[:, cs], rhs=qt[:, cs], start=True, stop=True),
                         reads=[kt, qt], writes=[sc])
                P.op("dve", lambda e: e.tensor_tensor(out=scm[:], in0=sc[0:64, :], in1=mc[:], op=ALU.mult),
                     reads=[sc, mc], writes=[scm])
                for c in range(NCH):
                    cs = slice(c * 64, (c + 1) * 64)
                    P.op("pe", lambda e: e.matmul(out=oT[:, cs], lhsT=v_sb[:, c, Hs], rhs=scm[:, cs], start=True, stop=False),
                         reads=[v_sb, scm], writes=[oT])
                    P.op("pe", lambda e: e.matmul(out=oT[:, cs], lhsT=Sb[:, H, :], rhs=qt[:, cs], start=False, stop=True),
                         reads=[(Sb, H), qt], writes=[oT])
                    P.op("pe", lambda e: e.matmul(out=dS[:, 0:128], lhsT=ktm[:, c, :], rhs=v_sb[:, c, Hs], start=True, stop=True),
                         reads=[ktm, v_sb], writes=[dS])
                    acol = T["A"][:, c * 64 + 63:c * 64 + 64]
                    P.op("dve", lambda e: e.tensor_tensor(out=Tt[:], in0=dS[:, 0:128], in1=Sf[:, H, :], op=ALU.add),
                         reads=[dS, (Sf, H)], writes=[Tt])
                    P.op("dve", lambda e: e.tensor_scalar(out=Sf[:, H, :], in0=Tt[:], scalar1=acol, scalar2=None, op0=ALU.mult),
                         reads=[Tt, T["A"]], writes=[(Sf, H)])
                    P.op("act", lambda e: e.activation(out=Sb[:, H, :], in_=Tt[:], func=AF.Copy, scale=acol),
                         reads=[Tt, T["A"]], writes=[(Sb, H)])
                P.op("act", lambda e: e.activation(out=T["sq"][:], in_=oT[:], func=AF.Square), reads=[oT], writes=[T["sq"]])
                P.op("pe", lambda e: e.matmul(out=ssps[:], lhsT=onesf[:], rhs=T["sq"][:], start=True, stop=True),
                     reads=[onesf, T["sq"]], writes=[ssps])
                emit_rstd(P, C, (ssps, ssps[:]), (T["rr"], T["rr"][:]), 128)
                P.op("dve", lambda e: e.tensor_tensor(out=T["sq"][:], in0=oT[:], in1=T["rr"][:], op=ALU.mult),
                     reads=[oT, T["rr"]], writes=[T["sq"]])
                P.op("dve", lambda e: e.scalar_tensor_tensor(out=ogT[:, H, :], in0=T["sq"][:], scalar=gn[:, 0:1], in1=T["sgate"][:],
                                                             op0=ALU.mult, op1=ALU.mult),
                     reads=[T["sq"], gn, T["sgate"]], writes=[(ogT, H)])
            emit_outproj(P, C, ogT, Wout, ST // 128, lambda j: mix_dst[t0 + j * 128:t0 + (j + 1) * 128, :], mix_tile, s_o,
                         obufs, ocnt)


def emit_rglru(P, C, S, h_src, h_deps, mix_dst, mix_tile, g_mix_ap, w_in, conv_w, conv_b, w_a, b_a, w_x, b_x, lam, w_out):
    ST = 512
    with Phase(P):
        s_w = P.dsem("d_rgw")
        s_c = P.dsem("d_rgc")
        s_o = P.dsem("d_rgo")
        Win = P.sb("rg_win", [128, 8, 2 * D], BF16)
        for q in range(2):
            load_w_bf16(P, Win, Win[:, :, q * D:(q + 1) * D], w_in[:, q * D:(q + 1) * D], s_w, key=q)
        Wout = P.sb("rg_wout", [128, 8, D], BF16)
        load_w_bf16(P, Wout, Wout[:], w_out, s_w)
        wa = P.sb("rg_wa_sb", [128, 8, 256], BF16)
        wx = P.sb("rg_wx_sb", [128, 8, 256], BF16)
        P.dma("pool", wa[:], w_a.rearrange("n (dh p) e -> p (n dh) e", p=128), s_w, writes=[wa])
        P.dma("pool", wx[:], w_x.rearrange("n (dh p) e -> p (n dh) e", p=128), s_w, writes=[wx])
        g_mix = bc_load(P, "sp", "rg_gmix", g_mix_ap, D, s_c)
        vrow = P.sb("rg_vrow", [64, 128], F32)
        P.dma("sp", vrow[0:32, :], conv_w.rearrange("t (k p) -> (t k) p", p=128), s_c, writes=[vrow])
        for i, v in enumerate((conv_b, b_a, b_x, lam)):
            P.dma("sp", vrow[32 + 8 * i:40 + 8 * i, :], v.rearrange("(k p) -> k p", p=128), s_c, writes=[vrow])
        P.op("pe", lambda e: e.transpose(out=C.small[:, 0:64], in_=vrow[:], identity=C.identf[0:64, 0:64]),
             reads=[vrow, C.identf], writes=[C.small])
        vT = P.sb("rg_vT", [128, 64], F32)
        P.op("dve", lambda e: e.tensor_copy(out=vT[:], in_=C.small[:, 0:64]), reads=[C.small], writes=[vT])
        cw = lambda t, k: vT[:, t * 8 + k:t * 8 + k + 1]
        cb = lambda k: vT[:, 32 + k:33 + k]
        ba = lambda k: vT[:, 40 + k:41 + k]
        bx = lambda k: vT[:, 48 + k:49 + k]
        cl = P.sb("rg_cl", [128, 8], F32)
        P.op("act", lambda e: e.activation(out=cl[:], in_=vT[:, 56:64], func=AF.Exp, scale=-1.0), reads=[vT], writes=[cl])
        P.op("act", lambda e: e.activation(out=cl[:], in_=cl[:], func=AF.Ln, bias=1.0), reads=[cl], writes=[cl])
        P.op("dve", lambda e: e.tensor_scalar(out=cl[:], in0=cl[:], scalar1=-8.0, scalar2=None, op0=ALU.mult),
             reads=[cl], writes=[cl])
        xnT = P.sb("rg_xnT", [128, 8, ST], BF16)
        gate = P.sb("rg_gate", [128, 8, ST], F32)
        ubuf = [P.sb(f"rg_ubuf{i}", [128, 8, ST + 3], F32) for i in range(2)]
        P.op("pool", lambda e: e.memset(ubuf[1][:], 0.0), writes=[ubuf[1]])
        ucf = P.sb("rg_ucf", [128, 8, ST], F32)
        ucb = P.sb("rg_ucb", [128, 8, ST], BF16)
        tt = [P.sb(f"rg_t{i}", [128, ST], F32) for i in range(6)]
        ti = [0]

        def tmp():
            t = tt[ti[0] % len(tt)]
            ti[0] += 1
            return t
        hlast = P.sb("rg_hlast", [128, 8], F32)
        P.op("pool", lambda e: e.memset(hlast[:], 0.0), writes=[hlast])
        yT = P.sb("rg_yT", [128, 8, ST], BF16)
        obufs = [P.sb(f"rg_ob{i}", [128, D], F32) for i in range(2)]
        ocnt = [0]
        nt = NormT(P, C, "rg")
        for st in range(S // ST):
            t0 = st * ST
            ub, ubp = ubuf[st % 2], ubuf[(st + 1) % 2]
            for j in range(ST // 128):
                nt.emit(h_src[t0 + j * 128:t0 + (j + 1) * 128, :], h_deps, g_mix, xnT, xnT[:, :, j * 128:(j + 1) * 128], j)
            for kc in range(8):
                ks = slice(kc * 128, (kc + 1) * 128)
                pg, pu = C.bank(), C.bank()
                for k in range(8):
                    P.op("pe", lambda e: e.matmul(out=pg[:], lhsT=Win[:, k, ks], rhs=xnT[:, k, :], start=(k == 0), stop=(k == 7)),
                         reads=[xnT, (Win, 0)], writes=[pg])
                for k in range(8):
                    P.op("pe", lambda e: e.matmul(out=pu[:], lhsT=Win[:, k, D + kc * 128:D + (kc + 1) * 128], rhs=xnT[:, k, :],
                                                  start=(k == 0), stop=(k == 7)), reads=[xnT, (Win, 1)], writes=[pu])
                t1, t2 = tmp(), tmp()
                P.op("act", lambda e: e.activation(out=t1[:], in_=pg[:], func=AF.Square), reads=[pg], writes=[t1])
                P.op("dve", lambda e: e.tensor_scalar(out=t1[:], in0=t1[:], scalar1=0.044715, scalar2=1.0, op0=ALU.mult, op1=ALU.add),
                     reads=[t1], writes=[t1])
                P.op("dve", lambda e: e.tensor_tensor(out=t1[:], in0=t1[:], in1=pg[:], op=ALU.mult), reads=[t1, pg], writes=[t1])
                P.op("act", lambda e: e.activation(out=t2[:], in_=t1[:], func=AF.Sigmoid, scale=1.5957691216),
                     reads=[t1], writes=[t2])
                P.op("dve", lambda e: e.tensor_tensor(out=gate[:, kc, :], in0=t2[:], in1=pg[:], op=ALU.mult),
                     reads=[t2, pg], writes=[(gate, kc)])
                P.op("pool", lambda e: e.tensor_copy(out=ub[:, kc, 0:3], in_=ubp[:, kc, ST:ST + 3]),
                     reads=[(ubp, kc)], writes=[(ub, kc)])
                P.op("act", lambda e: e.copy(out=ub[:, kc, 3:ST + 3], in_=pu[:]), reads=[pu], writes=[(ub, kc)])
                P.op("dve", lambda e: e.tensor_scalar(out=ucf[:, kc, :], in0=ub[:, kc, 0:ST], scalar1=cw(0, kc), scalar2=cb(kc),
                                                      op0=ALU.mult, op1=ALU.add), reads=[(ub, kc), vT], writes=[(ucf, kc)])
                for tap in range(1, 4):
                    P.op("dve", lambda e: e.scalar_tensor_tensor(out=ucf[:, kc, :], in0=ub[:, kc, tap:tap + ST], scalar=cw(tap, kc),
                                                                 in1=ucf[:, kc, :], op0=ALU.mult, op1=ALU.add),
                         reads=[(ub, kc), vT, (ucf, kc)], writes=[(ucf, kc)])
                P.op("pool", lambda e: e.tensor_copy(out=ucb[:, kc, :], in_=ucf[:, kc, :]), reads=[(ucf, kc)], writes=[(ucb, kc)])
            for oc in range(8):
                n, eh = oc // 2, oc % 2
                es_ = slice(eh * 128, (eh + 1) * 128)
                pr, pi = C.bank(), C.bank()
                for dh in range(2):
                    P.op("pe", lambda e: e.matmul(out=pr[:], lhsT=wa[:, 2 * n + dh, es_], rhs=ucb[:, 2 * n + dh, :],
                                                  start=(dh == 0), stop=(dh == 1)), reads=[wa, (ucb, 2 * n + dh)], writes=[pr])
                for dh in range(2):
                    P.op("pe", lambda e: e.matmul(out=pi[:], lhsT=wx[:, 2 * n + dh, es_], rhs=ucb[:, 2 * n + dh, :],
                                                  start=(dh == 0), stop=(dh == 1)), reads=[wx, (ucb, 2 * n + dh)], writes=[pi])
                r, ig, a, om = tmp(), tmp(), tmp(), tmp()
                P.op("act", lambda e: e.activation(out=r[:], in_=pr[:], func=AF.Sigmoid, bias=ba(oc)), reads=[pr, vT], writes=[r])
                P.op("act", lambda e: e.activation(out=ig[:], in_=pi[:], func=AF.Sigmoid, bias=bx(oc)), reads=[pi, vT], writes=[ig])
                P.op("act", lambda e: e.activation(out=a[:], in_=r[:], func=AF.Exp, scale=cl[:, oc:oc + 1]), reads=[r, cl], writes=[a])
                P.op("pool", lambda e: e.tensor_tensor(out=om[:], in0=a[:], in1=a[:], op=ALU.mult), reads=[a], writes=[om])
                P.op("dve", lambda e: e.tensor_scalar(out=om[:], in0=om[:], scalar1=-1.0, scalar2=1.0, op0=ALU.mult, op1=ALU.add),
                     reads=[om], writes=[om])
                P.op("act", lambda e: e.activation(out=om[:], in_=om[:], func=AF.Sqrt), reads=[om], writes=[om])
                P.op("pool", lambda e: e.tensor_tensor(out=ig[:], in0=ig[:], in1=ucf[:, oc, :], op=ALU.mult),
                     reads=[ig, (ucf, oc)], writes=[ig])
                P.op("dve", lambda e: e.tensor_tensor(out=ig[:], in0=ig[:], in1=om[:], op=ALU.mult), reads=[ig, om], writes=[ig])
                P.op("dve", lambda e: e.tensor_tensor_scan(out=r[:], data0=a[:], data1=ig[:], initial=hlast[:, oc:oc + 1],
                                                           op0=ALU.mult, op1=ALU.add), reads=[a, ig, hlast], writes=[r])
                P.op("pool", lambda e: e.tensor_copy(out=hlast[:, oc:oc + 1], in_=r[:, ST - 1:ST]), reads=[r], writes=[hlast])
                P.op("dve", lambda e: e.tensor_tensor(out=yT[:, oc, :], in0=r[:], in1=gate[:, oc, :], op=ALU.mult),
                     reads=[r, (gate, oc)], writes=[(yT, oc)])
            emit_outproj(P, C, yT, Wout, ST // 128, lambda j: mix_dst[t0 + j * 128:t0 + (j + 1) * 128, :], mix_tile, s_o,
                         obufs, ocnt)


def emit_fox(P, C, S, h_src, h_deps, mix_dst, mix_tile, g_mix_ap, w_in, f_bias, q_norm, k_norm, w_out):
    ST = 512
    NH, HD = 16, 64
    NT_ = S // 128
    NQ = S // ST
    nc = P.nc
    QTd = nc.dram_tensor("fox_QTd", [NH * HD, S], BF16, kind="Internal").ap()
    KTd = nc.dram_tensor("fox_KTd", [NH * HD, S], BF16, kind="Internal").ap()
    Vd = nc.dram_tensor("fox_Vd", [NH, 128, NT_ * 65], BF16, kind="Internal").ap()
    SGd = nc.dram_tensor("fox_SGd", [S, D], F32, kind="Internal").ap()
    Od = nc.dram_tensor("fox_Od", [S, D], F32, kind="Internal").ap()
    CUMd = nc.dram_tensor("fox_CUMd", [NH, S], F32, kind="Internal").ap()
    with Phase(P):
        s_w = P.dsem("d_fxw")
        s_c = P.dsem("d_fxc")
        s_o = [P.dsem(f"d_fxo{i}") for i in range(2)]
        Win = P.sb("fx_win", [128, 8, 4 * D + NH], BF16)
        for q in range(4):
            load_w_bf16(P, Win, Win[:, :, q * D:(q + 1) * D], w_in[:, q * D:(q + 1) * D], s_w, key=q)
        load_w_bf16(P, Win, Win[:, :, 4 * D:4 * D + NH], w_in[:, 4 * D:4 * D + NH], s_w, key=4)
        g_mix = bc_load(P, "sp", "fx_gmix", g_mix_ap, D, s_c)
        qn_bc = bc_load(P, "sp", "fx_qn", q_norm, HD, s_c)
        kn_bc = bc_load(P, "sp", "fx_kn", k_norm, HD, s_c)
        fb = P.sb("fx_fb", [NH, 1], F32)
        P.dma("sp", fb[:], f_bias.rearrange("(p o) -> p o", o=1), s_c, writes=[fb])
        ones16 = P.sb("fx_ones16", [NH, ST], F32)
        P.op("pool", lambda e: e.memset(ones16[:], 1.0), writes=[ones16])
        xnT = P.sb("fx_xnT", [128, 8, ST], BF16)
        cumst = [P.sb(f"fx_cumst{i}", [NH, ST], F32) for i in range(2)]
        P.op("pool", lambda e: e.memset(cumst[1][:], 0.0), writes=[cumst[1]])
        ls = P.sb("fx_ls", [NH, ST], F32)
        sq = P.sb("fx_sq", [128, ST], F32)
        qb = P.sb("fx_qb", [128, ST], BF16)
        ssq = P.sb("fx_ssq", [128, 8], F32)
        rs = P.sb("fx_rs", [128, 8], F32)
        Q2 = [P.sb(f"fx_Q2_{i}", [128, 8, 128], BF16) for i in range(2)]
        K2 = [P.sb(f"fx_K2_{i}", [128, 8, 128], BF16) for i in range(2)]
        vb = [P.sb(f"fx_vb{i}", [128, NH, 65], BF16) for i in range(2)]
        for i in range(2):
            P.op("pool", lambda e: e.memset(vb[i][:], 1.0), writes=[vb[i]])
        sgt = [P.sb(f"fx_sg{i}", [128, D], F32) for i in range(2)]
        nt = NormT(P, C, "fx")
        tix = 0
        for st in range(S // ST):
            t0 = st * ST
            for j in range(ST // 128):
                nt.emit(h_src[t0 + j * 128:t0 + (j + 1) * 128, :], h_deps, g_mix, xnT, xnT[:, :, j * 128:(j + 1) * 128], j)
            pf = C.bank()
            for k in range(8):
                P.op("pe", lambda e: e.matmul(out=pf[0:NH, :], lhsT=Win[:, k, 4 * D:4 * D + NH], rhs=xnT[:, k, :],
                                              start=(k == 0), stop=(k == 7)), reads=[xnT, (Win, 4)], writes=[pf])
            P.op("act", lambda e: e.activation(out=ls[:], in_=pf[0:NH, :], func=AF.Sigmoid, bias=fb[:, 0:1]),
                 reads=[pf, fb], writes=[ls])
            P.op("act", lambda e: e.activation(out=ls[:], in_=ls[:], func=AF.Ln), reads=[ls], writes=[ls])
            cs_, csp = cumst[st % 2], cumst[(st + 1) % 2]
            P.op("dve", lambda e: e.tensor_tensor_scan(out=cs_[:], data0=ones16[:], data1=ls[:], initial=csp[:, ST - 1:ST],
                                                       op0=ALU.mult, op1=ALU.add), reads=[ones16, ls, csp], writes=[cs_])
            P.dma("sp", CUMd[:, t0:t0 + ST], cs_[:], s_o[0], reads=[cs_])
            for j in range(ST // 128):
                r0 = t0 + j * 128
                tj = r0 // 128
                q2, k2, vbt, sg = Q2[tix % 2], K2[tix % 2], vb[tix % 2], sgt[tix % 2]
                osem = s_o[tix % 2]
                tix += 1
                for blk in range(8):
                    pp = C.bank()
                    for k in range(8):
                        P.op("pe", lambda e: e.matmul(out=pp[:], lhsT=xnT[:, k, j * 128:(j + 1) * 128],
                                                      rhs=Win[:, k, blk * 512:(blk + 1) * 512], start=(k == 0), stop=(k == 7)),
                             reads=[(xnT, j), (Win, blk // 2)], writes=[pp])
                    if blk < 4:
                        isq = blk < 2
                        P.op("act", lambda e: e.activation(out=sq[:], in_=pp[:], func=AF.Square), reads=[pp], writes=[sq])
                        P.op("dve", lambda e: e.tensor_reduce(out=ssq[:], in_=sq[:].rearrange("p (h d) -> p h d", d=HD), axis=AX.X,
                                                              op=ALU.add), reads=[sq], writes=[ssq])
                        emit_rstd(P, C, (ssq, ssq[:]), (rs, rs[:]), HD)
                        P.op("dve", lambda e: e.tensor_tensor(out=sq[:].rearrange("p (h d) -> p h d", d=HD),
                                                              in0=pp[:].rearrange("p (h d) -> p h d", d=HD),
                                                              in1=rs[:].unsqueeze(2).to_broadcast([128, 8, HD]), op=ALU.mult),
                             reads=[pp, rs], writes=[sq])
                        gbc = qn_bc if isq else kn_bc
                        P.op("dve", lambda e: e.scalar_tensor_tensor(out=qb[:].rearrange("p (h d) -> p h d", d=HD),
                                                                     in0=sq[:].rearrange("p (h d) -> p h d", d=HD),
                                                                     scalar=(1.0 if isq else HD ** -0.5),
                                                                     in1=gbc[:].unsqueeze(1).to_broadcast([128, 8, HD]),
                                                                     op0=ALU.mult, op1=ALU.mult), reads=[sq, gbc], writes=[qb])
                        for r in range(4):
                            P.op("pe", lambda e: e.transpose(out=C.trb[:, r * 128:(r + 1) * 128], in_=qb[:, r * 128:(r + 1) * 128],
                                                             identity=C.identb[:]), reads=[qb, C.identb], writes=[C.trb])
                        dst = q2 if isq else k2
                        P.op("act", lambda e: e.copy(out=dst[:, (blk % 2) * 4:(blk % 2) * 4 + 4, :],
                                                     in_=C.trb[:, 0:512].rearrange("p (r t) -> p r t", r=4)),
                             reads=[C.trb], writes=[(dst, blk % 2)])
                    elif blk < 6:
                        P.op("act", lambda e: e.copy(out=vbt[:, (blk - 4) * 8:(blk - 4) * 8 + 8, 0:HD],
                                                     in_=pp[:].rearrange("p (h d) -> p h d", d=HD)),
                             reads=[pp], writes=[(vbt, blk - 4)])
                    else:
                        P.op("act", lambda e: e.activation(out=sg[:, (blk - 6) * 512:(blk - 5) * 512], in_=pp[:], func=AF.Sigmoid),
                             reads=[pp], writes=[(sg, blk - 6)])
                P.dma("sp", QTd[:, r0:r0 + 128].rearrange("(pr p) t -> p pr t", p=128), q2[:], osem, reads=[q2])
                P.dma("sp", KTd[:, r0:r0 + 128].rearrange("(pr p) t -> p pr t", p=128), k2[:], osem, reads=[k2])
                P.dma("sp", Vd[:, :, tj * 65:(tj + 1) * 65].rearrange("h p c -> p h c"), vbt[:], osem, reads=[vbt])
                P.dma("sp", SGd[r0:r0 + 128, :], sg[:], osem, reads=[sg])
    with Phase(P):
        s_l = [P.dsem(f"d_fxl{i}") for i in range(2)]
        s_c2 = P.dsem("d_fxc2")
        s_o2 = P.dsem("d_fxo2")
        cumT = P.sb("fx_cumT", [NH, S], F32)
        P.dma("sp", cumT[:], CUMd, s_c2, writes=[cumT])
        cumtm = P.sb("fx_cumtm", [128, NT_, NH], F32)
        for t in range(NT_):
            P.op("pe", lambda e: e.transpose(out=C.small[:, 0:NH], in_=cumT[:, t * 128:(t + 1) * 128], identity=C.identf[0:NH, 0:NH]),
                 reads=[cumT, C.identf], writes=[C.small])
            P.op("dve", lambda e: e.tensor_copy(out=cumtm[:, t, :], in_=C.small[:, 0:NH]), reads=[C.small], writes=[(cumtm, t)])
        E0 = P.sb("fx_E0", [128, 128], F32)
        P.op("pool", lambda e: e.memset(E0[:], 0.0), writes=[E0])
        P.op("pool", lambda e: e.memset(E0[0:1, :], 1.0), reads=[E0], writes=[E0])
        Cbc = P.sb("fx_Cbc", [128, NQ, NH], F32)
        nCbc = P.sb("fx_nCbc", [128, NQ, NH], F32)
        for qt_ in range(NQ):
            P.op("pe", lambda e: e.matmul(out=C.small[:, 0:NH], lhsT=E0[:], rhs=cumtm[:, 4 * qt_, :], start=True, stop=True),
                 reads=[E0, (cumtm, 4 * qt_)], writes=[C.small])
            P.op("dve", lambda e: e.tensor_copy(out=Cbc[:, qt_, :], in_=C.small[:, 0:NH]), reads=[C.small], writes=[(Cbc, qt_)])
        P.op("dve", lambda e: e.tensor_scalar(out=nCbc[:], in0=Cbc[:], scalar1=-1.0, scalar2=None, op0=ALU.mult),
             reads=[Cbc], writes=[nCbc])
        Sel = P.sb("fx_Sel", [NH, NH, 128], F32)
        P.op("pool", lambda e: e.memset(Sel[:], 0.0), writes=[Sel])
        for col in (64, 96):
            P.op("dve", lambda e: e.tensor_copy(out=Sel[:, :, col], in_=C.identf[0:NH, 0:NH]), reads=[Sel, C.identf], writes=[Sel])
        mneg = P.sb("fx_mneg", [128, 4, ST], F32)
        P.op("pool", lambda e: e.memset(mneg[:], 0.0), writes=[mneg])
        for r in range(4):
            P.op("pool", lambda e: e.affine_select(out=mneg[:, r, :], in_=mneg[:, r, :], pattern=[[1, ST]], compare_op=ALU.is_ge,
                                                   fill=-30000.0, base=-r * 128, channel_multiplier=-1),
                 reads=[mneg], writes=[mneg])
        KT = [P.sb(f"fx_KT{i}", [128, S], BF16) for i in range(2)]
        QT = [P.sb(f"fx_QT{i}", [128, S], BF16) for i in range(2)]
        VV = [P.sb(f"fx_VV{i}", [128, NT_ * 65], BF16) for i in range(2)]
        for i in range(2):
            P.op("pool", lambda e: e.memset(KT[i][64:128, :], 0.0), writes=[KT[i]])
            P.op("pool", lambda e: e.memset(KT[i][64:65, :], 1.0), reads=[KT[i]], writes=[KT[i]])
            P.op("pool", lambda e: e.memset(KT[i][96:97, :], 1.0), reads=[KT[i]], writes=[KT[i]])
            P.op("pool", lambda e: e.memset(QT[i][64:128, :], 0.0), writes=[QT[i]])
        bias = P.sb("fx_bias", [128, NT_], F32)
        hi96 = P.sb("fx_hi96", [128, ST], BF16)
        smk = [P.sb(f"fx_smk{i}", [128, ST], F32) for i in range(2)]
        PT = [P.sb(f"fx_PT{i}", [128, ST], BF16) for i in range(3)]
        OTs = P.sb("fx_OTs", [65, ST], F32)
        rden = P.sb("fx_rden", [128, 4], F32)
        otm = [P.sb(f"fx_otm{i}", [128, 4, HD], F32) for i in range(2)]
        OTp, TRp = C.trp[0], C.trp[1]
        pti = 0
        for h in range(NH):
            kt_, qt2, vv, sl = KT[h % 2], QT[h % 2], VV[h % 2], s_l[h % 2]
            P.dma("sp", kt_[0:HD, :], KTd[h * HD:(h + 1) * HD, :], sl, writes=[kt_])
            P.dma("sp", qt2[0:HD, :], QTd[h * HD:(h + 1) * HD, :], sl, writes=[qt2])
            P.dma("sp", vv[:], Vd[h], sl, writes=[vv])
            for Q in range(NQ):
                qs_ = slice(Q * ST, (Q + 1) * ST)
                nkt = 4 * (Q + 1)
                pgm = C.bank()
                P.op("pe", lambda e: e.matmul(out=pgm[:], lhsT=Sel[:, h, :], rhs=cumT[:, qs_], start=True, stop=True),
                     reads=[Sel, cumT], writes=[pgm])
                P.op("act", lambda e: e.activation(out=qt2[64:65, qs_], in_=pgm[64:65, :], func=AF.Identity,
                                                   bias=nCbc[64:65, Q, h:h + 1]), reads=[pgm, nCbc], writes=[qt2])
                P.op("act", lambda e: e.activation(out=hi96[96:97, :], in_=pgm[96:97, :], func=AF.Identity,
                                                   bias=nCbc[96:97, Q, h:h + 1]), reads=[pgm, nCbc], writes=[hi96])
                P.op("dve", lambda e: e.scalar_tensor_tensor(out=qt2[96:97, qs_], in0=pgm[96:97, :], scalar=Cbc[96:97, Q, h:h + 1],
                                                             in1=hi96[96:97, :], op0=ALU.subtract, op1=ALU.subtract),
                     reads=[pgm, Cbc, hi96], writes=[qt2])
                P.op("dve", lambda e: e.tensor_scalar(out=bias[:, 0:nkt], in0=cumtm[:, 0:nkt, h], scalar1=-1.0,
                                                      scalar2=Cbc[:, Q, h:h + 1], op0=ALU.mult, op1=ALU.add),
                     reads=[cumtm, Cbc], writes=[bias])
                for kt in range(nkt):
                    ps_ = C.bank()
                    P.op("pe", lambda e: e.matmul(out=ps_[:], lhsT=kt_[:, kt * 128:(kt + 1) * 128], rhs=qt2[:, qs_],
                                                  start=True, stop=True), reads=[kt_, qt2], writes=[ps_])
                    pt = PT[pti % 3]
                    pti += 1
                    r = kt - 4 * Q
                    if r >= 0:
                        sm = smk[r % 2]
                        P.op("dve", lambda e: e.tensor_tensor(out=sm[:], in0=ps_[:], in1=mneg[:, r, :], op=ALU.add),
                             reads=[ps_, mneg], writes=[sm])
                        P.op("act", lambda e: e.activation(out=pt[:], in_=sm[:], func=AF.Exp, bias=bias[:, kt:kt + 1]),
                             reads=[sm, bias], writes=[pt])
                    else:
                        P.op("act", lambda e: e.activation(out=pt[:], in_=ps_[:], func=AF.Exp, bias=bias[:, kt:kt + 1]),
                             reads=[ps_, bias], writes=[pt])
                    P.op("pe", lambda e: e.matmul(out=OTp[0:65, :], lhsT=vv[:, kt * 65:(kt + 1) * 65], rhs=pt[:],
                                                  start=(kt == 0), stop=(kt == nkt - 1)), reads=[vv, pt], writes=[OTp])
                P.op("act", lambda e: e.copy(out=OTs[:], in_=OTp[0:65, :]), reads=[OTp], writes=[OTs])
                for r in range(4):
                    P.op("pe", lambda e: e.transpose(out=TRp[:, r * 128:r * 128 + 65], in_=OTs[:, r * 128:(r + 1) * 128],
                                                     identity=C.identf[0:65, 0:65]), reads=[OTs, C.identf], writes=[TRp])
                ot = otm[(h * NQ + Q) % 2]
                trv = TRp[:].rearrange("p (r c) -> p r c", r=4)
                P.op("dve", lambda e: e.reciprocal(out=rden[:], in_=trv[:, :, 64]), reads=[TRp], writes=[rden])
                P.op("dve", lambda e: e.tensor_tensor(out=ot[:], in0=trv[:, :, 0:HD], in1=rden[:].unsqueeze(2).to_broadcast([128, 4, HD]),
                                                      op=ALU.mult), reads=[TRp, rden], writes=[ot])
                P.dma("sp", Od[Q * ST:(Q + 1) * ST, h * HD:(h + 1) * HD].rearrange("(t p) d -> p t d", p=128), ot[:], s_o2, reads=[ot])
    with Phase(P):
        s_w3 = P.dsem("d_fxw3")
        s_l3 = [P.dsem(f"d_fxl3{i}") for i in range(2)]
        s_o3 = P.dsem("d_fxo3")
        Wout = P.sb("fx_wout", [128, 8, D], BF16)
        load_w_bf16(P, Wout, Wout[:], w_out, s_w3)
        ob_ = [P.sb(f"fx_o{i}", [128, D], F32) for i in range(2)]
        sb_ = [P.sb(f"fx_s{i}", [128, D], F32) for i in range(2)]
        yb = P.sb("fx_yb", [128, D], BF16)
        yT = P.sb("fx_yT", [128, 8, ST], BF16)
        obufs = [P.sb(f"fx_ob{i}", [128, D], F32) for i in range(2)]
        ocnt = [0]
        ti = 0
        for st in range(S // ST):
            t0 = st * ST
            for j in range(ST // 128):
                r0 = t0 + j * 128
                o_, s_, sl = ob_[ti % 2], sb_[ti % 2], s_l3[ti % 2]
                ti += 1
                P.dma("sp", o_[:], Od[r0:r0 + 128, :], sl, writes=[o_])
                P.dma("sp", s_[:], SGd[r0:r0 + 128, :], sl, writes=[s_])
                P.op("dve", lambda e: e.tensor_tensor(out=yb[:], in0=o_[:], in1=s_[:], op=ALU.mult), reads=[o_, s_], writes=[yb])
                for k in range(8):
                    P.op("pe", lambda e: e.transpose(out=C.trb[:, k * 128:(k + 1) * 128], in_=yb[:, k * 128:(k + 1) * 128],
                                                     identity=C.identb[:]), reads=[yb, C.identb], writes=[C.trb])
                P.op("act", lambda e: e.copy(out=yT[:, :, j * 128:(j + 1) * 128], in_=C.trb[:].rearrange("p (k t) -> p k t", k=8)),
                     reads=[C.trb], writes=[(yT, j)])
            emit_outproj(P, C, yT, Wout, ST // 128, lambda j: mix_dst[t0 + j * 128:t0 + (j + 1) * 128, :], mix_tile, s_o3,
                         obufs, ocnt)


DEPTH = 4
IN_SHAPES = {
    "norm_mix": [4, D], "norm_ffn": [4, D], "hg_w_in": [2, D, 4 * D], "hg_w_out": [2, D, D], "hg_gnorm": [2, 128],
    "hg_lb_param": [4, D], "fox_w_in": [1, D, 4 * D + 16], "fox_f_bias": [1, 16], "fox_qnorm": [1, 64], "fox_knorm": [1, 64],
    "fox_w_out": [1, D, D], "rg_w_in": [1, D, 2 * D], "rg_conv_w": [1, 4, D], "rg_conv_b": [1, D], "rg_wa": [1, 4, 256, 256],
    "rg_ba": [1, D], "rg_wx": [1, 4, 256, 256], "rg_bx": [1, D], "rg_lambda": [1, D], "rg_w_out": [1, D, D],
    "router_w": [4, D, NE], "router_b": [4, NE], "moe_w_gu": [4, NE, D, 2 * D], "moe_b_gu": [4, NE, 2 * D],
    "moe_w_dn": [4, NE, D, D], "moe_b_dn": [4, NE, D], "ple_w": [4, 256, D], "ple_norm": [4, D], "ple_gate_norm": [4, D],
    "ple_gate_w": [4, D, D],
}


def build_full(S, depth=DEPTH):
    P = Prog()
    C = Ctx(P)
    nc = P.nc
    a = {}
    x = P.dram("i_x", [S, D], F32, "ExternalInput").ap()
    p = P.dram("i_p", [4, S, 256], F32, "ExternalInput").ap()
    for n, shp in IN_SHAPES.items():
        a[n] = P.dram("i_" + n, shp, F32, "ExternalInput").ap()
    out = P.dram("o_h", [S, D], F32, "ExternalOutput").ap()
    hb = [nc.dram_tensor(f"hbuf{i}", [S, D], F32, kind="Internal").ap() for i in range(2)]
    mixd = nc.dram_tensor("mixd", [S, D], F32, kind="Internal").ap()
    hsrc = x
    for i in range(depth):
        kind, j = i % 3, i // 3
        if kind == 0:
            emit_hgrn2(P, C, S, i, hsrc, None, mixd, None, a["norm_mix"][i], a["hg_w_in"][j], a["hg_w_out"][j], a["hg_gnorm"][j],
                       a["hg_lb_param"])
        elif kind == 1:
            emit_fox(P, C, S, hsrc, None, mixd, None, a["norm_mix"][i], a["fox_w_in"][j], a["fox_f_bias"][j], a["fox_qnorm"][j],
                     a["fox_knorm"][j], a["fox_w_out"][j])
        else:
            emit_rglru(P, C, S, hsrc, None, mixd, None, a["norm_mix"][i], a["rg_w_in"][j], a["rg_conv_w"][j], a["rg_conv_b"][j],
                       a["rg_wa"][j], a["rg_ba"][j], a["rg_wx"][j], a["rg_bx"][j], a["rg_lambda"][j], a["rg_w_out"][j])
        dst = out if i == depth - 1 else hb[i % 2]
        emit_ffn(P, C, S, hsrc, [mixd], p[i], a["norm_ffn"][i], a["router_w"][i], a["router_b"][i], a["moe_w_gu"][i], a["moe_b_gu"][i],
                 a["moe_w_dn"][i], a["moe_b_dn"][i], a["ple_w"][i], a["ple_norm"][i], a["ple_gate_norm"][i], a["ple_gate_w"][i], dst)
        hsrc = dst
    n_inst = P.n_inst
    ncf = P.finish(P.dsems)
    return ncf, n_inst


_NC_CACHE = {}


def kernel(**inputs):
    x = np.ascontiguousarray(np.asarray(inputs["x"], dtype=np.float32))
    p = np.asarray(inputs["p"], dtype=np.float32)
    B, S, _ = x.shape
    if S not in _NC_CACHE:
        _NC_CACHE[S] = build_full(S)[0]
    nc = _NC_CACHE[S]
    shared = {"i_" + n: np.ascontiguousarray(np.asarray(inputs[n], dtype=np.float32)) for n in IN_SHAPES}
    in_maps = []
    for c in range(B):
        m = dict(shared)
        m["i_x"] = np.ascontiguousarray(x[c])
        m["i_p"] = np.ascontiguousarray(p[:, c])
        in_maps.append(m)
    res = run_bass_kernel_spmd(nc, in_maps, core_ids=list(range(B)))
    return np.stack([np.asarray(res.results[c]["o_h"], dtype=np.float32) for c in range(B)], axis=0)
```
